# Optimizing a Trainium2 kernel written in Bass

```python
import math
import jax, jax.numpy as jnp
from jax import lax
import numpy as np

D_MODEL = 2048
BATCH = 2
SEQ = 4096
DEPTH = 1
DEC_BATCH = 128
DEC_SEQ = 8
PAST_LEN = 2048
PAGE_SIZE = 128

ATT_GROUPS = ((128, 1), (512, 4), (2048, 16))
N_ATT_GROUPS = 3
ATT_HEADS = 4
ATT_HEAD_DIM = 128
ATT_WIDTH = N_ATT_GROUPS * ATT_HEADS * ATT_HEAD_DIM
ATT_OUT = ATT_HEADS * ATT_HEAD_DIM
ROPE_THETA = 10000.0
Q_BLOCK = 128
HGRN_KDIM = 128
HGRN_HEADS = D_MODEL // HGRN_KDIM
HGRN_VDIM = D_MODEL // HGRN_HEADS
HGRN_WIDTH = HGRN_HEADS * HGRN_KDIM
HGRN_VWIDTH = HGRN_HEADS * HGRN_VDIM
HGRN_CHUNK = 64
MEM_LEN = 256
CROSS_HEADS = 4
CROSS_HEAD_DIM = 128
CROSS_WIDTH = CROSS_HEADS * CROSS_HEAD_DIM
N_GROUPS = 4
EXPERTS_PER_GROUP = 8
N_EXPERTS = N_GROUPS * EXPERTS_PER_GROUP
EXPERT_FF = 512
TOP_K_INNER = 2
EPS = 1e-6
IN_SIZES = (ATT_WIDTH, ATT_WIDTH, ATT_WIDTH, HGRN_WIDTH, HGRN_WIDTH, HGRN_VWIDTH, HGRN_VWIDTH, D_MODEL, D_MODEL)
IN_WIDTH = 3 * ATT_WIDTH + 2 * HGRN_WIDTH + 2 * HGRN_VWIDTH + 2 * D_MODEL

kernel_name = 'hybrid_dilated_hgrn2_hmoe_step'


def _split_points(sizes):
    pts, acc = [], 0
    for s in sizes[:-1]:
        acc += s
        pts.append(acc)
    return pts


def _rmsnorm(x, g):
    xf = x.astype(jnp.float32)
    y = xf * lax.rsqrt(jnp.mean(xf * xf, axis=-1, keepdims=True) + EPS)
    return (y * g.astype(jnp.float32)).astype(x.dtype)


def _rope(x, pos):
    half = x.shape[-1] // 2
    inv_freq = ROPE_THETA ** (-jnp.arange(half, dtype=jnp.float32) / half)
    ang = pos.astype(jnp.float32)[:, None] * inv_freq[None, :]
    ang = ang.reshape((1, pos.shape[0]) + (1,) * (x.ndim - 3) + (half,))
    cos, sin = jnp.cos(ang), jnp.sin(ang)
    xf = x.astype(jnp.float32)
    x1, x2 = xf[..., :half], xf[..., half:]
    return jnp.concatenate([x1 * cos - x2 * sin, x2 * cos + x1 * sin], axis=-1).astype(x.dtype)


def _dilated_attend(q, k_all, v_all, q_off, window, dil):
    B, Tq, H, Dh = q.shape
    Tk = k_all.shape[1]
    n_taps = window // dil + 1
    qb = min(Q_BLOCK, Tq)
    nb = -(-Tq // qb)
    qp = jnp.pad(q, ((0, 0), (0, nb * qb - Tq), (0, 0), (0, 0)))
    q_blocks = qp.reshape(B, nb, qb, H, Dh).swapaxes(0, 1)
    starts = jnp.arange(nb, dtype=jnp.int32) * qb
    taps = jnp.arange(n_taps, dtype=jnp.int32) * dil
    scale = Dh ** -0.5

    def one_block(args):
        qblk, start = args
        kpos = q_off + start + jnp.arange(qb, dtype=jnp.int32)[:, None] - taps[None, :]
        valid = kpos >= 0
        idx = jnp.clip(kpos, 0, Tk - 1)
        kg = jnp.take(k_all, idx, axis=1).astype(jnp.float32)
        vg = jnp.take(v_all, idx, axis=1).astype(jnp.float32)
        s = jnp.einsum('bqhd,bqnhd->bqhn', qblk.astype(jnp.float32), kg) * scale
        s = jnp.where(valid[None, :, None, :], s, -jnp.inf)
        lse = jax.nn.logsumexp(s, axis=-1)
        p = jnp.exp(s - lse[..., None])
        o = jnp.einsum('bqhn,bqnhd->bqhd', p, vg)
        return o, lse

    o, lse = lax.map(one_block, (q_blocks, starts))
    o = o.swapaxes(0, 1).reshape(B, nb * qb, H, Dh)[:, :Tq]
    lse = lse.swapaxes(0, 1).reshape(B, nb * qb, H)[:, :Tq]
    return o, lse


def _hgrn2_scan(q, k, v, log_f, S0):
    B, T, H, Dk = q.shape
    C = min(HGRN_CHUNK, T)
    n = -(-T // C)
    pad = n * C - T

    def prep(a):
        a = jnp.pad(a.astype(jnp.float32), ((0, 0), (0, pad), (0, 0), (0, 0)))
        return a.reshape(B, n, C, H, a.shape[-1]).transpose(1, 0, 3, 2, 4)

    causal = jnp.tril(jnp.ones((C, C), dtype=bool))

    def step(S, inp):
        qc, kc, vc, gc = inp
        b = jnp.cumsum(gc, axis=2)
        o_inter = jnp.einsum('bhtk,bhkv->bhtv', qc * jnp.exp(b), S)
        rel = jnp.where(causal[None, None, :, :, None], b[:, :, :, None, :] - b[:, :, None, :, :], -jnp.inf)
        a = jnp.einsum('bhtk,bhsk,bhtsk->bhts', qc, kc, jnp.exp(rel))
        o = o_inter + jnp.einsum('bhts,bhsv->bhtv', a, vc)
        b_last = b[:, :, -1:, :]
        S_new = S * jnp.exp(b_last[:, :, 0, :])[..., None] + jnp.einsum('bhsk,bhsv->bhkv', kc * jnp.exp(b_last - b), vc)
        return S_new, o

    S_T, o = lax.scan(step, S0.astype(jnp.float32), (prep(q), prep(k), prep(v), prep(log_f)))
    o = o.transpose(1, 0, 3, 2, 4).reshape(B, n * C, H, v.shape[-1])[:, :T]
    return o, S_T


def _cross_attend(xn, mem_kv, w_cq, w_co):
    B, T, _ = xn.shape
    q = (xn @ w_cq).reshape(B, T, CROSS_HEADS, CROSS_HEAD_DIM).astype(jnp.float32)
    kv = mem_kv.astype(jnp.float32)
    s = jnp.einsum('bqhd,bmhd->bhqm', q, kv[:, :, 0]) * CROSS_HEAD_DIM ** -0.5
    p = jax.nn.softmax(s, axis=-1)
    o = jnp.einsum('bhqm,bmhd->bqhd', p, kv[:, :, 1]).reshape(B, T, CROSS_WIDTH).astype(xn.dtype)
    return o @ w_co


def _hier_moe(xn, w_rg, b_rg, w_re, b_re, w_e_gate, w_e_up, w_e_down):
    B, T, D = xn.shape
    xt = xn.reshape(B * T, D)
    pg = jax.nn.softmax((xt @ w_rg).astype(jnp.float32) + b_rg.astype(jnp.float32), axis=-1)
    p_top, g_idx = lax.top_k(pg, 1)
    le = ((xt @ w_re).astype(jnp.float32) + b_re.astype(jnp.float32)).reshape(-1, N_GROUPS, EXPERTS_PER_GROUP)
    le_sel = jnp.take_along_axis(le, g_idx[:, :, None], axis=1)[:, 0]
    v2, e_idx = lax.top_k(le_sel, TOP_K_INNER)
    w2 = jax.nn.softmax(v2, axis=-1) * p_top
    inner = jnp.sum(jax.nn.one_hot(e_idx, EXPERTS_PER_GROUP, dtype=jnp.float32) * w2[..., None], axis=1)
    cw = jax.nn.one_hot(g_idx[:, 0], N_GROUPS, dtype=jnp.float32)[:, :, None] * inner[:, None, :]
    y = jnp.zeros((B * T, D), jnp.float32)
    for g in range(N_GROUPS):
        h = jax.nn.silu(jnp.einsum('nd,edf->nef', xt, w_e_gate[g])) * jnp.einsum('nd,edf->nef', xt, w_e_up[g])
        h = (h.astype(jnp.float32) * cw[:, g, :, None]).astype(xt.dtype)
        y = y + jnp.einsum('nef,efd->nd', h, w_e_down[g]).astype(jnp.float32)
    return y.reshape(B, T, D).astype(xn.dtype)


def _layer(x, pos0, win_bufs, S0, mem_kv, layer, hgrn_lb_logits, norm_mix, w_in, w_proj_attn, w_proj_hgrn,
           w_out, hgrn_norm, norm_cross, w_cq, w_co, norm_ffn, w_rg, b_rg, w_re, b_re, w_e_gate, w_e_up, w_e_down):
    B, T, _ = x.shape
    dt = x.dtype
    pos = pos0 + jnp.arange(T, dtype=jnp.int32)
    xn = _rmsnorm(x, norm_mix)
    proj = xn @ w_in
    qa, ka, va, qh, fh, ih, ogh, ga, gh = jnp.split(proj, _split_points(IN_SIZES), axis=-1)

    att_shape = (B, T, N_ATT_GROUPS, ATT_HEADS, ATT_HEAD_DIM)
    qa = _rope(qa.reshape(att_shape), pos)
    ka = _rope(ka.reshape(att_shape), pos)
    va = va.reshape(att_shape)
    outs, lses, kv_rows = [], [], []
    for g, (window, dil) in enumerate(ATT_GROUPS):
        kg, vg = ka[:, :, g], va[:, :, g]
        kv_rows.append(jnp.stack([kg, vg], axis=2))
        if win_bufs is None:
            k_all, v_all, q_off = kg, vg, 0
        else:
            buf = win_bufs[g]
            k_all = jnp.concatenate([buf[:, :, 0].astype(dt), kg], axis=1)
            v_all = jnp.concatenate([buf[:, :, 1].astype(dt), vg], axis=1)
            q_off = buf.shape[1]
        o, lse = _dilated_attend(qa[:, :, g], k_all, v_all, q_off, window, dil)
        outs.append(o)
        lses.append(lse)
    o = jnp.stack(outs, axis=2)
    wgt = jax.nn.softmax(jnp.stack(lses, axis=2), axis=2)
    attn = jnp.sum(o * wgt[..., None], axis=2).reshape(B, T, ATT_OUT).astype(dt)

    lb_all = jnp.cumsum(jax.nn.softmax(hgrn_lb_logits.astype(jnp.float32), axis=0), axis=0)
    lb = lb_all[layer].reshape(HGRN_HEADS, HGRN_KDIM)
    f = lb + (1.0 - lb) * jax.nn.sigmoid(fh.astype(jnp.float32).reshape(B, T, HGRN_HEADS, HGRN_KDIM))
    qf = jax.nn.silu(qh.astype(jnp.float32)).reshape(B, T, HGRN_HEADS, HGRN_KDIM)
    o_h, S_T = _hgrn2_scan(qf, 1.0 - f, ih.reshape(B, T, HGRN_HEADS, HGRN_VDIM), jnp.log(f), S0)
    o_h = _rmsnorm(o_h, hgrn_norm) * jax.nn.silu(ogh.astype(jnp.float32).reshape(B, T, HGRN_HEADS, HGRN_VDIM))
    hgrn = o_h.reshape(B, T, HGRN_VWIDTH).astype(dt)

    merged = jax.nn.sigmoid(ga) * (attn @ w_proj_attn) + jax.nn.sigmoid(gh) * (hgrn @ w_proj_hgrn)
    x = x + merged @ w_out
    x = x + _cross_attend(_rmsnorm(x, norm_cross), mem_kv, w_cq, w_co)
    x = x + _hier_moe(_rmsnorm(x, norm_ffn), w_rg, b_rg, w_re, b_re, w_e_gate, w_e_up, w_e_down)
    return x, kv_rows, S_T


def setup_inputs(seed: int = 0) -> dict:
    key = jax.random.key(seed)
    ks = jax.random.split(key, 32)
    f32 = jnp.float32

    def nrm(k, shape, scale):
        return jax.random.normal(k, shape, f32) * scale

    def gain(k, shape):
        return 1.0 + 0.01 * jax.random.normal(k, shape, f32)

    D = D_MODEL
    return {
        'x_prompt': nrm(ks[0], (BATCH, SEQ, D), 1.0),
        'x_sample': nrm(ks[1], (DEC_BATCH, DEC_SEQ, D), 1.0),
        'cache_swa1': nrm(ks[2], (DEPTH, DEC_BATCH, min(ATT_GROUPS[0][0], PAST_LEN), 2, ATT_HEADS, ATT_HEAD_DIM), 1.0),
        'cache_swa2': nrm(ks[3], (DEPTH, DEC_BATCH, min(ATT_GROUPS[1][0], PAST_LEN), 2, ATT_HEADS, ATT_HEAD_DIM), 1.0),
        'cache_swa3': nrm(ks[4], (DEPTH, DEC_BATCH, min(ATT_GROUPS[2][0], PAST_LEN), 2, ATT_HEADS, ATT_HEAD_DIM), 1.0),
        'state_hgrn': nrm(ks[5], (DEPTH, DEC_BATCH, HGRN_HEADS, HGRN_KDIM, HGRN_VDIM), 0.3),
        'cache_mem_kv': nrm(ks[6], (DEPTH, DEC_BATCH, MEM_LEN, 2, CROSS_HEADS, CROSS_HEAD_DIM), 1.0),
        'mem_prompt': nrm(ks[7], (BATCH, MEM_LEN, D), 1.0),
        'hgrn_lb_logits': nrm(ks[8], (DEPTH + 1, HGRN_WIDTH), 0.5),
        'norm_mix': gain(ks[9], (DEPTH, D)),
        'w_in': nrm(ks[10], (DEPTH, D, IN_WIDTH), D ** -0.5),
        'w_proj_attn': nrm(ks[11], (DEPTH, ATT_OUT, D), ATT_OUT ** -0.5),
        'w_proj_hgrn': nrm(ks[12], (DEPTH, HGRN_VWIDTH, D), HGRN_VWIDTH ** -0.5),
        'w_out': nrm(ks[13], (DEPTH, D, D), D ** -0.5),
        'hgrn_norm': gain(ks[14], (DEPTH, HGRN_VDIM)),
        'norm_cross': gain(ks[15], (DEPTH, D)),
        'norm_mem': gain(ks[16], (DEPTH, D)),
        'w_cq': nrm(ks[17], (DEPTH, D, CROSS_WIDTH), D ** -0.5),
        'w_ckv': nrm(ks[18], (DEPTH, D, 2 * CROSS_WIDTH), D ** -0.5),
        'w_co': nrm(ks[19], (DEPTH, CROSS_WIDTH, D), CROSS_WIDTH ** -0.5),
        'norm_ffn': gain(ks[20], (DEPTH, D)),
        'w_rg': nrm(ks[21], (DEPTH, D, N_GROUPS), D ** -0.5),
        'b_rg': nrm(ks[22], (DEPTH, N_GROUPS), 0.01),
        'w_re': nrm(ks[23], (DEPTH, D, N_EXPERTS), D ** -0.5),
        'b_re': nrm(ks[24], (DEPTH, N_EXPERTS), 0.01),
        'w_e_gate': nrm(ks[25], (DEPTH, N_GROUPS, EXPERTS_PER_GROUP, D, EXPERT_FF), D ** -0.5),
        'w_e_up': nrm(ks[26], (DEPTH, N_GROUPS, EXPERTS_PER_GROUP, D, EXPERT_FF), D ** -0.5),
        'w_e_down': nrm(ks[27], (DEPTH, N_GROUPS, EXPERTS_PER_GROUP, EXPERT_FF, D), EXPERT_FF ** -0.5),
        'norm_final': gain(ks[28], (D,)),
    }


def reference(x_prompt, x_sample, cache_swa1, cache_swa2, cache_swa3, state_hgrn, cache_mem_kv, mem_prompt,
              hgrn_lb_logits, norm_mix, w_in, w_proj_attn, w_proj_hgrn, w_out, hgrn_norm, norm_cross, norm_mem,
              w_cq, w_ckv, w_co, norm_ffn, w_rg, b_rg, w_re, b_re, w_e_gate, w_e_up, w_e_down, norm_final):
    Bp = x_prompt.shape[0]
    h_p, h_s = x_prompt, x_sample
    swa_p = ([], [], [])
    swa_s = ([], [], [])
    hg_p, hg_s, memkv_p = [], [], []
    for l in range(DEPTH):
        lw = (norm_mix[l], w_in[l], w_proj_attn[l], w_proj_hgrn[l], w_out[l], hgrn_norm[l], norm_cross[l],
              w_cq[l], w_co[l], norm_ffn[l], w_rg[l], b_rg[l], w_re[l], b_re[l], w_e_gate[l], w_e_up[l], w_e_down[l])
        mkv = (_rmsnorm(mem_prompt, norm_mem[l]) @ w_ckv[l]).reshape(Bp, MEM_LEN, 2, CROSS_HEADS, CROSS_HEAD_DIM)
        S0 = jnp.zeros((Bp, HGRN_HEADS, HGRN_KDIM, HGRN_VDIM), jnp.float32)
        h_p, rows_p, S_p = _layer(h_p, 0, None, S0, mkv, l, hgrn_lb_logits, *lw)
        bufs = (cache_swa1[l], cache_swa2[l], cache_swa3[l])
        h_s, rows_s, S_s = _layer(h_s, PAST_LEN, bufs, state_hgrn[l], cache_mem_kv[l], l, hgrn_lb_logits, *lw)
        for g, (window, _) in enumerate(ATT_GROUPS):
            swa_p[g].append(rows_p[g][:, max(0, rows_p[g].shape[1] - window):])
            swa_s[g].append(rows_s[g])
        hg_p.append(S_p.astype(x_prompt.dtype))
        hg_s.append(S_s.astype(x_sample.dtype))
        memkv_p.append(mkv)
    y_prompt = _rmsnorm(h_p, norm_final)
    y_sample = _rmsnorm(h_s, norm_final)
    return (y_prompt, y_sample,
            jnp.stack(swa_p[0]), jnp.stack(swa_p[1]), jnp.stack(swa_p[2]), jnp.stack(hg_p), jnp.stack(memkv_p),
            jnp.stack(swa_s[0]), jnp.stack(swa_s[1]), jnp.stack(swa_s[2]), jnp.stack(hg_s))
```

```python
import numpy as np
from contextlib import ExitStack
import concourse.bass as bass
import concourse.mybir as mybir
from concourse.bass_utils import run_bass_kernel_spmd

F32 = mybir.dt.float32
BF16 = mybir.dt.bfloat16
I32 = mybir.dt.int32
AF = mybir.ActivationFunctionType
ALU = mybir.AluOpType
AX = mybir.AxisListType

SAME_ENGINE_SYNC = True
N_DMA_SEMS = 32

D = 2048
KC = 16
TOWN = 1024
NSMP = 128
TALL = 1152
NHB = 3
N_EXP = 32
QA, KA, VA, QH, FH, IH, OG, GA, GH = 0, 1536, 3072, 4608, 6656, 8704, 10752, 12800, 14848
IN_W = 16896
EPS = 1e-6
PI = float(np.pi)
TWO_PI = float(2 * np.pi)
GROUPS = ((128, 1), (512, 4), (2048, 16))
DEBUG = {}


class Buf:
    __slots__ = ("name", "w", "r")

    def __init__(self, name):
        self.name = name
        self.w = None
        self.r = {}


class K:
    def __init__(self, nc):
        self.nc = nc
        self.eng = {"pe": nc.tensor, "act": nc.scalar, "dve": nc.vector, "pool": nc.gpsimd, "sp": nc.sync}
        self.sem = {}
        self.cnt = {}
        for e in ("pe", "act", "dve", "pool"):
            self.sem[e] = nc.alloc_semaphore(name=f"prog_{e}")
            self.cnt[e] = 0
        self.dsem = [nc.alloc_semaphore(name=f"dma_{i}") for i in range(N_DMA_SEMS)]
        self.dval = [0] * N_DMA_SEMS
        self.dnext = 0
        self.waited = {}
        self.pe_dirty = False
        self.n_inst = 0

    def _wait(self, w, ev, war=False):
        if ev is None:
            return
        kind, key, val = ev
        if kind == "eng":
            if key == w and (w == "pe" or war or not SAME_ENGINE_SYNC):
                return
            if key == "pe" and val > self.cnt["pe"]:
                self.pe_flush()
            sem = self.sem[key]
            wk = (w, "e" + key)
        else:
            sem = self.dsem[key]
            wk = (w, "d%d" % key)
        if self.waited.get(wk, 0) >= val:
            return
        self.eng[w].wait_ge(sem, val)
        self.waited[wk] = val

    def _deps(self, w, reads, writes):
        for b in reads:
            self._wait(w, b.w)
        for b in writes:
            self._wait(w, b.w)
            for ev in b.r.values():
                self._wait(w, ev, war=True)

    def _record(self, ev, reads, writes):
        rk = (ev[0], ev[1])
        for b in reads:
            b.r[rk] = ev
        for b in writes:
            b.w = ev
            b.r = {}

    def op(self, e, fn, reads=(), writes=()):
        self._deps(e, reads, writes)
        inst = fn()
        self.cnt[e] += 1
        inst.then_inc(self.sem[e], 1)
        self._record(("eng", e, self.cnt[e]), reads, writes)
        self.n_inst += 1
        return inst

    def mm(self, out, lhsT, rhs, start=True, stop=True, reads=(), writes=(), inc=None, **kw):
        self._deps("pe", reads, writes)
        inst = self.nc.tensor.matmul(out, lhsT, rhs, start=start, stop=stop, **kw)
        self._pe_after(inst, reads, writes, stop if inc is None else inc)
        return inst

    def tr(self, out, in_, ident, reads=(), writes=(), inc=True):
        self._deps("pe", reads, writes)
        inst = self.nc.tensor.transpose(out, in_, ident)
        self._pe_after(inst, reads, writes, inc)
        return inst

    def _pe_after(self, inst, reads, writes, inc):
        self.n_inst += 1
        nxt = self.cnt["pe"] + 1
        self._record(("eng", "pe", nxt), reads, writes)
        if inc:
            inst.then_inc(self.sem["pe"], 1)
            self.cnt["pe"] = nxt
            self.pe_dirty = False
        else:
            self.pe_dirty = True

    def pe_flush(self):
        if self.pe_dirty:
            inst = self.nc.tensor.nop()
            inst.then_inc(self.sem["pe"], 1)
            self.cnt["pe"] += 1
            self.pe_dirty = False

    def dma(self, q, out, in_, reads=(), writes=(), **kw):
        self._deps(q, reads, writes)
        i = self.dnext
        self.dnext = (i + 1) % N_DMA_SEMS
        if self.dval[i] > 0:
            self._wait(q, ("dma", i, self.dval[i]))
        inst = self.eng[q].dma_start(out=out, in_=in_, **kw)
        self.dval[i] += 16
        inst.then_inc(self.dsem[i], 16)
        ev = ("dma", i, self.dval[i])
        self._record(ev, reads, writes)
        self.n_inst += 1
        return ev

    def barrier(self):
        self.pe_flush()
        for w in ("pe", "act", "dve", "pool", "sp"):
            for p in ("pe", "act", "dve", "pool"):
                if p != w and self.cnt[p] > 0:
                    self._wait(w, ("eng", p, self.cnt[p]))
            for i in range(N_DMA_SEMS):
                if self.dval[i] > 0:
                    self._wait(w, ("dma", i, self.dval[i]))

    def finish(self):
        self.pe_flush()
        for p in ("pe", "act", "dve", "pool"):
            if self.cnt[p] > 0:
                self._wait("sp", ("eng", p, self.cnt[p]))
        for i in range(N_DMA_SEMS):
            if self.dval[i] > 0:
                self._wait("sp", ("dma", i, self.dval[i]))
        done = self.nc.alloc_semaphore(name="prog_done")
        for e in ("pe", "act", "dve", "sp"):
            self.eng[e].nop().then_inc(done, 1)
        self.eng["pool"].wait_ge(done, 4)


class TB:
    __slots__ = ("t", "b")

    def __init__(self, t, name):
        self.t = t
        self.b = Buf(name)

    def __getitem__(self, key):
        return self.t[key]


class _Stop(Exception):
    pass


def build_program(stop_after=None, dbg=None):
    holder = {}
    try:
        return _build_inner(stop_after, dbg, holder)
    except _Stop:
        return holder["nc"]


def _build_inner(stop_after, dbg, holder):
    dbg = dbg or {}
    nc = bass.Bass("TRN2", target_bir_lowering=False)
    holder["nc"] = nc

    def din(name, shape):
        return nc.dram_tensor(name, list(shape), F32, kind="ExternalInput").ap()

    def dout(name, shape):
        return nc.dram_tensor(name, list(shape), F32, kind="ExternalOutput").ap()

    xh = din("xh", [NHB * 1024, D]); xo = din("xo", [TALL, D])
    hval_d = din("hval", [128, 3]); pos0_d = din("pos0", [128, 1])
    c_d = [din("c1", [16, 128, 1024]), din("c2", [16, 512, 1024]), din("c3", [16, 2048, 1024])]
    st_d = din("st", [16, 16, 128, 128]); cm_d = din("cm", [16, 256, 1024]); mp_d = din("mp", [256, D])
    lbl_d = din("lbl", [2, 2048])
    n_mix = din("n_mix", [D]); n_cross = din("n_cross", [D]); n_mem = din("n_mem", [D])
    n_ffn = din("n_ffn", [D]); n_fin = din("n_fin", [D]); hn_d = din("hn", [128])
    w_in = din("w_in", [D, IN_W]); wpa = din("wpa", [512, D]); wph = din("wph", [D, D])
    w_out = din("w_out", [D, D]); w_cq = din("w_cq", [D, 512]); w_ckv = din("w_ckv", [D, 1024])
    w_co = din("w_co", [512, D]); w_r = din("w_r", [D, 36]); b_r = din("b_r", [36])
    weg = din("weg", [N_EXP, D, 512]); weu = din("weu", [N_EXP, D, 512]); wed = din("wed", [N_EXP, 512, D])
    y_d = dout("y", [TALL, D]); kvo = dout("kvo", [3, TALL, 2, 512]); hp_d = dout("hp", [16, 128, 128])
    mkv_d = dout("mkv", [256, 1024]); hs_d = dout("hs", [16, 16, 128, 128])

    k = K(nc)
    V = nc.vector
    A = nc.scalar
    G = nc.gpsimd

    def act(out, in_, func, reads, writes, **kw):
        return k.op("act", lambda: A.activation(out=out, in_=in_, func=func, **kw), reads, writes)

    def ts(e, out, in0, s1, s2, op0, op1, reads, writes):
        eng = V if e == "dve" else G
        if op1 is None:
            return k.op(e, lambda: eng.tensor_scalar(out=out, in0=in0, scalar1=s1, scalar2=None, op0=op0), reads, writes)
        return k.op(e, lambda: eng.tensor_scalar(out=out, in0=in0, scalar1=s1, scalar2=s2, op0=op0, op1=op1), reads, writes)

    def tt(e, out, in0, in1, op, reads, writes):
        eng = V if e == "dve" else G
        return k.op(e, lambda: eng.tensor_tensor(out=out, in0=in0, in1=in1, op=op), reads, writes)

    def stt(out, in0, scalar, in1, op0, op1, reads, writes):
        return k.op("dve", lambda: V.scalar_tensor_tensor(out=out, in0=in0, scalar=scalar, in1=in1, op0=op0, op1=op1), reads, writes)

    def cp(e, out, in_, reads, writes):
        if e == "act":
            return act(out, in_, AF.Copy, reads, writes)
        eng = V if e == "dve" else G
        return k.op(e, lambda: eng.tensor_copy(out=out, in_=in_), reads, writes)

    def mset(e, ap, val, writes):
        eng = V if e == "dve" else G
        return k.op(e, lambda: eng.memset(ap, val), (), writes)

    def asel(out, pattern, cmp_, base, cm, buf, fill=0.0):
        return k.op("pool", lambda: G.affine_select(out=out, in_=out, pattern=pattern, compare_op=cmp_, fill=fill, base=base,
                                                    channel_multiplier=cm), [buf], [buf])

    def rsqrt_cols(dst, src, scale, buf):
        ts("dve", dst, src, scale, EPS, ALU.mult, ALU.add, [buf], [buf])
        act(dst, dst, AF.Ln, [buf], [buf])
        act(dst, dst, AF.Exp, [buf], [buf], scale=-0.5)

    with ExitStack() as top:
        uniq = [0]

        def alloc(stack, name, shape, dt, side="left"):
            uniq[0] += 1
            nm = "sb%d_%s" % (uniq[0], name)
            return TB(stack.enter_context(nc.sbuf_tensor(nm, list(shape), dt, side=side)), nm)

        def checkpoint(name):
            if stop_after == name:
                k.finish()
                raise _Stop()

        def dbg_dump(name, ap, shape, reads):
            if name in dbg:
                o = dout("dbg_" + name, shape)
                k.dma("pool", o, ap, reads=reads)

        pst = [top.enter_context(nc.psum_tensor(f"ps{i}", [128, 1024], F32)) for i in range(4)]
        PB = [Buf(f"psb{i}") for i in range(8)]

        def pbank(i):
            return pst[i // 2][:, (i % 2) * 512:(i % 2 + 1) * 512]

        def pwide(i):
            return pst[i][:, :]

        def pbank_bf(i):
            return pbank(i).bitcast(BF16)

        ident_f = alloc(top, "ident_f", [128, 128], F32)
        ident_b = alloc(top, "ident_b", [128, 128], BF16)
        ones_b = alloc(top, "ones_b", [128, 128], BF16)
        mcur = alloc(top, "mcur", [128, 4, 128], BF16)
        mprev = alloc(top, "mprev", [128, 4, 128], BF16)
        mcur64 = alloc(top, "mcur64", [128, 8, 64], BF16)
        mprev64 = alloc(top, "mprev64", [128, 8, 64], BF16)
        mbd = alloc(top, "mbd", [128, 8, 128], F32)
        rm = alloc(top, "rm", [128, 1024], F32)
        rm8 = alloc(top, "rm8", [128, 128], F32)
        mbd8 = alloc(top, "mbd8", [128, 128], F32)
        sel16 = alloc(top, "sel16", [128, 16], F32)
        small = alloc(top, "small", [128, 64], F32)
        lbc = alloc(top, "lbc", [128, 16, 3], F32)
        hnc = alloc(top, "hnc", [128, 1], F32)
        hval = alloc(top, "hval", [128, 4], F32)
        pos0 = alloc(top, "pos0", [128, 1], F32)
        rs = alloc(top, "rs", [128, 8], F32)

        mset("pool", ident_f[:], 0.0, [ident_f.b])
        asel(ident_f[:], [[-1, 128]], ALU.not_equal, 0, 1, ident_f.b, fill=1.0)
        cp("dve", ident_b[:], ident_f[:], [ident_f.b], [ident_b.b])
        mset("pool", ones_b[:], 1.0, [ones_b.b])
        mset("pool", mcur[:], 1.0, [mcur.b])
        asel(mcur[:], [[0, 4], [1, 128]], ALU.is_ge, 0, -1, mcur.b)
        mset("pool", mprev[:], 1.0, [mprev.b])
        asel(mprev[:], [[0, 4], [-1, 128]], ALU.is_ge, 0, 1, mprev.b)
        mset("pool", mcur64[:], 1.0, [mcur64.b])
        asel(mcur64[:], [[0, 8], [1, 64]], ALU.is_ge, 0, -1, mcur64.b)
        mset("pool", mprev64[:], 1.0, [mprev64.b])
        asel(mprev64[:], [[0, 8], [-1, 64]], ALU.is_ge, 0, 1, mprev64.b)
        mset("pool", mbd[:], 1.0, [mbd.b])
        asel(mbd[:], [[0, 8], [1, 128]], ALU.is_ge, 0, -1, mbd.b)
        mset("pool", mbd[0:64, :, 64:128], 0.0, [mbd.b])
        mset("pool", rm[:], 1.0, [rm.b])
        mset("pool", rm[:].rearrange("p (c t) -> p c t", t=64)[:, :, 0:1], 0.0, [rm.b])
        mset("pool", rm8[:], 1.0, [rm8.b])
        mset("pool", rm8[:].rearrange("p (c t) -> p c t", t=8)[:, :, 0:1], 0.0, [rm8.b])
        mset("pool", mbd8[:], 1.0, [mbd8.b])
        asel(mbd8[:], [[1, 128]], ALU.is_ge, 0, -1, mbd8.b)
        asel(mbd8[:].rearrange("p (b i) -> p b i", i=8), [[-8, 16], [0, 8]], ALU.is_ge, 0, 1, mbd8.b)
        mset("pool", sel16[:], 1.0, [sel16.b])
        asel(sel16[:], [[-8, 16]], ALU.is_ge, 0, 1, sel16.b)
        asel(sel16[:], [[8, 16]], ALU.is_ge, 7, -1, sel16.b)
        mset("pool", small[:], 0.0, [small.b])
        k.dma("sp", hval[:, 0:3], hval_d, writes=[hval.b])
        k.dma("sp", pos0[:], pos0_d, writes=[pos0.b])
        with nc.allow_non_contiguous_dma(reason="tiny param vectors"):
            k.dma("sp", hnc[:], hn_d.rearrange("(p o) -> p o", o=1), writes=[hnc.b])
            k.dma("sp", small[:, 0:16], lbl_d[0, :].rearrange("(h p) -> p h", p=128), writes=[small.b])
            k.dma("sp", small[:, 16:32], lbl_d[1, :].rearrange("(h p) -> p h", p=128), writes=[small.b])
        tt("dve", small[:, 32:48], small[:, 0:16], small[:, 16:32], ALU.subtract, [small.b], [small.b])
        act(lbc[:, :, 0], small[:, 32:48], AF.Sigmoid, [small.b], [lbc.b])
        ts("dve", lbc[:, :, 1], lbc[:, :, 0], -1.0, 1.0, ALU.mult, ALU.add, [lbc.b], [lbc.b])

        xnT = alloc(top, "xnT", [128, KC, TALL], BF16)
        state = {"xt": 0, "ps": 0, "ev": 0, "w": 0}

        def next_bank():
            state["ps"] = (state["ps"] + 1) % 8
            return state["ps"]

        def evac_eng():
            state["ev"] ^= 1
            return "act" if state["ev"] else "dve"

        def load_w(dst_ap, src_cols_ap, buf):
            k.dma("pool", dst_ap, src_cols_ap.rearrange("(kc p) c -> p kc c", p=128), writes=[buf])

        def make_norm(stack, n_xt=2):
            nb = {}
            nb["gb"] = alloc(stack, "gb", [128, D], F32)
            nb["xt"] = [alloc(stack, f"xt{i}", [128, D], F32) for i in range(n_xt)]
            nb["sqj"] = alloc(stack, "sqj", [128, D], BF16)
            nb["i"] = 0
            return nb

        def load_gain(nb, vec_d):
            k.dma("sp", nb["gb"][:], vec_d.partition_broadcast(128), writes=[nb["gb"].b])

        def norm_tile(nb, src_dram=None, src_sb=None, nrows=128):
            x_t = nb["xt"][nb["i"]]; nb["i"] = (nb["i"] + 1) % len(nb["xt"])
            gbt = nb["gb"]; sqj = nb["sqj"]
            if src_dram is not None:
                k.dma("sp", x_t[0:nrows, :], src_dram, writes=[x_t.b])
                xin, xb = x_t[0:nrows, :], x_t.b
            else:
                xin, xb = src_sb
            act(sqj[0:nrows, :], xin, AF.Square, [xb], [sqj.b, rs.b], accum_out=rs[0:nrows, 0:1])
            rsqrt_cols(rs[0:nrows, 1:2], rs[0:nrows, 0:1], 1.0 / D, rs.b)
            stt(x_t[0:nrows, :], xin, rs[0:nrows, 1:2], gbt[0:nrows, :], ALU.mult, ALU.mult, [xb, rs.b, gbt.b], [x_t.b])
            return x_t

        def transpose_tile(x_t, dstT, col0, nrows=128, f32dst=None):
            for q in range(4):
                bi = next_bank()
                for a in range(4):
                    kc = q * 4 + a
                    k.tr(pbank(bi)[:, a * 128:a * 128 + nrows], x_t[0:nrows, kc * 128:(kc + 1) * 128],
                         ident_f[0:nrows, 0:nrows], reads=[x_t.b, ident_f.b], writes=[PB[bi]], inc=(a == 3))
                src = pbank(bi).rearrange("p (a b) -> p a b", b=128)[:, :, 0:nrows]
                if f32dst is None:
                    cp(evac_eng(), dstT[:, q * 4:q * 4 + 4, col0:col0 + nrows], src, [PB[bi]], [dstT.b])
                else:
                    cp("act", f32dst[:, q * 4:q * 4 + 4, 0:nrows], src, [PB[bi]], [f32dst.b])
                    cp("dve", dstT[:, q * 4:q * 4 + 4, col0:col0 + nrows], f32dst[:, q * 4:q * 4 + 4, 0:nrows], [f32dst.b], [dstT.b])

        def build_tables(stack, specs, sample_col=None):
            n = len(specs)
            cosT = alloc(stack, "cosT", [128, n, 64], F32)
            sinT = alloc(stack, "sinT", [128, n, 64], F32)
            with ExitStack() as tmp:
                posi = alloc(tmp, "posi", [128, n], I32)
                posf = alloc(tmp, "posf", [128, n], F32)
                invf = alloc(tmp, "invf", [128, 64], F32)
                invi = alloc(tmp, "invi", [128, 64], I32)
                ang = alloc(tmp, "ang", [128, n, 64], F32)
                t1 = alloc(tmp, "t1", [128, n, 64], F32)
                t2 = alloc(tmp, "t2", [128, n, 64], F32)
                ti = alloc(tmp, "ti", [128, n, 64], I32)
                for ci, (base, cm_) in enumerate(specs):
                    if base is None:
                        k.op("pool", lambda ci=ci: G.iota(posi[:, ci:ci + 1], [[0, 1]], base=0, channel_multiplier=1), (), [posi.b])
                    else:
                        k.op("pool", lambda ci=ci, base=base, cm_=cm_: G.iota(posi[:, ci:ci + 1], [[0, 1]], base=base, channel_multiplier=cm_), (), [posi.b])
                cp("dve", posf[:], posi[:], [posi.b], [posf.b])
                ts("dve", posf[:], posf[:], pos0[:, 0:1], 0.0, ALU.add, ALU.max, [posf.b, pos0.b], [posf.b])
                if sample_col is not None:
                    sc = sample_col
                    k.op("dve", lambda: V.tensor_single_scalar(out=posi[:, sc:sc + 1], in_=posi[:, sc:sc + 1], scalar=7, op=ALU.bitwise_and), [posi.b], [posi.b])
                    cp("dve", posf[:, sc:sc + 1], posi[:, sc:sc + 1], [posi.b], [posf.b])
                    ts("dve", posf[:, sc:sc + 1], posf[:, sc:sc + 1], 2048.0, None, ALU.add, None, [posf.b], [posf.b])
                k.op("pool", lambda: G.iota(invi[:], [[1, 64]], base=0, channel_multiplier=0), (), [invi.b])
                cp("dve", invf[:], invi[:], [invi.b], [invf.b])
                act(invf[:], invf[:], AF.Exp, [invf.b], [invf.b], scale=-float(np.log(10000.0)) / 64.0)
                tt("dve", ang[:], posf[:].unsqueeze(2).to_broadcast([128, n, 64]),
                   invf[:].unsqueeze(1).to_broadcast([128, n, 64]), ALU.mult, [posf.b, invf.b], [ang.b])

                def sin_of(dst, shift):
                    ts("dve", t1[:], ang[:], shift, 1.0 / TWO_PI, ALU.add, ALU.mult, [ang.b], [t1.b])
                    cp("dve", ti[:], t1[:], [t1.b], [ti.b])
                    cp("dve", t2[:], ti[:], [ti.b], [t2.b])
                    ts("dve", t1[:], ang[:], shift, None, ALU.add, None, [ang.b], [t1.b])
                    stt(t1[:], t2[:], -TWO_PI, t1[:], ALU.mult, ALU.add, [t2.b, t1.b], [t1.b])
                    k.op("dve", lambda: V.tensor_single_scalar(out=t2[:], in_=t1[:], scalar=PI, op=ALU.is_gt), [t1.b], [t2.b])
                    stt(t1[:], t2[:], -TWO_PI, t1[:], ALU.mult, ALU.add, [t2.b, t1.b], [t1.b])
                    k.op("dve", lambda: V.tensor_single_scalar(out=t2[:], in_=t1[:], scalar=-PI, op=ALU.is_lt), [t1.b], [t2.b])
                    stt(t1[:], t2[:], TWO_PI, t1[:], ALU.mult, ALU.add, [t2.b, t1.b], [t1.b])
                    ts("dve", t1[:], t1[:], PI, -PI, ALU.min, ALU.max, [t1.b], [t1.b])
                    act(dst[:], t1[:], AF.Sin, [t1.b], [dst.b])

                sin_of(sinT, 0.0)
                sin_of(cosT, PI / 2)
                k.barrier()
            return cosT, sinT

        def rope(dst_ap, src_ps_ap, tabs, tabi, nrows, nh, src_bufs, dst_buf, tmpA, tmpB):
            cosT, sinT = tabs
            s4 = src_ps_ap.rearrange("p (h t f) -> p h t f", h=nh, t=2)
            d4 = dst_ap.rearrange("p (h t f) -> p h t f", h=nh, t=2)
            cosb = cosT[0:nrows, tabi, :].unsqueeze(1).to_broadcast([nrows, nh, 64])
            sinb = sinT[0:nrows, tabi, :].unsqueeze(1).to_broadcast([nrows, nh, 64])
            a3 = tmpA[0:nrows, 0:nh * 64].rearrange("p (h f) -> p h f", h=nh)
            b3 = tmpB[0:nrows, 0:nh * 64].rearrange("p (h f) -> p h f", h=nh)
            x1 = s4[:, :, 0, :]; x2 = s4[:, :, 1, :]
            sb_ = list(src_bufs)
            tt("dve", a3, x1, cosb, ALU.mult, sb_ + [cosT.b], [tmpA.b])
            tt("dve", b3, x2, sinb, ALU.mult, sb_ + [sinT.b], [tmpB.b])
            tt("dve", d4[:, :, 0, :], a3, b3, ALU.subtract, [tmpA.b, tmpB.b], [dst_buf])
            tt("dve", a3, x2, cosb, ALU.mult, sb_ + [cosT.b], [tmpA.b])
            tt("dve", b3, x1, sinb, ALU.mult, sb_ + [sinT.b], [tmpB.b])
            tt("dve", d4[:, :, 1, :], a3, b3, ALU.add, [tmpA.b, tmpB.b], [dst_buf])

        def make_hgrn_bufs(stack):
            hb = {}
            for nm in ("s1", "s2", "s3", "s4"):
                hb[nm] = alloc(stack, "h_" + nm, [128, 1024], F32)
            hb["dec"] = alloc(stack, "h_dec", [128, 16], F32)
            hb["kT"] = alloc(stack, "h_kT", [128, 1024], BF16)
            hb["ktok"] = alloc(stack, "h_ktok", [128, 8, 128], BF16)
            hb["Vb"] = alloc(stack, "h_Vb", [128, 8, 128], BF16)
            hb["Scur"] = [alloc(stack, f"h_Scur{i}", [128, 128], F32) for i in range(2)]
            return hb

        def hgrn_proj(hb, W, fcol, icol):
            for half in range(2):
                for kc in range(KC):
                    k.mm(pbank(half), W[:, kc, fcol:fcol + 128], xnT[:, kc, half * 512:(half + 1) * 512],
                         start=(kc == 0), stop=(kc == KC - 1), reads=[W.b, xnT.b], writes=[PB[half]])
            for tl in range(8):
                bi = 2 + tl // 4
                o = pbank(bi)[:, (tl % 4) * 128:(tl % 4 + 1) * 128]
                for kc in range(KC):
                    k.mm(o, xnT[:, kc, tl * 128:(tl + 1) * 128], W[:, kc, icol:icol + 128],
                         start=(kc == 0), stop=(kc == KC - 1), reads=[W.b, xnT.b], writes=[PB[bi]], inc=(kc == KC - 1 and tl % 4 == 3))

        def hgrn_prep(hb, h):
            s1, s2, s3, s4 = hb["s1"], hb["s2"], hb["s3"], hb["s4"]
            kT, Vb, dec = hb["kT"], hb["Vb"], hb["dec"]
            fps = pwide(0); fb = [PB[0], PB[1]]
            act(s1[:], fps, AF.Sigmoid, fb, [s1.b])
            act(s2[:], fps, AF.Sigmoid, fb, [s2.b], scale=-1.0)
            cp("act", Vb[:], pwide(1).rearrange("p (a b) -> p a b", b=128), [PB[2], PB[3]], [Vb.b])
            ts("dve", s1[:], s1[:], lbc[:, h, 1:2], lbc[:, h, 0:1], ALU.mult, ALU.add, [s1.b, lbc.b], [s1.b])
            act(s1[:], s1[:], AF.Ln, [s1.b], [s1.b])
            k.op("dve", lambda: V.tensor_tensor_scan(out=s3[:], data0=rm[:], data1=s1[:], initial=0.0, op0=ALU.mult, op1=ALU.add),
                 [rm.b, s1.b], [s3.b])
            b3 = s3[:].rearrange("p (c t) -> p c t", t=64)
            act(dec[:, 0:16], b3[:, :, 63], AF.Exp, [s3.b], [dec.b])
            tt("dve", b3, b3, b3[:, :, 63:64].to_broadcast([128, 16, 64]), ALU.subtract, [s3.b], [s3.b])
            act(s4[:], s3[:], AF.Exp, [s3.b], [s4.b], scale=-1.0)
            stt(kT[:], s2[:], lbc[:, h, 1:2], s4[:], ALU.mult, ALU.mult, [s2.b, lbc.b, s4.b], [kT.b])

        def hgrn_tok(hb):
            kT, ktok = hb["kT"], hb["ktok"]
            for tl in range(8):
                k.tr(pbank_bf(4)[:, tl * 128:(tl + 1) * 128], kT[:, tl * 128:(tl + 1) * 128], ident_b[:],
                     reads=[kT.b, ident_b.b], writes=[PB[4]], inc=(tl == 7))
            cp("act", ktok[:], pbank_bf(4).rearrange("p (a b) -> p a b", b=128), [PB[4]], [ktok.b])

        def hgrn_common(hb, S_all, h, W, fcol, icol):
            hgrn_proj(hb, W, fcol, icol)
            hgrn_prep(hb, h)
            hgrn_tok(hb)

        def hgrn_state_chain(hb, S_all, h, on_chunk=None, ubanks=(5,)):
            ktok, Vb, dec, Scur = hb["ktok"], hb["Vb"], hb["dec"], hb["Scur"]
            cur = None
            for c in range(16):
                tl, pr = c // 2, (c % 2) * 64
                if cur is None:
                    sp_ap, sp_b = S_all[:, h, :], S_all.b
                else:
                    sp_ap, sp_b = Scur[cur][:], Scur[cur].b
                if on_chunk is not None:
                    on_chunk(c, sp_ap, sp_b)
                sl_ = c % (4 * len(ubanks))
                ubi = ubanks[sl_ // 4]
                uo = pbank(ubi)[:, (sl_ % 4) * 128:(sl_ % 4 + 1) * 128]
                k.mm(uo, ktok[pr:pr + 64, tl, :], Vb[pr:pr + 64, tl, :], reads=[ktok.b, Vb.b], writes=[PB[ubi]])
                last = (c == 15)
                if last:
                    d_ap, d_b = S_all[:, h, :], S_all.b
                else:
                    nxt = 0 if cur is None else cur ^ 1
                    d_ap, d_b = Scur[nxt][:], Scur[nxt].b
                stt(d_ap, sp_ap, dec[:, c:c + 1], uo, ALU.mult, ALU.add, [sp_b, dec.b, PB[ubi]], [d_b])
                if not last:
                    cur = 0 if cur is None else cur ^ 1

        if True:
            la = ExitStack(); top.enter_context(la)
            attnT = alloc(la, "attnT", [128, 4, TALL], BF16)
            rS1 = ExitStack(); rS2 = ExitStack(); rS3 = ExitStack(); rS4 = ExitStack()
            for st_ in (rS1, rS2, rS3, rS4):
                top.enter_context(st_)
            S_all = alloc(rS1, "S_all", [128, 16, 128], F32, side="right")
            mset("pool", S_all[:], 0.0, [S_all.b])
            QTs = alloc(rS2, "QTs", [128, 12, 128], BF16, side="right")
            KTs = alloc(rS2, "KTs", [128, 12, 128], BF16, side="right")
            Vs = alloc(rS2, "Vs", [128, 12, 128], BF16, side="right")
            KTh = [alloc(rS3, f"KTh{g}", [128, 4, 128 * GROUPS[g][1]], BF16, side="right") for g in range(3)]
            Vh = [alloc(rS3, f"Vh{g}", [128, GROUPS[g][1], 512], BF16, side="right") for g in range(3)]

            with ExitStack() as p1:
                hspecs = [(-128, 1)] + [(-512 + r, 4) for r in range(4)]
                htab = {("h", 0, 0, 2): 0}
                for r in range(4):
                    htab[("h", 1, r, 2)] = 1 + r
                for beta in (1, 2):
                    for r in range(16):
                        htab[("h", 2, r, beta)] = len(hspecs)
                        hspecs.append((-3072 + 1024 * beta + r, 16))
                htabs = build_tables(p1, hspecs)
                nb = make_norm(p1)
                WR = [alloc(p1, f"wr{i}", [128, KC, 256], BF16) for i in range(2)]
                rtA = alloc(p1, "rtA", [128, 128], F32)
                rtB = alloc(p1, "rtB", [128, 128], F32)
                krot = alloc(p1, "krot", [128, 256], BF16)
                vsh = alloc(p1, "vsh", [128, 256], BF16)
                hb = make_hgrn_bufs(p1)

                def wslot():
                    state["w"] ^= 1
                    return WR[state["w"]]

                load_gain(nb, n_mix)
                if "skip_p1" in dbg:
                    for g_ in range(3):
                        mset("pool", KTh[g_][:], 0.0, [KTh[g_].b])
                        mset("pool", Vh[g_][:], 0.0, [Vh[g_].b])
                for beta in (range(NHB) if "skip_p1" not in dbg else ()):
                    for tl in range(8):
                        r0 = beta * 1024 + tl * 128
                        x_t = norm_tile(nb, src_dram=xh[r0:r0 + 128, :])
                        transpose_tile(x_t, xnT, tl * 128)
                    def p1_load(h):
                        W = wslot()
                        load_w(W[:, :, 0:128], w_in[:, FH + h * 128:FH + (h + 1) * 128], W.b)
                        load_w(W[:, :, 128:256], w_in[:, IH + h * 128:IH + (h + 1) * 128], W.b)
                        return W
                    import os
                    UB = tuple(int(v) for v in os.environ.get("P1_UB", "5,6,7").split(","))
                    if os.environ.get("P1_PIPE", "1") == "1":
                        Wn = p1_load(0)
                        hgrn_proj(hb, Wn, 0, 128)
                        for h in range(16):
                            if h + 1 < 16:
                                Wn = p1_load(h + 1)
                            hgrn_prep(hb, h)
                            if h + 1 < 16:
                                hgrn_proj(hb, Wn, 0, 128)
                            hgrn_tok(hb)
                            hgrn_state_chain(hb, S_all, h, ubanks=UB)
                    else:
                        for h in range(16):
                            Wn = p1_load(h)
                            hgrn_common(hb, S_all, h, Wn, 0, 128)
                            hgrn_state_chain(hb, S_all, h, ubanks=UB)
                    for g, (win, dil) in enumerate(GROUPS):
                        first_tok = NHB * 1024 - win
                        lo = max(first_tok, beta * 1024)
                        if lo >= (beta + 1) * 1024:
                            continue
                        loc0 = lo - beta * 1024
                        per_r = (1024 - loc0) // dil
                        n_off = (lo - first_tok) // dil
                        for hf in range(2):
                            Wk = wslot(); load_w(Wk[:, :, :], w_in[:, KA + g * 512 + hf * 256:KA + g * 512 + (hf + 1) * 256], Wk.b)
                            Wv = wslot(); load_w(Wv[:, :, :], w_in[:, VA + g * 512 + hf * 256:VA + g * 512 + (hf + 1) * 256], Wv.b)
                            for r in range(dil):
                                tabi = htab[("h", g, r, beta if g == 2 else 2)]
                                cols = slice(loc0 + r, 1024, dil)
                                bi = next_bank()
                                for kc in range(KC):
                                    k.mm(pbank(bi)[0:per_r, 0:256], xnT[:, kc, cols], Wk[:, kc, :], start=(kc == 0), stop=(kc == KC - 1),
                                         reads=[xnT.b, Wk.b], writes=[PB[bi]])
                                rope(krot[0:per_r, :], pbank(bi)[0:per_r, 0:256], htabs, tabi, per_r, 2, [PB[bi]], krot.b, rtA, rtB)
                                bj = next_bank()
                                for hh in range(2):
                                    k.tr(pbank_bf(bj)[:, hh * 128:hh * 128 + per_r], krot[0:per_r, hh * 128:(hh + 1) * 128],
                                         ident_b[0:per_r, 0:per_r], reads=[krot.b, ident_b.b], writes=[PB[bj]], inc=(hh == 1))
                                dst = KTh[g][:, hf * 2:hf * 2 + 2, r * 128 + n_off:r * 128 + n_off + per_r]
                                cp(evac_eng(), dst, pbank_bf(bj)[:, 0:256].rearrange("p (h n) -> p h n", h=2)[:, :, 0:per_r], [PB[bj]], [KTh[g].b])
                                bv = next_bank()
                                for kc in range(KC):
                                    k.mm(pbank(bv)[0:per_r, 0:256], xnT[:, kc, cols], Wv[:, kc, :], start=(kc == 0), stop=(kc == KC - 1),
                                         reads=[xnT.b, Wv.b], writes=[PB[bv]])
                                vdst = Vh[g][n_off:n_off + per_r, r, hf * 256:(hf + 1) * 256]
                                if n_off == 0:
                                    cp(evac_eng(), vdst, pbank(bv)[0:per_r, 0:256], [PB[bv]], [Vh[g].b])
                                else:
                                    cp(evac_eng(), vsh[0:per_r, :], pbank(bv)[0:per_r, 0:256], [PB[bv]], [vsh.b])
                                    k.dma("sp", vdst, vsh[0:per_r, :], reads=[vsh.b], writes=[Vh[g].b])
                k.barrier()
            if stop_after == "p1":
                dbg_dump("S_all", S_all[:], [128, 16, 128], [S_all.b])
                dbg_dump("KTh2", KTh[2][:], [128, 4, 2048], [KTh[2].b])
                dbg_dump("Vh2", Vh[2][:], [128, 16, 512], [Vh[2].b])
                k.finish()
                return nc

            with ExitStack() as p2n:
                nb = make_norm(p2n)
                load_gain(nb, n_mix)
                for tl in range(9):
                    x_t = norm_tile(nb, src_dram=xo[tl * 128:(tl + 1) * 128, :])
                    transpose_tile(x_t, xnT, tl * 128)
                k.barrier()
            dbg_dump("xnT", xnT[:], [128, KC, TALL], [xnT.b])
            checkpoint("p2n")

            SCALE = float(128 ** -0.5)
            with ExitStack() as at:
                ospecs = []
                otab = {}
                for nb_ in range(8):
                    otab[("o", 0, 0, nb_)] = len(ospecs); ospecs.append((128 * nb_, 1))
                for r in range(4):
                    for nb_ in range(2):
                        otab[("o", 1, r, nb_)] = len(ospecs); ospecs.append((r + 4 * 128 * nb_, 4))
                for r in range(16):
                    otab[("o", 2, r, 0)] = len(ospecs); ospecs.append((r, 16))
                otab[("s",)] = len(ospecs); ospecs.append((None, None))
                otabs = build_tables(at, ospecs, sample_col=otab[("s",)])
                checkpoint("p2a_tab")
                if True:
                    pa = at
                    QT = [alloc(pa, f"QT{g}", [128, 1024], BF16) for g in range(3)]
                    KT = [alloc(pa, f"KT{g}", [128, 1024], BF16) for g in range(3)]
                    Vo = [alloc(pa, f"Vo{g}", [128, 8 if g < 2 else 16, 128], BF16) for g in range(3)]
                    Oacc = alloc(pa, "Oacc", [128, 1024], F32)
                    Dacc = alloc(pa, "Dacc", [128, 1024], F32)
                    Pp = alloc(pa, "Pp", [128, 512], BF16)
                    Pc = alloc(pa, "Pc", [128, 512], BF16)
                    Wqs = [alloc(pa, f"Wq{i}", [128, KC, 384], BF16) for i in range(2)]
                    rtA = alloc(pa, "rtA2", [128, 64], F32)
                    rtB = alloc(pa, "rtB2", [128, 64], F32)
                    krot = alloc(pa, "krot2", [128, 256], BF16)
                    kstage = alloc(pa, "kstage", [128, 128], F32)
                    vstage = alloc(pa, "vstage", [128, 128], F32)
                    wqi = 0
                    for h in (range(4) if "skip_mix" not in dbg else ()):
                        for g, (win, dil) in enumerate(GROUPS):
                            W = Wqs[wqi]; wqi ^= 1
                            c0 = g * 512 + h * 128
                            load_w(W[:, :, 0:128], w_in[:, QA + c0:QA + c0 + 128], W.b)
                            load_w(W[:, :, 128:256], w_in[:, KA + c0:KA + c0 + 128], W.b)
                            load_w(W[:, :, 256:384], w_in[:, VA + c0:VA + c0 + 128], W.b)
                            R = min(128, 1024 // dil)
                            nbk = (1024 // dil) // R
                            tiles = [(r, nb_) for r in range(dil) for nb_ in range(nbk)] + [("s", 0)]
                            for (r, nb_) in tiles:
                                if r == "s":
                                    cols = slice(1024, 1152); nr = 128; tabi = otab[("s",)]
                                else:
                                    t0 = r + dil * nb_ * R
                                    cols = slice(t0, t0 + dil * (R - 1) + 1, dil); nr = R; tabi = otab[("o", g, r, nb_)]
                                bi = next_bank()
                                pq = pbank(bi)
                                for which in range(3):
                                    for kc in range(KC):
                                        k.mm(pq[0:nr, which * 128:(which + 1) * 128], xnT[:, kc, cols], W[:, kc, which * 128:(which + 1) * 128],
                                             start=(kc == 0), stop=(kc == KC - 1), reads=[xnT.b, W.b], writes=[PB[bi]],
                                             inc=(kc == KC - 1 and which == 2))
                                rope(krot[0:nr, 0:128], pq[0:nr, 0:128], otabs, tabi, nr, 1, [PB[bi]], krot.b, rtA, rtB)
                                rope(kstage[0:nr, :], pq[0:nr, 128:256], otabs, tabi, nr, 1, [PB[bi]], kstage.b, rtA, rtB)
                                k.dma("sp", kvo[g, cols, 0, h * 128:(h + 1) * 128], kstage[0:nr, :], reads=[kstage.b])
                                cp("act", krot[0:nr, 128:256], kstage[0:nr, :], [kstage.b], [krot.b])
                                cp("act", vstage[0:nr, :], pq[0:nr, 256:384], [PB[bi]], [vstage.b])
                                k.dma("sp", kvo[g, cols, 1, h * 128:(h + 1) * 128], vstage[0:nr, :], reads=[vstage.b])
                                bj = next_bank()
                                k.tr(pbank_bf(bj)[:, 0:nr], krot[0:nr, 0:128], ident_b[0:nr, 0:nr], reads=[krot.b, ident_b.b], writes=[PB[bj]], inc=False)
                                k.tr(pbank_bf(bj)[:, 128:128 + nr], krot[0:nr, 128:256], ident_b[0:nr, 0:nr], reads=[krot.b, ident_b.b], writes=[PB[bj]])
                                if r == "s":
                                    cp("dve", QTs[:, g * 4 + h, :], pbank_bf(bj)[:, 0:128], [PB[bj]], [QTs.b])
                                    cp("dve", KTs[:, g * 4 + h, :], pbank_bf(bj)[:, 128:256], [PB[bj]], [KTs.b])
                                    cp("act", Vs[:, g * 4 + h, :], pq[:, 256:384], [PB[bi]], [Vs.b])
                                else:
                                    si = r * nbk + nb_
                                    cp("dve", QT[g][:, si * R:(si + 1) * R], pbank_bf(bj)[:, 0:nr], [PB[bj]], [QT[g].b])
                                    cp("dve", KT[g][:, si * R:(si + 1) * R], pbank_bf(bj)[:, 128:128 + nr], [PB[bj]], [KT[g].b])
                                    cp("act", Vo[g][0:nr, si, :], pq[0:nr, 256:384], [PB[bi]], [Vo[g].b])
                            checkpoint("p2a_proj%d%d" % (h, g))
                            nblk = dil * nbk
                            per = 512 // R
                            mprev_t = mprev if R == 128 else mprev64
                            mcur_t = mcur if R == 128 else mcur64
                            for reg in range(nblk // per):
                                blocks = list(range(reg * per, (reg + 1) * per))
                                bp, bc_, bo, bd_ = 0, 1, 2, 3
                                halo_cols = []
                                for ii, si in enumerate(blocks):
                                    r, nb_ = si // nbk, si % nbk
                                    qs = QT[g][:, si * R:(si + 1) * R]
                                    if nb_ == 0:
                                        kprev = KTh[g][:, h, r * 128:(r + 1) * 128]; kb = KTh[g].b
                                        halo_cols.append(ii)
                                    else:
                                        kprev = KT[g][:, (si - 1) * R:si * R]; kb = KT[g].b
                                    k.mm(pbank(bp)[:, ii * R:(ii + 1) * R], kprev, qs, reads=[kb, QT[g].b], writes=[PB[bp]], inc=(ii == per - 1))
                                    k.mm(pbank(bc_)[0:R, ii * R:(ii + 1) * R], KT[g][:, si * R:(si + 1) * R], qs, reads=[KT[g].b, QT[g].b],
                                         writes=[PB[bc_]], inc=(ii == per - 1))
                                act(Pp[:, :], pbank(bp), AF.Exp, [PB[bp]], [Pp.b], scale=SCALE)
                                act(Pc[0:R, :], pbank(bc_)[0:R, :], AF.Exp, [PB[bc_]], [Pc.b], scale=SCALE)
                                tt("dve", Pp[:, :], Pp[:, :], mprev_t[:].rearrange("p a b -> p (a b)"), ALU.mult, [Pp.b, mprev_t.b], [Pp.b])
                                tt("dve", Pc[0:R, :], Pc[0:R, :], mcur_t[0:R].rearrange("p a b -> p (a b)"), ALU.mult, [Pc.b, mcur_t.b], [Pc.b])
                                for ii in halo_cols:
                                    sl = slice(ii * R, (ii + 1) * R)
                                    if g == 2:
                                        ts("dve", Pp[0:64, sl], Pp[0:64, sl], hval[0:64, 1:2], None, ALU.mult, None, [Pp.b, hval.b], [Pp.b])
                                        ts("dve", Pp[64:128, sl], Pp[64:128, sl], hval[64:128, 2:3], None, ALU.mult, None, [Pp.b, hval.b], [Pp.b])
                                    else:
                                        ts("dve", Pp[:, sl], Pp[:, sl], hval[:, 2:3], None, ALU.mult, None, [Pp.b, hval.b], [Pp.b])
                                for ii, si in enumerate(blocks):
                                    r, nb_ = si // nbk, si % nbk
                                    if nb_ == 0:
                                        vprev = Vh[g][:, r, h * 128:(h + 1) * 128]; vb_ = Vh[g].b
                                    else:
                                        vprev = Vo[g][:, si - 1, :]; vb_ = Vo[g].b
                                    sl = slice(ii * R, (ii + 1) * R)
                                    k.mm(pbank(bo)[:, sl], vprev, Pp[:, sl], start=True, stop=False, reads=[vb_, Pp.b], writes=[PB[bo]])
                                    k.mm(pbank(bo)[:, sl], Vo[g][0:R, si, :], Pc[0:R, sl], start=False, stop=True, reads=[Vo[g].b, Pc.b],
                                         writes=[PB[bo]], inc=(ii == per - 1))
                                    k.mm(pbank(bd_)[:, sl], ones_b[:, :], Pp[:, sl], start=True, stop=False, reads=[ones_b.b, Pp.b], writes=[PB[bd_]])
                                    k.mm(pbank(bd_)[:, sl], ones_b[0:R, :], Pc[0:R, sl], start=False, stop=True, reads=[ones_b.b, Pc.b],
                                         writes=[PB[bd_]], inc=(ii == per - 1))
                                for ii, si in enumerate(blocks):
                                    r, nb_ = si // nbk, si % nbk
                                    t0 = r + dil * nb_ * R
                                    dsl = slice(t0, t0 + dil * (R - 1) + 1, dil)
                                    sl = slice(ii * R, (ii + 1) * R)
                                    if g == 0:
                                        cp("dve", Oacc[:, dsl], pbank(bo)[:, sl], [PB[bo]], [Oacc.b])
                                        cp("act", Dacc[:, dsl], pbank(bd_)[:, sl], [PB[bd_]], [Dacc.b])
                                    else:
                                        tt("dve", Oacc[:, dsl], Oacc[:, dsl], pbank(bo)[:, sl], ALU.add, [Oacc.b, PB[bo]], [Oacc.b])
                                        tt("dve", Dacc[:, dsl], Dacc[:, dsl], pbank(bd_)[:, sl], ALU.add, [Dacc.b, PB[bd_]], [Dacc.b])
                        k.op("dve", lambda: V.reciprocal(out=Dacc[:, :], in_=Dacc[:, :]), [Dacc.b], [Dacc.b])
                        tt("dve", attnT[:, h, 0:1024], Oacc[:, :], Dacc[:, :], ALU.mult, [Oacc.b, Dacc.b], [attnT.b])
                        checkpoint("p2a_slot%d" % h)
                    k.barrier()
            rS3.close()
            dbg_dump("attnT_p", attnT[:], [128, 4, TALL], [attnT.b])
            checkpoint("p2a_prompt")
            if True:
                with ExitStack() as sa:
                    smk = alloc(sa, "smk", [128, 13, 4, 8], BF16)
                    smn = alloc(sa, "smn", [128, 3, 128], BF16)
                    mset("pool", smk[:], 1.0, [smk.b])
                    asel(smk[:, 0, :, :], [[0, 4], [-1, 8]], ALU.is_ge, 0, 1, smk.b)
                    for rho in range(4):
                        tI = 1 + rho
                        mset("pool", smk[:, tI, :, :], 0.0, [smk.b])
                        mset("pool", smk[:, tI, :, rho:rho + 1], 1.0, [smk.b])
                        mset("pool", smk[:, tI, :, rho + 4:rho + 5], 1.0, [smk.b])
                        mset("pool", smk[0:1, tI, :, rho + 4:rho + 5], 0.0, [smk.b])
                    for rho in range(8):
                        tI = 5 + rho
                        mset("pool", smk[:, tI, :, :], 0.0, [smk.b])
                        mset("pool", smk[:, tI, :, rho:rho + 1], 1.0, [smk.b])
                    mset("pool", smn[:], 1.0, [smn.b])
                    asel(smn[:], [[0, 3], [1, 128]], ALU.is_ge, 0, -1, smn.b)
                    asel(smn[:].rearrange("p g (b i) -> p g b i", i=8), [[0, 3], [-8, 16], [0, 8]], ALU.is_ge, 0, 1, smn.b)
                    asel(smn[:, 2, :], [[1, 128]], ALU.is_equal, 0, -1, smn.b)
                    for dlt in (1, 2, 3, 5, 6, 7):
                        asel(smn[:, 1, :], [[1, 128]], ALU.not_equal, -dlt, -1, smn.b)
                    checkpoint("sa_masks")
                    ck = [alloc(sa, f"ck{i}", [128, 13, 1024], BF16) for i in range(2)]
                    ckT = alloc(sa, "ckT", [128, 13, 4, 128], BF16)
                    Ps = alloc(sa, "Ps", [128, 13, 4, 8], BF16)
                    Osm = alloc(sa, "Osm", [128, 4, 128], F32)
                    Dsm = alloc(sa, "Dsm", [128, 4, 128], F32)
                    Pn = alloc(sa, "Pn", [128, 128], BF16)
                    for h in (range(4) if "skip_mix" not in dbg else ()):
                        for g in range(3):
                            gh_ = g * 4 + h
                            k.mm(pbank(0)[:, 0:128], KTs[:, gh_, :], QTs[:, gh_, :], reads=[KTs.b, QTs.b], writes=[PB[0]])
                            act(Pn[:, :], pbank(0)[:, 0:128], AF.Exp, [PB[0]], [Pn.b], scale=SCALE)
                            tt("dve", Pn[:, :], Pn[:, :], smn[:, g, :], ALU.mult, [Pn.b, smn.b], [Pn.b])
                            k.mm(pbank(1)[:, 0:128], Vs[:, gh_, :], Pn[:, :], reads=[Vs.b, Pn.b], writes=[PB[1]])
                            k.mm(pbank(2)[:, 0:128], ones_b[:, :], Pn[:, :], reads=[ones_b.b, Pn.b], writes=[PB[2]])
                            if g == 0:
                                cp("dve", Osm[:, h, :], pbank(1)[:, 0:128], [PB[1]], [Osm.b])
                                cp("dve", Dsm[:, h, :], pbank(2)[:, 0:128], [PB[2]], [Dsm.b])
                            else:
                                tt("dve", Osm[:, h, :], Osm[:, h, :], pbank(1)[:, 0:128], ALU.add, [Osm.b, PB[1]], [Osm.b])
                                tt("dve", Dsm[:, h, :], Dsm[:, h, :], pbank(2)[:, 0:128], ALU.add, [Dsm.b, PB[2]], [Dsm.b])
                    checkpoint("sa_new")
                    for b in (range(16) if "skip_mix" not in dbg else ()):
                        if b == 1:
                            checkpoint("sa_b0")
                        C = ck[b % 2]
                        k.dma("pool", C[:, 0, :], c_d[0][b, :, :], writes=[C.b])
                        k.dma("pool", C[:, 1:5, :], c_d[1][b].rearrange("(m r) c -> m r c", r=4), writes=[C.b])
                        k.dma("pool", C[:, 5:13, :], c_d[2][b].rearrange("(m r) c -> m r c", r=16)[:, 0:8, :], writes=[C.b])
                        for tI in range(13):
                            bj = next_bank()
                            for hh in range(4):
                                k.tr(pbank_bf(bj)[:, hh * 128:(hh + 1) * 128], C[:, tI, hh * 128:(hh + 1) * 128], ident_b[:],
                                     reads=[C.b, ident_b.b], writes=[PB[bj]], inc=(hh == 3))
                            cp(evac_eng(), ckT[:, tI, :, :], pbank_bf(bj)[:, 0:512].rearrange("p (h n) -> p h n", h=4), [PB[bj]], [ckT.b])
                        bs = next_bank()
                        for tI in range(13):
                            g = 0 if tI == 0 else (1 if tI < 5 else 2)
                            for hh in range(4):
                                o = pbank(bs)[:, (tI * 4 + hh) * 8:(tI * 4 + hh + 1) * 8]
                                k.mm(o, ckT[:, tI, hh, :], QTs[:, g * 4 + hh, b * 8:(b + 1) * 8], reads=[ckT.b, QTs.b], writes=[PB[bs]],
                                     inc=(tI == 12 and hh == 3))
                        psf = Ps[:].rearrange("p a h i -> p (a h i)")
                        act(psf, pbank(bs)[:, 0:416], AF.Exp, [PB[bs]], [Ps.b], scale=SCALE)
                        tt("dve", psf, psf, smk[:].rearrange("p a h i -> p (a h i)"), ALU.mult, [Ps.b, smk.b], [Ps.b])
                        bo = next_bank()
                        for hh in range(4):
                            for tI in range(13):
                                k.mm(pbank(bo)[:, hh * 8:(hh + 1) * 8], C[:, tI, 512 + hh * 128:512 + (hh + 1) * 128], Ps[:, tI, hh, :],
                                     start=(tI == 0), stop=(tI == 12), reads=[C.b, Ps.b], writes=[PB[bo]], inc=False)
                            for tI in range(13):
                                k.mm(pbank(bo)[:, 32 + hh * 8:32 + (hh + 1) * 8], ones_b[:, :], Ps[:, tI, hh, :],
                                     start=(tI == 0), stop=(tI == 12), reads=[ones_b.b, Ps.b], writes=[PB[bo]], inc=(tI == 12 and hh == 3))
                        tt("dve", Osm[:, :, b * 8:(b + 1) * 8], Osm[:, :, b * 8:(b + 1) * 8], pbank(bo)[:, 0:32].rearrange("p (h i) -> p h i", h=4),
                           ALU.add, [Osm.b, PB[bo]], [Osm.b])
                        tt("dve", Dsm[:, :, b * 8:(b + 1) * 8], Dsm[:, :, b * 8:(b + 1) * 8], pbank(bo)[:, 32:64].rearrange("p (h i) -> p h i", h=4),
                           ALU.add, [Dsm.b, PB[bo]], [Dsm.b])
                    k.op("dve", lambda: V.reciprocal(out=Dsm[:], in_=Dsm[:]), [Dsm.b], [Dsm.b])
                    tt("dve", attnT[:, :, 1024:1152], Osm[:], Dsm[:], ALU.mult, [Osm.b, Dsm.b], [attnT.b])
                    k.barrier()
            rS2.close()
            dbg_dump("attnT", attnT[:], [128, 4, TALL], [attnT.b])
            if stop_after == "p2a":
                k.finish()
                return nc

            lh = ExitStack(); top.enter_context(lh)
            hgT = alloc(lh, "hgT", [128, 16, TALL], BF16)
            with ExitStack() as p2b:
                WR = [alloc(p2b, f"wrb{i}", [128, KC, 512], BF16) for i in range(2)]
                hb = make_hgrn_bufs(p2b)
                qT = alloc(p2b, "qT", [128, 1024], BF16)
                AT = alloc(p2b, "AT", [128, 1024], BF16)
                o2 = alloc(p2b, "o2", [128, 1024], BF16)
                Sdb = [alloc(p2b, f"Sdb{i}", [128, 128], BF16) for i in range(2)]
                S0 = alloc(p2b, "S0", [128, 16, 128], F32)
                Sn = alloc(p2b, "Sn", [128, 16, 128], F32)
                sm = {nm: alloc(p2b, "sm_" + nm, [128, 128], F32) for nm in ("a", "b", "c", "d", "e")}
                smb = {nm: alloc(p2b, "smb_" + nm, [128, 128], BF16) for nm in ("kT", "qT", "ktok", "V", "AT", "Vm", "o2")}
                sdec = alloc(p2b, "sdec", [128, 16], F32)
                s1, s2, s3, s4 = hb["s1"], hb["s2"], hb["s3"], hb["s4"]
                def p2b_load(h):
                    state["w"] ^= 1
                    W_ = WR[state["w"]]
                    load_w(W_[:, :, 0:128], w_in[:, FH + h * 128:FH + (h + 1) * 128], W_.b)
                    load_w(W_[:, :, 128:256], w_in[:, IH + h * 128:IH + (h + 1) * 128], W_.b)
                    load_w(W_[:, :, 256:384], w_in[:, QH + h * 128:QH + (h + 1) * 128], W_.b)
                    load_w(W_[:, :, 384:512], w_in[:, OG + h * 128:OG + (h + 1) * 128], W_.b)
                    return W_
                Wnext = p2b_load(0) if "skip_mix" not in dbg else None
                for h in (range(16) if "skip_mix" not in dbg else ()):
                    W = Wnext
                    k.dma("sp", S0[:], st_d[:, h, :, :].rearrange("b k v -> k b v"), writes=[S0.b])
                    hgrn_common(hb, S_all, h, W, 0, 128)
                    if h + 1 < 16:
                        Wnext = p2b_load(h + 1)
                    for half in range(2):
                        for kc in range(KC):
                            k.mm(pbank(half), W[:, kc, 256:384], xnT[:, kc, half * 512:(half + 1) * 512],
                                 start=(kc == 0), stop=(kc == KC - 1), reads=[W.b, xnT.b], writes=[PB[half]])
                    qb = [PB[0], PB[1]]
                    act(s1[:], pwide(0), AF.Sigmoid, qb, [s1.b])
                    act(s4[:], s3[:], AF.Exp, [s3.b], [s4.b])
                    tt("dve", s1[:], pwide(0), s1[:], ALU.mult, qb + [s1.b], [s1.b])
                    tt("dve", qT[:], s1[:], s4[:], ALU.mult, [s1.b, s4.b], [qT.b])
                    for tl in range(8):
                        bi = 2 + tl // 4
                        k.mm(pbank(bi)[:, (tl % 4) * 128:(tl % 4 + 1) * 128], hb["kT"][:, tl * 128:(tl + 1) * 128], qT[:, tl * 128:(tl + 1) * 128],
                             reads=[hb["kT"].b, qT.b], writes=[PB[bi]], inc=(tl % 4 == 3))
                    tt("dve", AT[:], pwide(1), mbd[:].rearrange("p a b -> p (a b)"), ALU.mult, [PB[2], PB[3], mbd.b], [AT.b])

                    def on_chunk(c, sp_ap, sp_b):
                        tl, pr = c // 2, (c % 2) * 64
                        sd = Sdb[c % 2]
                        ts("dve", sd[:], sp_ap, hb["dec"][:, c:c + 1], None, ALU.mult, None, [sp_b, hb["dec"].b], [sd.b])
                        bi = 6 + c // 8
                        o = pbank(bi)[:, (c % 8) * 64:(c % 8 + 1) * 64]
                        k.mm(o, sd[:], qT[:, c * 64:(c + 1) * 64], start=True, stop=False, reads=[sd.b, qT.b], writes=[PB[bi]])
                        k.mm(o, hb["Vb"][pr:pr + 64, tl, :], AT[pr:pr + 64, c * 64:(c + 1) * 64], start=False, stop=True,
                             reads=[hb["Vb"].b, AT.b], writes=[PB[bi]], inc=(c % 8 == 7))

                    hgrn_state_chain(hb, S_all, h, on_chunk=on_chunk, ubanks=(4, 5))
                    k.dma("sp", hp_d[h], S_all[:, h, :], reads=[S_all.b])
                    ob = [PB[6], PB[7]]
                    act(o2[:], pwide(3), AF.Square, ob, [o2.b])
                    for half in range(2):
                        k.mm(pbank(2 + half), ones_b[:, :], o2[:, half * 512:(half + 1) * 512], reads=[ones_b.b, o2.b], writes=[PB[2 + half]])
                    ts("dve", s2[:], pwide(1), 1.0 / 128, EPS, ALU.mult, ALU.add, [PB[2], PB[3]], [s2.b])
                    act(s2[:], s2[:], AF.Ln, [s2.b], [s2.b])
                    act(s2[:], s2[:], AF.Exp, [s2.b], [s2.b], scale=-0.5)
                    tt("dve", s3[:], pwide(3), s2[:], ALU.mult, ob + [s2.b], [s3.b])
                    for half in range(2):
                        for kc in range(KC):
                            k.mm(pbank(half), W[:, kc, 384:512], xnT[:, kc, half * 512:(half + 1) * 512],
                                 start=(kc == 0), stop=(kc == KC - 1), reads=[W.b, xnT.b], writes=[PB[half]])
                    act(s1[:], pwide(0), AF.Sigmoid, qb, [s1.b])
                    tt("dve", s1[:], pwide(0), s1[:], ALU.mult, qb + [s1.b], [s1.b])
                    stt(hgT[:, h, 0:1024], s3[:], hnc[:, 0:1], s1[:], ALU.mult, ALU.mult, [s3.b, hnc.b, s1.b], [hgT.b])

                    sc_ = slice(1024, 1152)
                    pb5 = pbank(4)
                    for which, c0_ in ((0, 0), (2, 256), (3, 384)):
                        for kc in range(KC):
                            k.mm(pb5[:, which * 128:(which + 1) * 128] if which == 0 else pb5[:, (1 if which == 2 else 3) * 128:(2 if which == 2 else 4) * 128],
                                 W[:, kc, c0_:c0_ + 128], xnT[:, kc, sc_], start=(kc == 0), stop=(kc == KC - 1),
                                 reads=[W.b, xnT.b], writes=[PB[4]], inc=False)
                    for kc in range(KC):
                        k.mm(pb5[:, 256:384], xnT[:, kc, sc_], W[:, kc, 128:256], start=(kc == 0), stop=(kc == KC - 1),
                             reads=[W.b, xnT.b], writes=[PB[4]])
                    fh_ps, qh_ps, v_ps, og_ps = pb5[:, 0:128], pb5[:, 128:256], pb5[:, 256:384], pb5[:, 384:512]
                    a_, b_, c_, d_, e_ = sm["a"], sm["b"], sm["c"], sm["d"], sm["e"]
                    act(a_[:], fh_ps, AF.Sigmoid, [PB[4]], [a_.b])
                    act(b_[:], fh_ps, AF.Sigmoid, [PB[4]], [b_.b], scale=-1.0)
                    cp("act", smb["V"][:], v_ps, [PB[4]], [smb["V"].b])
                    ts("dve", a_[:], a_[:], lbc[:, h, 1:2], lbc[:, h, 0:1], ALU.mult, ALU.add, [a_.b, lbc.b], [a_.b])
                    act(a_[:], a_[:], AF.Ln, [a_.b], [a_.b])
                    k.op("dve", lambda: V.tensor_tensor_scan(out=c_[:], data0=rm8[:], data1=a_[:], initial=0.0, op0=ALU.mult, op1=ALU.add),
                         [rm8.b, a_.b], [c_.b])
                    c3 = c_[:].rearrange("p (c t) -> p c t", t=8)
                    act(sdec[:], c3[:, :, 7], AF.Exp, [c_.b], [sdec.b])
                    tt("dve", c3, c3, c3[:, :, 7:8].to_broadcast([128, 16, 8]), ALU.subtract, [c_.b], [c_.b])
                    act(d_[:], c_[:], AF.Exp, [c_.b], [d_.b], scale=-1.0)
                    stt(smb["kT"][:], b_[:], lbc[:, h, 1:2], d_[:], ALU.mult, ALU.mult, [b_.b, lbc.b, d_.b], [smb["kT"].b])
                    act(a_[:], qh_ps, AF.Sigmoid, [PB[4]], [a_.b])
                    act(d_[:], c_[:], AF.Exp, [c_.b], [d_.b])
                    tt("dve", a_[:], qh_ps, a_[:], ALU.mult, [PB[4], a_.b], [a_.b])
                    tt("dve", smb["qT"][:], a_[:], d_[:], ALU.mult, [a_.b, d_.b], [smb["qT"].b])
                    act(e_[:], og_ps, AF.Sigmoid, [PB[4]], [e_.b])
                    tt("dve", e_[:], og_ps, e_[:], ALU.mult, [PB[4], e_.b], [e_.b])
                    k.tr(pbank_bf(5)[:, 0:128], smb["kT"][:], ident_b[:], reads=[smb["kT"].b, ident_b.b], writes=[PB[5]])
                    cp("act", smb["ktok"][:], pbank_bf(5)[:, 0:128], [PB[5]], [smb["ktok"].b])
                    k.mm(pbank(5)[:, 128:256], smb["kT"][:], smb["qT"][:], reads=[smb["kT"].b, smb["qT"].b], writes=[PB[5]])
                    tt("dve", smb["AT"][:], pbank(5)[:, 128:256], mbd8[:], ALU.mult, [PB[5], mbd8.b], [smb["AT"].b])
                    k.mm(pbank(5)[:, 256:384], smb["V"][:], smb["AT"][:], reads=[smb["V"].b, smb["AT"].b], writes=[PB[5]])
                    cp("act", b_[:], pbank(5)[:, 256:384], [PB[5]], [b_.b])
                    for b in range(16):
                        sd = Sdb[b % 2]
                        ts("dve", sd[:], S0[:, b, :], sdec[:, b:b + 1], None, ALU.mult, None, [S0.b, sdec.b], [sd.b])
                        k.mm(pbank(6)[:, b * 8:(b + 1) * 8], sd[:], smb["qT"][:, b * 8:(b + 1) * 8], reads=[sd.b, smb["qT"].b], writes=[PB[6]])
                        ts("dve", smb["Vm"][:], smb["V"][:], sel16[:, b:b + 1], None, ALU.mult, None, [smb["V"].b, sel16.b], [smb["Vm"].b])
                        uo = pbank(7)[:, (b % 4) * 128:(b % 4 + 1) * 128]
                        k.mm(uo, smb["ktok"][:], smb["Vm"][:], reads=[smb["ktok"].b, smb["Vm"].b], writes=[PB[7]])
                        stt(Sn[:, b, :], S0[:, b, :], sdec[:, b:b + 1], uo, ALU.mult, ALU.add, [S0.b, sdec.b, PB[7]], [Sn.b])
                    k.dma("sp", hs_d[:, h, :, :].rearrange("b k v -> k b v"), Sn[:], reads=[Sn.b])
                    tt("dve", b_[:], b_[:], pbank(6)[:, 0:128], ALU.add, [b_.b, PB[6]], [b_.b])
                    act(smb["o2"][:], b_[:], AF.Square, [b_.b], [smb["o2"].b])
                    k.mm(pbank(5)[:, 384:512], ones_b[:, :], smb["o2"][:], reads=[ones_b.b, smb["o2"].b], writes=[PB[5]])
                    ts("dve", a_[:], pbank(5)[:, 384:512], 1.0 / 128, EPS, ALU.mult, ALU.add, [PB[5]], [a_.b])
                    act(a_[:], a_[:], AF.Ln, [a_.b], [a_.b])
                    act(a_[:], a_[:], AF.Exp, [a_.b], [a_.b], scale=-0.5)
                    tt("dve", b_[:], b_[:], a_[:], ALU.mult, [b_.b, a_.b], [b_.b])
                    stt(hgT[:, h, 1024:1152], b_[:], hnc[:, 0:1], e_[:], ALU.mult, ALU.mult, [b_.b, hnc.b, e_.b], [hgT.b])
                k.barrier()
            rS1.close()
            dbg_dump("hgT", hgT[:], [128, 16, TALL], [hgT.b])
            if stop_after == "p2b":
                k.finish()
                return nc

            mT = alloc(rS4, "mT", [128, 16, TALL], BF16, side="right")
            TB3 = ((0, 512), (512, 1024), (1024, 1152))
            with ExitStack() as p2c:
                WR = [alloc(p2c, f"wrc{i}", [128, KC, 512], BF16) for i in range(2)]
                g1 = alloc(p2c, "g1", [128, 512], F32)
                g2 = alloc(p2c, "g2", [128, 512], F32)
                for i in (range(16) if "skip_mix" not in dbg else ()):
                    state["w"] ^= 1
                    W = WR[state["w"]]
                    cs = slice(i * 128, (i + 1) * 128)
                    load_w(W[:, :, 0:128], w_in[:, GA + i * 128:GA + (i + 1) * 128], W.b)
                    load_w(W[:, :, 128:256], w_in[:, GH + i * 128:GH + (i + 1) * 128], W.b)
                    load_w(W[:, :, 256:384], wph[:, cs], W.b)
                    load_w(W[:, 0:4, 384:512], wpa[:, cs], W.b)
                    for (a0, a1) in TB3:
                        n = a1 - a0
                        ba, bb, bc2, bd2 = next_bank(), next_bank(), next_bank(), next_bank()
                        for kc in range(KC):
                            k.mm(pbank(ba)[:, 0:n], W[:, kc, 0:128], xnT[:, kc, a0:a1], start=(kc == 0), stop=(kc == KC - 1), reads=[W.b, xnT.b], writes=[PB[ba]])
                        for kc in range(KC):
                            k.mm(pbank(bb)[:, 0:n], W[:, kc, 128:256], xnT[:, kc, a0:a1], start=(kc == 0), stop=(kc == KC - 1), reads=[W.b, xnT.b], writes=[PB[bb]])
                        for kc in range(4):
                            k.mm(pbank(bc2)[:, 0:n], W[:, kc, 384:512], attnT[:, kc, a0:a1], start=(kc == 0), stop=(kc == 3), reads=[W.b, attnT.b], writes=[PB[bc2]])
                        for kc in range(KC):
                            k.mm(pbank(bd2)[:, 0:n], W[:, kc, 256:384], hgT[:, kc, a0:a1], start=(kc == 0), stop=(kc == KC - 1), reads=[W.b, hgT.b], writes=[PB[bd2]])
                        act(g1[:, 0:n], pbank(ba)[:, 0:n], AF.Sigmoid, [PB[ba]], [g1.b])
                        act(g2[:, 0:n], pbank(bb)[:, 0:n], AF.Sigmoid, [PB[bb]], [g2.b])
                        tt("dve", g1[:, 0:n], g1[:, 0:n], pbank(bc2)[:, 0:n], ALU.mult, [g1.b, PB[bc2]], [g1.b])
                        tt("dve", g2[:, 0:n], g2[:, 0:n], pbank(bd2)[:, 0:n], ALU.mult, [g2.b, PB[bd2]], [g2.b])
                        tt("dve", mT[:, i, a0:a1], g1[:, 0:n], g2[:, 0:n], ALU.add, [g1.b, g2.b], [mT.b])
                k.barrier()
            lh.close()
            la.close()
        dbg_dump("mT", mT[:], [128, 16, TALL], [mT.b])
        if stop_after == "p2c":
            k.finish()
            return nc

        xres = alloc(top, "xres", [128, 9, D], F32)
        for tl in range(9):
            k.dma("sp", xres[:, tl, :], xo[tl * 128:(tl + 1) * 128, :], writes=[xres.b])
        with ExitStack() as p2d:
            WR = [alloc(p2d, f"wrd{i}", [128, KC, 512], BF16) for i in range(2)]
            for cb in (range(4) if "skip_mix" not in dbg else ()):
                state["w"] ^= 1
                W = WR[state["w"]]
                load_w(W[:, :, :], w_out[:, cb * 512:(cb + 1) * 512], W.b)
                for tl in range(9):
                    bi = next_bank()
                    for kc in range(KC):
                        k.mm(pbank(bi), mT[:, kc, tl * 128:(tl + 1) * 128], W[:, kc, :], start=(kc == 0), stop=(kc == KC - 1),
                             reads=[mT.b, W.b], writes=[PB[bi]])
                    xs = xres[:, tl, cb * 512:(cb + 1) * 512]
                    tt("dve", xs, xs, pbank(bi), ALU.add, [xres.b, PB[bi]], [xres.b])
            k.barrier()
        rS4.close()
        import os
        for _ in range(int(os.environ.get("DUMMY_ACT", "0"))):
            act(rs[:, 4:5], rs[:, 4:5], AF.Copy, [rs.b], [rs.b])
        for _ in range(int(os.environ.get("DUMMY_DVE", "0"))):
            cp("dve", rs[:, 5:6], rs[:, 5:6], [rs.b], [rs.b])
        dbg_dump("x1", xres[:], [128, 9, D], [xres.b])
        if stop_after == "p2d":
            k.finish()
            return nc

        SCALE = float(128 ** -0.5)
        with ExitStack() as p3:
            nb = make_norm(p3, n_xt=1)
            checkpoint("p3_alloc")
            load_gain(nb, n_cross)
            checkpoint("p3_gain")
            import os
            for tl in [int(v) for v in os.environ.get("P3_TLIST", "0,1,2,3,4,5,6,7,8").split(",")]:
                x_t = norm_tile(nb, src_sb=(xres[:, tl, :], xres.b))
                if tl == 0:
                    checkpoint("p3_n0")
                transpose_tile(x_t, xnT, tl * 128)
                if tl == 0:
                    checkpoint("p3_t0")
            checkpoint("p3_norm")
            WR = [alloc(p3, f"wre{i}", [128, KC, 256], BF16) for i in range(2)]

            def wslot3():
                state["w"] ^= 1
                return WR[state["w"]]

            memT = alloc(p3, "memT", [128, KC, 256], BF16)
            KmT = alloc(p3, "KmT", [128, 4, 256], BF16)
            Vm = alloc(p3, "Vm", [128, 2, 512], BF16)
            qcT = alloc(p3, "qcT", [128, 4, TALL], BF16)
            ocT = alloc(p3, "ocT", [128, 4, TALL], BF16)
            mst = alloc(p3, "mst", [128, 256], F32)
            load_gain(nb, n_mem)
            for mt in range(2):
                x_t = norm_tile(nb, src_dram=mp_d[mt * 128:(mt + 1) * 128, :])
                transpose_tile(x_t, memT, mt * 128)
            checkpoint("p3_memn")
            for cb in range(4):
                W = wslot3()
                load_w(W[:, :, :], w_ckv[:, cb * 256:(cb + 1) * 256], W.b)
                for mt in range(2):
                    bi = next_bank()
                    for kc in range(KC):
                        k.mm(pbank(bi)[:, 0:256], memT[:, kc, mt * 128:(mt + 1) * 128], W[:, kc, :], start=(kc == 0), stop=(kc == KC - 1),
                             reads=[memT.b, W.b], writes=[PB[bi]])
                    MSK = os.environ.get("MEMKV_SKIP", "")
                    cp("act", mst[:], pbank(bi)[:, 0:256], [PB[bi]], [mst.b])
                    if "dma" not in MSK:
                        k.dma("sp", mkv_d[mt * 128:(mt + 1) * 128, cb * 256:(cb + 1) * 256], mst[:], reads=[mst.b])
                    if cb >= 2 and "vm" not in MSK:
                        cp("dve", Vm[:, mt, (cb - 2) * 256:(cb - 1) * 256], mst[:], [mst.b], [Vm.b])
                if cb < 2 and "kt" not in MSK:
                    for hh in range(2):
                        bi = next_bank()
                        for kc in range(KC):
                            k.mm(pbank(bi)[:, 0:256], W[:, kc, hh * 128:(hh + 1) * 128], memT[:, kc, :], start=(kc == 0), stop=(kc == KC - 1),
                                 reads=[memT.b, W.b], writes=[PB[bi]])
                        cp("act", KmT[:, cb * 2 + hh, :], pbank(bi)[:, 0:256], [PB[bi]], [KmT.b])
            checkpoint("p3_mem")
            for cb in range(2):
                W = wslot3()
                load_w(W[:, :, :], w_cq[:, cb * 256:(cb + 1) * 256], W.b)
                for hh in range(2):
                    h = cb * 2 + hh
                    for (a0, a1) in TB3:
                        n = a1 - a0
                        bi = next_bank()
                        for kc in range(KC):
                            k.mm(pbank(bi)[:, 0:n], W[:, kc, hh * 128:(hh + 1) * 128], xnT[:, kc, a0:a1], start=(kc == 0), stop=(kc == KC - 1),
                                 reads=[W.b, xnT.b], writes=[PB[bi]])
                        cp(evac_eng(), qcT[:, h, a0:a1], pbank(bi)[:, 0:n], [PB[bi]], [qcT.b])
            checkpoint("p3_q")
            Pm = alloc(p3, "Pm", [128, 2, 512], BF16)
            rec = alloc(p3, "rec", [128, 512], F32)
            for h in range(4):
                for tb in range(2):
                    a0, a1 = tb * 512, (tb + 1) * 512
                    for mt in range(2):
                        k.mm(pbank(mt), KmT[:, h, mt * 128:(mt + 1) * 128], qcT[:, h, a0:a1], reads=[KmT.b, qcT.b], writes=[PB[mt]])
                    act(Pm[:].rearrange("p a b -> p (a b)"), pwide(0), AF.Exp, [PB[0], PB[1]], [Pm.b], scale=SCALE)
                    for mt in range(2):
                        k.mm(pbank(2), Vm[:, mt, h * 128:(h + 1) * 128], Pm[:, mt, :], start=(mt == 0), stop=(mt == 1), reads=[Vm.b, Pm.b], writes=[PB[2]])
                    for mt in range(2):
                        k.mm(pbank(3), ones_b[:, :], Pm[:, mt, :], start=(mt == 0), stop=(mt == 1), reads=[ones_b.b, Pm.b], writes=[PB[3]])
                    k.op("dve", lambda: V.reciprocal(out=rec[:], in_=pbank(3)), [PB[3]], [rec.b])
                    tt("dve", ocT[:, h, a0:a1], pbank(2), rec[:], ALU.mult, [PB[2], rec.b], [ocT.b])
            checkpoint("p3_prompt")
            cmb = [alloc(p3, f"cmb{i}", [128, 2, 1024], BF16) for i in range(2)]
            cKT = alloc(p3, "cKT", [128, 2, 4, 128], BF16)
            Psm = alloc(p3, "Psm", [128, 64], BF16)
            Osc = alloc(p3, "Osc", [128, 4, 128], F32)
            Dsc = alloc(p3, "Dsc", [128, 4, 128], F32)
            for b in range(16):
                C = cmb[b % 2]
                k.dma("pool", C[:], cm_d[b].rearrange("(t m) c -> m t c", m=128), writes=[C.b])
                bj = next_bank()
                for mt in range(2):
                    for hh in range(4):
                        k.tr(pbank_bf(bj)[:, (mt * 4 + hh) * 128:(mt * 4 + hh + 1) * 128], C[:, mt, hh * 128:(hh + 1) * 128], ident_b[:],
                             reads=[C.b, ident_b.b], writes=[PB[bj]], inc=(mt == 1 and hh == 3))
                cp(evac_eng(), cKT[:].rearrange("p a h n -> p (a h n)"), pbank_bf(bj), [PB[bj]], [cKT.b])
                bs = next_bank()
                for hh in range(4):
                    for mt in range(2):
                        k.mm(pbank(bs)[:, (hh * 2 + mt) * 8:(hh * 2 + mt + 1) * 8], cKT[:, mt, hh, :], qcT[:, hh, 1024 + b * 8:1024 + (b + 1) * 8],
                             reads=[cKT.b, qcT.b], writes=[PB[bs]], inc=(hh == 3 and mt == 1))
                act(Psm[:], pbank(bs)[:, 0:64], AF.Exp, [PB[bs]], [Psm.b], scale=SCALE)
                bo = next_bank()
                for hh in range(4):
                    for mt in range(2):
                        k.mm(pbank(bo)[:, hh * 8:(hh + 1) * 8], C[:, mt, 512 + hh * 128:512 + (hh + 1) * 128], Psm[:, (hh * 2 + mt) * 8:(hh * 2 + mt + 1) * 8],
                             start=(mt == 0), stop=(mt == 1), reads=[C.b, Psm.b], writes=[PB[bo]], inc=False)
                    for mt in range(2):
                        k.mm(pbank(bo)[:, 32 + hh * 8:32 + (hh + 1) * 8], ones_b[:, :], Psm[:, (hh * 2 + mt) * 8:(hh * 2 + mt + 1) * 8],
                             start=(mt == 0), stop=(mt == 1), reads=[ones_b.b, Psm.b], writes=[PB[bo]], inc=(hh == 3 and mt == 1))
                cp("dve", Osc[:, :, b * 8:(b + 1) * 8], pbank(bo)[:, 0:32].rearrange("p (h i) -> p h i", h=4), [PB[bo]], [Osc.b])
                cp("dve", Dsc[:, :, b * 8:(b + 1) * 8], pbank(bo)[:, 32:64].rearrange("p (h i) -> p h i", h=4), [PB[bo]], [Dsc.b])
            k.op("dve", lambda: V.reciprocal(out=Dsc[:], in_=Dsc[:]), [Dsc.b], [Dsc.b])
            tt("dve", ocT[:, :, 1024:1152], Osc[:], Dsc[:], ALU.mult, [Osc.b, Dsc.b], [ocT.b])
            checkpoint("p3_sample")
            for cb in range(8):
                W = wslot3()
                load_w(W[:, 0:4, :], w_co[:, cb * 256:(cb + 1) * 256], W.b)
                for tl in range(9):
                    bi = next_bank()
                    for kc in range(4):
                        k.mm(pbank(bi)[:, 0:256], ocT[:, kc, tl * 128:(tl + 1) * 128], W[:, kc, :], start=(kc == 0), stop=(kc == 3),
                             reads=[ocT.b, W.b], writes=[PB[bi]])
                    xs = xres[:, tl, cb * 256:(cb + 1) * 256]
                    tt("dve", xs, xs, pbank(bi)[:, 0:256], ALU.add, [xres.b, PB[bi]], [xres.b])
            k.barrier()
        dbg_dump("x2", xres[:], [128, 9, D], [xres.b])
        if stop_after == "p3":
            k.finish()
            return nc

        cw = alloc(top, "cw", [128, 9, 32], F32)
        with ExitStack() as p4n:
            nb = make_norm(p4n, n_xt=1)
            load_gain(nb, n_ffn)
            x32 = alloc(p4n, "x32", [128, KC, 128], F32)
            wr32 = alloc(p4n, "wr32", [128, KC, 36], F32)
            bb = alloc(p4n, "bb", [128, 36], F32)
            L = alloc(p4n, "L", [128, 36], F32)
            r_ = alloc(p4n, "r_", [128, 64], F32)
            k.dma("sp", wr32[:], w_r.rearrange("(kc p) c -> p kc c", p=128), writes=[wr32.b])
            k.dma("sp", bb[:], b_r.partition_broadcast(128), writes=[bb.b])
            for tl in range(9):
                x_t = norm_tile(nb, src_sb=(xres[:, tl, :], xres.b))
                transpose_tile(x_t, xnT, tl * 128, f32dst=x32)
                bi = next_bank()
                for kc in range(KC):
                    k.mm(pbank(bi)[:, 0:36], x32[:, kc, :], wr32[:, kc, :], start=(kc == 0), stop=(kc == KC - 1), reads=[x32.b, wr32.b], writes=[PB[bi]])
                tt("dve", L[:], pbank(bi)[:, 0:36], bb[:], ALU.add, [PB[bi], bb.b], [L.b])
                R_ = [r_.b]
                k.op("dve", lambda: V.reduce_max(out=r_[:, 0:1], in_=L[:, 0:4], axis=AX.X), [L.b], R_)
                ts("dve", r_[:, 4:8], L[:, 0:4], r_[:, 0:1], None, ALU.subtract, None, [L.b] + R_, R_)
                act(r_[:, 4:8], r_[:, 4:8], AF.Exp, R_, R_, accum_out=r_[:, 1:2])
                k.op("dve", lambda: V.reciprocal(out=r_[:, 2:3], in_=r_[:, 1:2]), R_, R_)
                ts("dve", r_[:, 8:12], L[:, 0:4], r_[:, 0:1], None, ALU.is_ge, None, [L.b] + R_, R_)
                ts("dve", r_[:, 16:24], L[:, 4:12], r_[:, 8:9], None, ALU.mult, None, [L.b] + R_, R_)
                for g in range(1, 4):
                    stt(r_[:, 16:24], L[:, 4 + 8 * g:12 + 8 * g], r_[:, 8 + g:9 + g], r_[:, 16:24], ALU.mult, ALU.add, [L.b] + R_, R_)
                k.op("dve", lambda: V.reduce_max(out=r_[:, 3:4], in_=r_[:, 16:24], axis=AX.X), R_, R_)
                ts("dve", r_[:, 24:32], r_[:, 16:24], r_[:, 3:4], None, ALU.is_ge, None, R_, R_)
                stt(r_[:, 32:40], r_[:, 24:32], -1e30, r_[:, 16:24], ALU.mult, ALU.add, R_, R_)
                k.op("dve", lambda: V.reduce_max(out=r_[:, 12:13], in_=r_[:, 32:40], axis=AX.X), R_, R_)
                ts("dve", r_[:, 40:48], r_[:, 32:40], r_[:, 12:13], None, ALU.is_ge, None, R_, R_)
                tt("dve", r_[:, 13:14], r_[:, 12:13], r_[:, 3:4], ALU.subtract, R_, R_)
                act(r_[:, 13:14], r_[:, 13:14], AF.Exp, R_, R_)
                ts("dve", r_[:, 14:15], r_[:, 13:14], 1.0, None, ALU.add, None, R_, R_)
                k.op("dve", lambda: V.reciprocal(out=r_[:, 14:15], in_=r_[:, 14:15]), R_, R_)
                tt("dve", r_[:, 14:15], r_[:, 14:15], r_[:, 2:3], ALU.mult, R_, R_)
                tt("dve", r_[:, 15:16], r_[:, 14:15], r_[:, 13:14], ALU.mult, R_, R_)
                ts("dve", r_[:, 48:56], r_[:, 24:32], r_[:, 14:15], None, ALU.mult, None, R_, R_)
                stt(r_[:, 48:56], r_[:, 40:48], r_[:, 15:16], r_[:, 48:56], ALU.mult, ALU.add, R_, R_)
                for g in range(4):
                    ts("dve", cw[:, tl, g * 8:(g + 1) * 8], r_[:, 48:56], r_[:, 8 + g:9 + g], None, ALU.mult, None, R_, [cw.b])
            k.barrier()
        dbg_dump("cw", cw[:], [128, 9, 32], [cw.b])
        with ExitStack() as p4:
            GU = [alloc(p4, f"gu{i}", [128, KC, 256], BF16) for i in range(3)]
            WD = [alloc(p4, f"wd{i}", [128, 4, D], BF16) for i in range(2)]
            hT = alloc(p4, "hT", [128, 4, TALL], BF16)
            sg = [alloc(p4, f"sg{i}", [128, 512], F32) for i in range(2)]
            gi = 0
            for e in range(N_EXP):
                Wd = WD[e % 2]
                for fb in range(4):
                    W = GU[gi % 3]; gi += 1
                    load_w(W[:, :, 0:128], weg[e, :, fb * 128:(fb + 1) * 128], W.b)
                    load_w(W[:, :, 128:256], weu[e, :, fb * 128:(fb + 1) * 128], W.b)
                    if fb == 1:
                        k.dma("pool", Wd[:], wed[e].rearrange("(kc p) c -> p kc c", p=128), writes=[Wd.b])
                    for ti_, (a0, a1) in enumerate(TB3):
                        n = a1 - a0
                        ba, bb2 = next_bank(), next_bank()
                        for kc in range(KC):
                            k.mm(pbank(ba)[:, 0:n], W[:, kc, 0:128], xnT[:, kc, a0:a1], start=(kc == 0), stop=(kc == KC - 1), reads=[W.b, xnT.b], writes=[PB[ba]])
                        for kc in range(KC):
                            k.mm(pbank(bb2)[:, 0:n], W[:, kc, 128:256], xnT[:, kc, a0:a1], start=(kc == 0), stop=(kc == KC - 1), reads=[W.b, xnT.b], writes=[PB[bb2]])
                        s_ = sg[ti_ % 2]
                        act(s_[:, 0:n], pbank(ba)[:, 0:n], AF.Silu, [PB[ba]], [s_.b])
                        tt("dve", hT[:, fb, a0:a1], s_[:, 0:n], pbank(bb2)[:, 0:n], ALU.mult, [s_.b, PB[bb2]], [hT.b])
                for tl in range(9):
                    for cb in range(4):
                        bi = next_bank()
                        for fb in range(4):
                            k.mm(pbank(bi), hT[:, fb, tl * 128:(tl + 1) * 128], Wd[:, fb, cb * 512:(cb + 1) * 512], start=(fb == 0), stop=(fb == 3),
                                 reads=[hT.b, Wd.b], writes=[PB[bi]])
                        xs = xres[:, tl, cb * 512:(cb + 1) * 512]
                        stt(xs, pbank(bi), cw[:, tl, e:e + 1], xs, ALU.mult, ALU.add, [PB[bi], cw.b, xres.b], [xres.b])
            k.barrier()
        dbg_dump("x3", xres[:], [128, 9, D], [xres.b])

        with ExitStack() as p5:
            nb = make_norm(p5, n_xt=2)
            load_gain(nb, n_fin)
            for tl in range(9):
                x_t = norm_tile(nb, src_sb=(xres[:, tl, :], xres.b))
                k.dma("sp", y_d[tl * 128:(tl + 1) * 128, :], x_t[:, :], reads=[x_t.b])
            k.finish()
    return nc


_CACHE = {}


def make_core_inputs(c, inp):
    s, j = c // 4, c % 4
    f32 = np.float32
    xp = inp["x_prompt"]
    xh = np.zeros((NHB * 1024, D), f32)
    lo = 1024 * j - NHB * 1024
    for beta in range(NHB):
        t0 = lo + beta * 1024
        if t0 >= 0:
            xh[beta * 1024:(beta + 1) * 1024] = xp[s, t0:t0 + 1024]
    xo = np.concatenate([xp[s, 1024 * j:1024 * (j + 1)], inp["x_sample"][16 * c:16 * (c + 1)].reshape(128, D)], axis=0)
    hval = np.zeros((128, 3), f32)
    for beta in range(NHB):
        hval[:, beta] = 1.0 if (lo + beta * 1024) >= 0 else 0.0
    pos0 = np.full((128, 1), 1024.0 * j, f32)
    sl = slice(16 * c, 16 * (c + 1))
    m = {
        "xh": xh, "xo": np.ascontiguousarray(xo), "hval": hval, "pos0": pos0,
        "c1": np.ascontiguousarray(inp["cache_swa1"][0, sl].reshape(16, 128, 1024)),
        "c2": np.ascontiguousarray(inp["cache_swa2"][0, sl].reshape(16, 512, 1024)),
        "c3": np.ascontiguousarray(inp["cache_swa3"][0, sl].reshape(16, 2048, 1024)),
        "st": np.ascontiguousarray(inp["state_hgrn"][0, sl]),
        "cm": np.ascontiguousarray(inp["cache_mem_kv"][0, sl].reshape(16, 256, 1024)),
        "mp": np.ascontiguousarray(inp["mem_prompt"][s]),
    }
    return m


def shared_inputs(inp):
    f32 = np.float32
    g = lambda n: np.ascontiguousarray(np.asarray(inp[n], f32))
    return {
        "lbl": g("hgrn_lb_logits"),
        "n_mix": g("norm_mix")[0], "n_cross": g("norm_cross")[0], "n_mem": g("norm_mem")[0],
        "n_ffn": g("norm_ffn")[0], "n_fin": g("norm_final"), "hn": g("hgrn_norm")[0],
        "w_in": g("w_in")[0], "wpa": g("w_proj_attn")[0], "wph": g("w_proj_hgrn")[0], "w_out": g("w_out")[0],
        "w_cq": g("w_cq")[0], "w_ckv": g("w_ckv")[0], "w_co": g("w_co")[0],
        "w_r": np.ascontiguousarray(np.concatenate([g("w_rg")[0], g("w_re")[0]], axis=1)),
        "b_r": np.ascontiguousarray(np.concatenate([g("b_rg")[0], g("b_re")[0]], axis=0)),
        "weg": g("w_e_gate")[0].reshape(N_EXP, D, 512), "weu": g("w_e_up")[0].reshape(N_EXP, D, 512),
        "wed": g("w_e_down")[0].reshape(N_EXP, 512, D),
    }


def assemble(res):
    f32 = np.float32
    y_p = np.zeros((2, 4096, D), f32); y_s = np.zeros((128, 8, D), f32)
    swa_p = [np.zeros((1, 2, w, 2, 4, 128), f32) for w in (128, 512, 2048)]
    swa_s = [np.zeros((1, 128, 8, 2, 4, 128), f32) for _ in range(3)]
    hg_p = np.zeros((1, 2, 16, 128, 128), f32); hg_s = np.zeros((1, 128, 16, 128, 128), f32)
    mkv = np.zeros((1, 2, 256, 2, 4, 128), f32)
    for c in range(8):
        r = res[c]
        s, j = c // 4, c % 4
        y_p[s, 1024 * j:1024 * (j + 1)] = r["y"][0:1024]
        y_s[16 * c:16 * (c + 1)] = r["y"][1024:].reshape(16, 8, D)
        kv = r["kvo"]
        for g in range(3):
            swa_s[g][0, 16 * c:16 * (c + 1)] = kv[g, 1024:1152].reshape(16, 8, 2, 4, 128)
        if j == 3:
            swa_p[0][0, s] = kv[0, 896:1024].reshape(128, 2, 4, 128)
            swa_p[1][0, s] = kv[1, 512:1024].reshape(512, 2, 4, 128)
            swa_p[2][0, s, 1024:2048] = kv[2, 0:1024].reshape(1024, 2, 4, 128)
            hg_p[0, s] = r["hp"]
        if j == 2:
            swa_p[2][0, s, 0:1024] = kv[2, 0:1024].reshape(1024, 2, 4, 128)
        if j == 0:
            mkv[0, s] = r["mkv"].reshape(256, 2, 4, 128)
        hg_s[0, 16 * c:16 * (c + 1)] = r["hs"]
    return (y_p, y_s, swa_p[0], swa_p[1], swa_p[2], hg_p, mkv, swa_s[0], swa_s[1], swa_s[2], hg_s)


def kernel(**inputs):
    inp = {k_: np.asarray(v) for k_, v in inputs.items()}
    if "nc" not in _CACHE:
        _CACHE["nc"] = build_program()
    nc = _CACHE["nc"]
    shared = shared_inputs(inp)
    in_maps = []
    for c in range(8):
        m = make_core_inputs(c, inp)
        m.update(shared)
        in_maps.append(m)
    res = run_bass_kernel_spmd(nc, in_maps, core_ids=list(range(8)))
    return assemble(res.results)
```

```python
import numpy as np
from contextlib import ExitStack
import concourse.bass as bass
import concourse.mybir as mybir
from concourse.bass_utils import run_bass_kernel_spmd

F32 = mybir.dt.float32
BF16 = mybir.dt.bfloat16
I32 = mybir.dt.int32
AF = mybir.ActivationFunctionType
ALU = mybir.AluOpType
AX = mybir.AxisListType

SAME_ENGINE_SYNC = True
N_DMA_SEMS = 32

D = 2048
KC = 16
TOWN = 1024
NSMP = 128
TALL = 1152
NHB = 3
N_EXP = 32
QA, KA, VA, QH, FH, IH, OG, GA, GH = 0, 1536, 3072, 4608, 6656, 8704, 10752, 12800, 14848
IN_W = 16896
EPS = 1e-6
PI = float(np.pi)
TWO_PI = float(2 * np.pi)
GROUPS = ((128, 1), (512, 4), (2048, 16))
DEBUG = {}


class Buf:
    __slots__ = ("name", "w", "r")

    def __init__(self, name):
        self.name = name
        self.w = None
        self.r = {}


class K:
    def __init__(self, nc):
        self.nc = nc
        self.eng = {"pe": nc.tensor, "act": nc.scalar, "dve": nc.vector, "pool": nc.gpsimd, "sp": nc.sync}
        self.sem = {}
        self.cnt = {}
        for e in ("pe", "act", "dve", "pool"):
            self.sem[e] = nc.alloc_semaphore(name=f"prog_{e}")
            self.cnt[e] = 0
        self.dsem = [nc.alloc_semaphore(name=f"dma_{i}") for i in range(N_DMA_SEMS)]
        self.dval = [0] * N_DMA_SEMS
        self.dnext = 0
        self.waited = {}
        self.pe_dirty = False
        self.n_inst = 0

    def _wait(self, w, ev, war=False):
        if ev is None:
            return
        kind, key, val = ev
        if kind == "eng":
            if key == w and (w == "pe" or war or not SAME_ENGINE_SYNC):
                return
            if key == "pe" and val > self.cnt["pe"]:
                self.pe_flush()
            sem = self.sem[key]
            wk = (w, "e" + key)
        else:
            sem = self.dsem[key]
            wk = (w, "d%d" % key)
        if self.waited.get(wk, 0) >= val:
            return
        self.eng[w].wait_ge(sem, val)
        self.waited[wk] = val

    def _deps(self, w, reads, writes):
        for b in reads:
            self._wait(w, b.w)
        for b in writes:
            self._wait(w, b.w)
            for ev in b.r.values():
                self._wait(w, ev, war=True)

    def _record(self, ev, reads, writes):
        rk = (ev[0], ev[1])
        for b in reads:
            b.r[rk] = ev
        for b in writes:
            b.w = ev
            b.r = {}

    def op(self, e, fn, reads=(), writes=()):
        self._deps(e, reads, writes)
        inst = fn()
        self.cnt[e] += 1
        inst.then_inc(self.sem[e], 1)
        self._record(("eng", e, self.cnt[e]), reads, writes)
        self.n_inst += 1
        return inst

    def mm(self, out, lhsT, rhs, start=True, stop=True, reads=(), writes=(), inc=None, **kw):
        self._deps("pe", reads, writes)
        inst = self.nc.tensor.matmul(out, lhsT, rhs, start=start, stop=stop, **kw)
        self._pe_after(inst, reads, writes, stop if inc is None else inc)
        return inst

    def tr(self, out, in_, ident, reads=(), writes=(), inc=True):
        self._deps("pe", reads, writes)
        inst = self.nc.tensor.transpose(out, in_, ident)
        self._pe_after(inst, reads, writes, inc)
        return inst

    def _pe_after(self, inst, reads, writes, inc):
        self.n_inst += 1
        nxt = self.cnt["pe"] + 1
        self._record(("eng", "pe", nxt), reads, writes)
        if inc:
            inst.then_inc(self.sem["pe"], 1)
            self.cnt["pe"] = nxt
            self.pe_dirty = False
        else:
            self.pe_dirty = True

    def pe_flush(self):
        if self.pe_dirty:
            inst = self.nc.tensor.nop()
            inst.then_inc(self.sem["pe"], 1)
            self.cnt["pe"] += 1
            self.pe_dirty = False

    def dma(self, q, out, in_, reads=(), writes=(), **kw):
        self._deps(q, reads, writes)
        i = self.dnext
        self.dnext = (i + 1) % N_DMA_SEMS
        if self.dval[i] > 0:
            self._wait(q, ("dma", i, self.dval[i]))
        inst = self.eng[q].dma_start(out=out, in_=in_, **kw)
        self.dval[i] += 16
        inst.then_inc(self.dsem[i], 16)
        ev = ("dma", i, self.dval[i])
        self._record(ev, reads, writes)
        self.n_inst += 1
        return ev

    def barrier(self):
        self.pe_flush()
        for w in ("pe", "act", "dve", "pool", "sp"):
            for p in ("pe", "act", "dve", "pool"):
                if p != w and self.cnt[p] > 0:
                    self._wait(w, ("eng", p, self.cnt[p]))
            for i in range(N_DMA_SEMS):
                if self.dval[i] > 0:
                    self._wait(w, ("dma", i, self.dval[i]))

    def finish(self):
        self.pe_flush()
        for p in ("pe", "act", "dve", "pool"):
            if self.cnt[p] > 0:
                self._wait("sp", ("eng", p, self.cnt[p]))
        for i in range(N_DMA_SEMS):
            if self.dval[i] > 0:
                self._wait("sp", ("dma", i, self.dval[i]))
        done = self.nc.alloc_semaphore(name="prog_done")
        for e in ("pe", "act", "dve", "sp"):
            self.eng[e].nop().then_inc(done, 1)
        self.eng["pool"].wait_ge(done, 4)


class TB:
    __slots__ = ("t", "b")

    def __init__(self, t, name):
        self.t = t
        self.b = Buf(name)

    def __getitem__(self, key):
        return self.t[key]


class _Stop(Exception):
    pass


def build_program(stop_after=None, dbg=None):
    holder = {}
    try:
        return _build_inner(stop_after, dbg, holder)
    except _Stop:
        return holder["nc"]


def _build_inner(stop_after, dbg, holder):
    dbg = dbg or {}
    nc = bass.Bass("TRN2", target_bir_lowering=False)
    holder["nc"] = nc

    def din(name, shape):
        return nc.dram_tensor(name, list(shape), F32, kind="ExternalInput").ap()

    def dout(name, shape):
        return nc.dram_tensor(name, list(shape), F32, kind="ExternalOutput").ap()

    xh = din("xh", [NHB * 1024, D]); xo = din("xo", [TALL, D])
    hval_d = din("hval", [128, 3]); pos0_d = din("pos0", [128, 1])
    c_d = [din("c1", [16, 128, 1024]), din("c2", [16, 512, 1024]), din("c3", [16, 2048, 1024])]
    st_d = din("st", [16, 16, 128, 128]); cm_d = din("cm", [16, 256, 1024]); mp_d = din("mp", [256, D])
    lbl_d = din("lbl", [2, 2048])
    n_mix = din("n_mix", [D]); n_cross = din("n_cross", [D]); n_mem = din("n_mem", [D])
    n_ffn = din("n_ffn", [D]); n_fin = din("n_fin", [D]); hn_d = din("hn", [128])
    w_in = din("w_in", [D, IN_W]); wpa = din("wpa", [512, D]); wph = din("wph", [D, D])
    w_out = din("w_out", [D, D]); w_cq = din("w_cq", [D, 512]); w_ckv = din("w_ckv", [D, 1024])
    w_co = din("w_co", [512, D]); w_r = din("w_r", [D, 36]); b_r = din("b_r", [36])
    weg = din("weg", [N_EXP, D, 512]); weu = din("weu", [N_EXP, D, 512]); wed = din("wed", [N_EXP, 512, D])
    y_d = dout("y", [TALL, D]); kvo = dout("kvo", [3, TALL, 2, 512]); hp_d = dout("hp", [16, 128, 128])
    mkv_d = dout("mkv", [256, 1024]); hs_d = dout("hs", [16, 16, 128, 128])

    k = K(nc)
    V = nc.vector
    A = nc.scalar
    G = nc.gpsimd

    def act(out, in_, func, reads, writes, **kw):
        return k.op("act", lambda: A.activation(out=out, in_=in_, func=func, **kw), reads, writes)

    def ts(e, out, in0, s1, s2, op0, op1, reads, writes):
        eng = V if e == "dve" else G
        if op1 is None:
            return k.op(e, lambda: eng.tensor_scalar(out=out, in0=in0, scalar1=s1, scalar2=None, op0=op0), reads, writes)
        return k.op(e, lambda: eng.tensor_scalar(out=out, in0=in0, scalar1=s1, scalar2=s2, op0=op0, op1=op1), reads, writes)

    def tt(e, out, in0, in1, op, reads, writes):
        eng = V if e == "dve" else G
        return k.op(e, lambda: eng.tensor_tensor(out=out, in0=in0, in1=in1, op=op), reads, writes)

    def stt(out, in0, scalar, in1, op0, op1, reads, writes):
        return k.op("dve", lambda: V.scalar_tensor_tensor(out=out, in0=in0, scalar=scalar, in1=in1, op0=op0, op1=op1), reads, writes)

    def cp(e, out, in_, reads, writes):
        if e == "act":
            return act(out, in_, AF.Copy, reads, writes)
        eng = V if e == "dve" else G
        return k.op(e, lambda: eng.tensor_copy(out=out, in_=in_), reads, writes)

    def mset(e, ap, val, writes):
        eng = V if e == "dve" else G
        return k.op(e, lambda: eng.memset(ap, val), (), writes)

    def asel(out, pattern, cmp_, base, cm, buf, fill=0.0):
        return k.op("pool", lambda: G.affine_select(out=out, in_=out, pattern=pattern, compare_op=cmp_, fill=fill, base=base,
                                                    channel_multiplier=cm), [buf], [buf])

    def rsqrt_cols(dst, src, scale, buf):
        ts("dve", dst, src, scale, EPS, ALU.mult, ALU.add, [buf], [buf])
        act(dst, dst, AF.Ln, [buf], [buf])
        act(dst, dst, AF.Exp, [buf], [buf], scale=-0.5)

    with ExitStack() as top:
        uniq = [0]

        def alloc(stack, name, shape, dt, side="left"):
            uniq[0] += 1
            nm = "sb%d_%s" % (uniq[0], name)
            return TB(stack.enter_context(nc.sbuf_tensor(nm, list(shape), dt, side=side)), nm)

        def checkpoint(name):
            if stop_after == name:
                k.finish()
                raise _Stop()

        def dbg_dump(name, ap, shape, reads):
            if name in dbg:
                o = dout("dbg_" + name, shape)
                k.dma("pool", o, ap, reads=reads)

        pst = [top.enter_context(nc.psum_tensor(f"ps{i}", [128, 1024], F32)) for i in range(4)]
        PB = [Buf(f"psb{i}") for i in range(8)]

        def pbank(i):
            return pst[i // 2][:, (i % 2) * 512:(i % 2 + 1) * 512]

        def pwide(i):
            return pst[i][:, :]

        def pbank_bf(i):
            return pbank(i).bitcast(BF16)

        ident_f = alloc(top, "ident_f", [128, 128], F32)
        ident_b = alloc(top, "ident_b", [128, 128], BF16)
        ones_b = alloc(top, "ones_b", [128, 128], BF16)
        mcur = alloc(top, "mcur", [128, 4, 128], BF16)
        mprev = alloc(top, "mprev", [128, 4, 128], BF16)
        mcur64 = alloc(top, "mcur64", [128, 8, 64], BF16)
        mprev64 = alloc(top, "mprev64", [128, 8, 64], BF16)
        mbd = alloc(top, "mbd", [128, 8, 128], F32)
        rm = alloc(top, "rm", [128, 1024], F32)
        rm8 = alloc(top, "rm8", [128, 128], F32)
        mbd8 = alloc(top, "mbd8", [128, 128], F32)
        sel16 = alloc(top, "sel16", [128, 16], F32)
        small = alloc(top, "small", [128, 64], F32)
        lbc = alloc(top, "lbc", [128, 16, 3], F32)
        hnc = alloc(top, "hnc", [128, 1], F32)
        hval = alloc(top, "hval", [128, 4], F32)
        pos0 = alloc(top, "pos0", [128, 1], F32)
        rs = alloc(top, "rs", [128, 8], F32)

        mset("pool", ident_f[:], 0.0, [ident_f.b])
        asel(ident_f[:], [[-1, 128]], ALU.not_equal, 0, 1, ident_f.b, fill=1.0)
        cp("dve", ident_b[:], ident_f[:], [ident_f.b], [ident_b.b])
        mset("pool", ones_b[:], 1.0, [ones_b.b])
        mset("pool", mcur[:], 1.0, [mcur.b])
        asel(mcur[:], [[0, 4], [1, 128]], ALU.is_ge, 0, -1, mcur.b)
        mset("pool", mprev[:], 1.0, [mprev.b])
        asel(mprev[:], [[0, 4], [-1, 128]], ALU.is_ge, 0, 1, mprev.b)
        mset("pool", mcur64[:], 1.0, [mcur64.b])
        asel(mcur64[:], [[0, 8], [1, 64]], ALU.is_ge, 0, -1, mcur64.b)
        mset("pool", mprev64[:], 1.0, [mprev64.b])
        asel(mprev64[:], [[0, 8], [-1, 64]], ALU.is_ge, 0, 1, mprev64.b)
        mset("pool", mbd[:], 1.0, [mbd.b])
        asel(mbd[:], [[0, 8], [1, 128]], ALU.is_ge, 0, -1, mbd.b)
        mset("pool", mbd[0:64, :, 64:128], 0.0, [mbd.b])
        mset("pool", rm[:], 1.0, [rm.b])
        mset("pool", rm[:].rearrange("p (c t) -> p c t", t=64)[:, :, 0:1], 0.0, [rm.b])
        mset("pool", rm8[:], 1.0, [rm8.b])
        mset("pool", rm8[:].rearrange("p (c t) -> p c t", t=8)[:, :, 0:1], 0.0, [rm8.b])
        mset("pool", mbd8[:], 1.0, [mbd8.b])
        asel(mbd8[:], [[1, 128]], ALU.is_ge, 0, -1, mbd8.b)
        asel(mbd8[:].rearrange("p (b i) -> p b i", i=8), [[-8, 16], [0, 8]], ALU.is_ge, 0, 1, mbd8.b)
        mset("pool", sel16[:], 1.0, [sel16.b])
        asel(sel16[:], [[-8, 16]], ALU.is_ge, 0, 1, sel16.b)
        asel(sel16[:], [[8, 16]], ALU.is_ge, 7, -1, sel16.b)
        mset("pool", small[:], 0.0, [small.b])
        k.dma("sp", hval[:, 0:3], hval_d, writes=[hval.b])
        k.dma("sp", pos0[:], pos0_d, writes=[pos0.b])
        with nc.allow_non_contiguous_dma(reason="tiny param vectors"):
            k.dma("sp", hnc[:], hn_d.rearrange("(p o) -> p o", o=1), writes=[hnc.b])
            k.dma("sp", small[:, 0:16], lbl_d[0, :].rearrange("(h p) -> p h", p=128), writes=[small.b])
            k.dma("sp", small[:, 16:32], lbl_d[1, :].rearrange("(h p) -> p h", p=128), writes=[small.b])
        tt("dve", small[:, 32:48], small[:, 0:16], small[:, 16:32], ALU.subtract, [small.b], [small.b])
        act(lbc[:, :, 0], small[:, 32:48], AF.Sigmoid, [small.b], [lbc.b])
        ts("dve", lbc[:, :, 1], lbc[:, :, 0], -1.0, 1.0, ALU.mult, ALU.add, [lbc.b], [lbc.b])

        xnT = alloc(top, "xnT", [128, KC, TALL], BF16)
        state = {"xt": 0, "ps": 0, "ev": 0, "w": 0}

        def next_bank():
            state["ps"] = (state["ps"] + 1) % 8
            return state["ps"]

        def evac_eng():
            state["ev"] ^= 1
            return "act" if state["ev"] else "dve"

        def load_w(dst_ap, src_cols_ap, buf):
            k.dma("pool", dst_ap, src_cols_ap.rearrange("(kc p) c -> p kc c", p=128), writes=[buf])

        class _View:
            def __init__(self, ap, b):
                self.ap = ap
                self.b = b

            def __getitem__(self, key):
                return self.ap[key]

        def make_norm(stack, n_xt=2, sqj=None):
            nb = {}
            nb["gb"] = alloc(stack, "gb", [128, D], F32)
            nb["xt"] = [alloc(stack, f"xt{i}", [128, D], F32) for i in range(n_xt)]
            nb["sqj"] = sqj if sqj is not None else alloc(stack, "sqj", [128, D], BF16)
            nb["i"] = 0
            return nb

        def load_gain(nb, vec_d):
            k.dma("sp", nb["gb"][:], vec_d.partition_broadcast(128), writes=[nb["gb"].b])

        def norm_tile(nb, src_dram=None, src_sb=None, nrows=128):
            x_t = nb["xt"][nb["i"]]; nb["i"] = (nb["i"] + 1) % len(nb["xt"])
            gbt = nb["gb"]; sqj = nb["sqj"]
            if src_dram is not None:
                k.dma("sp", x_t[0:nrows, :], src_dram, writes=[x_t.b])
                xin, xb = x_t[0:nrows, :], x_t.b
            else:
                xin, xb = src_sb
            act(sqj[0:nrows, :], xin, AF.Square, [xb], [sqj.b, rs.b], accum_out=rs[0:nrows, 0:1])
            rsqrt_cols(rs[0:nrows, 1:2], rs[0:nrows, 0:1], 1.0 / D, rs.b)
            stt(x_t[0:nrows, :], xin, rs[0:nrows, 1:2], gbt[0:nrows, :], ALU.mult, ALU.mult, [xb, rs.b, gbt.b], [x_t.b])
            return x_t

        def transpose_tile(x_t, dstT, col0, nrows=128, f32dst=None):
            for q in range(4):
                bi = next_bank()
                for a in range(4):
                    kc = q * 4 + a
                    k.tr(pbank(bi)[:, a * 128:a * 128 + nrows], x_t[0:nrows, kc * 128:(kc + 1) * 128],
                         ident_f[0:nrows, 0:nrows], reads=[x_t.b, ident_f.b], writes=[PB[bi]], inc=(a == 3))
                src = pbank(bi).rearrange("p (a b) -> p a b", b=128)[:, :, 0:nrows]
                if f32dst is None:
                    cp(evac_eng(), dstT[:, q * 4:q * 4 + 4, col0:col0 + nrows], src, [PB[bi]], [dstT.b])
                else:
                    cp("act", f32dst[:, q * 4:q * 4 + 4, 0:nrows], src, [PB[bi]], [f32dst.b])
                    cp("dve", dstT[:, q * 4:q * 4 + 4, col0:col0 + nrows], f32dst[:, q * 4:q * 4 + 4, 0:nrows], [f32dst.b], [dstT.b])

        def build_tables(stack, specs, sample_col=None):
            n = len(specs)
            cosT = alloc(stack, "cosT", [128, n, 64], F32)
            sinT = alloc(stack, "sinT", [128, n, 64], F32)
            with ExitStack() as tmp:
                posi = alloc(tmp, "posi", [128, n], I32)
                posf = alloc(tmp, "posf", [128, n], F32)
                invf = alloc(tmp, "invf", [128, 64], F32)
                invi = alloc(tmp, "invi", [128, 64], I32)
                ang = alloc(tmp, "ang", [128, n, 64], F32)
                t1 = alloc(tmp, "t1", [128, n, 64], F32)
                t2 = alloc(tmp, "t2", [128, n, 64], F32)
                ti = alloc(tmp, "ti", [128, n, 64], I32)
                for ci, (base, cm_) in enumerate(specs):
                    if base is None:
                        k.op("pool", lambda ci=ci: G.iota(posi[:, ci:ci + 1], [[0, 1]], base=0, channel_multiplier=1), (), [posi.b])
                    else:
                        k.op("pool", lambda ci=ci, base=base, cm_=cm_: G.iota(posi[:, ci:ci + 1], [[0, 1]], base=base, channel_multiplier=cm_), (), [posi.b])
                cp("dve", posf[:], posi[:], [posi.b], [posf.b])
                ts("dve", posf[:], posf[:], pos0[:, 0:1], 0.0, ALU.add, ALU.max, [posf.b, pos0.b], [posf.b])
                if sample_col is not None:
                    sc = sample_col
                    k.op("dve", lambda: V.tensor_single_scalar(out=posi[:, sc:sc + 1], in_=posi[:, sc:sc + 1], scalar=7, op=ALU.bitwise_and), [posi.b], [posi.b])
                    cp("dve", posf[:, sc:sc + 1], posi[:, sc:sc + 1], [posi.b], [posf.b])
                    ts("dve", posf[:, sc:sc + 1], posf[:, sc:sc + 1], 2048.0, None, ALU.add, None, [posf.b], [posf.b])
                k.op("pool", lambda: G.iota(invi[:], [[1, 64]], base=0, channel_multiplier=0), (), [invi.b])
                cp("dve", invf[:], invi[:], [invi.b], [invf.b])
                act(invf[:], invf[:], AF.Exp, [invf.b], [invf.b], scale=-float(np.log(10000.0)) / 64.0)
                tt("dve", ang[:], posf[:].unsqueeze(2).to_broadcast([128, n, 64]),
                   invf[:].unsqueeze(1).to_broadcast([128, n, 64]), ALU.mult, [posf.b, invf.b], [ang.b])

                def sin_of(dst, shift):
                    ts("dve", t1[:], ang[:], shift, 1.0 / TWO_PI, ALU.add, ALU.mult, [ang.b], [t1.b])
                    cp("dve", ti[:], t1[:], [t1.b], [ti.b])
                    cp("dve", t2[:], ti[:], [ti.b], [t2.b])
                    ts("dve", t1[:], ang[:], shift, None, ALU.add, None, [ang.b], [t1.b])
                    stt(t1[:], t2[:], -TWO_PI, t1[:], ALU.mult, ALU.add, [t2.b, t1.b], [t1.b])
                    k.op("dve", lambda: V.tensor_single_scalar(out=t2[:], in_=t1[:], scalar=PI, op=ALU.is_gt), [t1.b], [t2.b])
                    stt(t1[:], t2[:], -TWO_PI, t1[:], ALU.mult, ALU.add, [t2.b, t1.b], [t1.b])
                    k.op("dve", lambda: V.tensor_single_scalar(out=t2[:], in_=t1[:], scalar=-PI, op=ALU.is_lt), [t1.b], [t2.b])
                    stt(t1[:], t2[:], TWO_PI, t1[:], ALU.mult, ALU.add, [t2.b, t1.b], [t1.b])
                    ts("dve", t1[:], t1[:], PI, -PI, ALU.min, ALU.max, [t1.b], [t1.b])
                    act(dst[:], t1[:], AF.Sin, [t1.b], [dst.b])

                sin_of(sinT, 0.0)
                sin_of(cosT, PI / 2)
                k.barrier()
            return cosT, sinT

        def rope(dst_ap, src_ps_ap, tabs, tabi, nrows, nh, src_bufs, dst_buf, tmpA, tmpB):
            cosT, sinT = tabs
            s4 = src_ps_ap.rearrange("p (h t f) -> p h t f", h=nh, t=2)
            d4 = dst_ap.rearrange("p (h t f) -> p h t f", h=nh, t=2)
            cosb = cosT[0:nrows, tabi, :].unsqueeze(1).to_broadcast([nrows, nh, 64])
            sinb = sinT[0:nrows, tabi, :].unsqueeze(1).to_broadcast([nrows, nh, 64])
            a3 = tmpA[0:nrows, 0:nh * 64].rearrange("p (h f) -> p h f", h=nh)
            b3 = tmpB[0:nrows, 0:nh * 64].rearrange("p (h f) -> p h f", h=nh)
            x1 = s4[:, :, 0, :]; x2 = s4[:, :, 1, :]
            sb_ = list(src_bufs)
            tt("dve", a3, x1, cosb, ALU.mult, sb_ + [cosT.b], [tmpA.b])
            tt("dve", b3, x2, sinb, ALU.mult, sb_ + [sinT.b], [tmpB.b])
            tt("dve", d4[:, :, 0, :], a3, b3, ALU.subtract, [tmpA.b, tmpB.b], [dst_buf])
            tt("dve", a3, x2, cosb, ALU.mult, sb_ + [cosT.b], [tmpA.b])
            tt("dve", b3, x1, sinb, ALU.mult, sb_ + [sinT.b], [tmpB.b])
            tt("dve", d4[:, :, 1, :], a3, b3, ALU.add, [tmpA.b, tmpB.b], [dst_buf])

        def make_hgrn_bufs(stack):
            hb = {}
            for nm in ("s1", "s2", "s3", "s4"):
                hb[nm] = alloc(stack, "h_" + nm, [128, 1024], F32)
            hb["decs"] = [alloc(stack, f"h_dec{i}", [128, 16], F32) for i in range(2)]
            hb["kT"] = alloc(stack, "h_kT", [128, 1024], BF16)
            hb["ktoks"] = [alloc(stack, f"h_ktok{i}", [128, 8, 128], BF16) for i in range(2)]
            hb["Vbs"] = [alloc(stack, f"h_Vb{i}", [128, 8, 128], BF16) for i in range(2)]
            hb["p"] = 0
            hb["dec"], hb["ktok"], hb["Vb"] = hb["decs"][0], hb["ktoks"][0], hb["Vbs"][0]
            hb["Scur"] = [alloc(stack, f"h_Scur{i}", [128, 128], F32) for i in range(2)]
            return hb

        def hgrn_set_parity(hb, p):
            hb["p"] = p
            hb["dec"], hb["ktok"], hb["Vb"] = hb["decs"][p], hb["ktoks"][p], hb["Vbs"][p]

        def hgrn_proj(hb, W, fcol, icol):
            for half in range(2):
                for kc in range(KC):
                    k.mm(pbank(half), W[:, kc, fcol:fcol + 128], xnT[:, kc, half * 512:(half + 1) * 512],
                         start=(kc == 0), stop=(kc == KC - 1), reads=[W.b, xnT.b], writes=[PB[half]])
            for tl in range(8):
                bi = 2 + tl // 4
                o = pbank(bi)[:, (tl % 4) * 128:(tl % 4 + 1) * 128]
                for kc in range(KC):
                    k.mm(o, xnT[:, kc, tl * 128:(tl + 1) * 128], W[:, kc, icol:icol + 128],
                         start=(kc == 0), stop=(kc == KC - 1), reads=[W.b, xnT.b], writes=[PB[bi]], inc=(kc == KC - 1 and tl % 4 == 3))

        def hgrn_prep(hb, h):
            s1, s2, s3, s4 = hb["s1"], hb["s2"], hb["s3"], hb["s4"]
            kT, Vb, dec = hb["kT"], hb["Vb"], hb["dec"]
            fps = pwide(0); fb = [PB[0], PB[1]]
            act(s1[:], fps, AF.Sigmoid, fb, [s1.b])
            act(s2[:], fps, AF.Sigmoid, fb, [s2.b], scale=-1.0)
            cp("act", Vb[:], pwide(1).rearrange("p (a b) -> p a b", b=128), [PB[2], PB[3]], [Vb.b])
            ts("dve", s1[:], s1[:], lbc[:, h, 1:2], lbc[:, h, 0:1], ALU.mult, ALU.add, [s1.b, lbc.b], [s1.b])
            act(s1[:], s1[:], AF.Ln, [s1.b], [s1.b])
            k.op("dve", lambda: V.tensor_tensor_scan(out=s3[:], data0=rm[:], data1=s1[:], initial=0.0, op0=ALU.mult, op1=ALU.add),
                 [rm.b, s1.b], [s3.b])
            b3 = s3[:].rearrange("p (c t) -> p c t", t=64)
            act(dec[:, 0:16], b3[:, :, 63], AF.Exp, [s3.b], [dec.b])
            tt("dve", b3, b3, b3[:, :, 63:64].to_broadcast([128, 16, 64]), ALU.subtract, [s3.b], [s3.b])
            act(s4[:], s3[:], AF.Exp, [s3.b], [s4.b], scale=-1.0)
            stt(kT[:], s2[:], lbc[:, h, 1:2], s4[:], ALU.mult, ALU.mult, [s2.b, lbc.b, s4.b], [kT.b])

        def hgrn_tok(hb):
            kT, ktok = hb["kT"], hb["ktok"]
            for tl in range(8):
                k.tr(pbank_bf(4)[:, tl * 128:(tl + 1) * 128], kT[:, tl * 128:(tl + 1) * 128], ident_b[:],
                     reads=[kT.b, ident_b.b], writes=[PB[4]], inc=(tl == 7))
            cp("act", ktok[:], pbank_bf(4).rearrange("p (a b) -> p a b", b=128), [PB[4]], [ktok.b])

        def hgrn_common(hb, S_all, h, W, fcol, icol):
            hgrn_proj(hb, W, fcol, icol)
            hgrn_prep(hb, h)
            hgrn_tok(hb)

        def hgrn_state_chain(hb, S_all, h, on_chunk=None, ubanks=(5,)):
            ktok, Vb, dec, Scur = hb["ktok"], hb["Vb"], hb["dec"], hb["Scur"]
            cur = None
            for c in range(16):
                tl, pr = c // 2, (c % 2) * 64
                if cur is None:
                    sp_ap, sp_b = S_all[:, h, :], S_all.b
                else:
                    sp_ap, sp_b = Scur[cur][:], Scur[cur].b
                if on_chunk is not None:
                    on_chunk(c, sp_ap, sp_b)
                sl_ = c % (4 * len(ubanks))
                ubi = ubanks[sl_ // 4]
                uo = pbank(ubi)[:, (sl_ % 4) * 128:(sl_ % 4 + 1) * 128]
                k.mm(uo, ktok[pr:pr + 64, tl, :], Vb[pr:pr + 64, tl, :], reads=[ktok.b, Vb.b], writes=[PB[ubi]])
                last = (c == 15)
                if last:
                    d_ap, d_b = S_all[:, h, :], S_all.b
                else:
                    nxt = 0 if cur is None else cur ^ 1
                    d_ap, d_b = Scur[nxt][:], Scur[nxt].b
                stt(d_ap, sp_ap, dec[:, c:c + 1], uo, ALU.mult, ALU.add, [sp_b, dec.b, PB[ubi]], [d_b])
                if not last:
                    cur = 0 if cur is None else cur ^ 1

        if True:
            la = ExitStack(); top.enter_context(la)
            attnT = alloc(la, "attnT", [128, 4, TALL], BF16)
            rS1 = ExitStack(); rS2 = ExitStack(); rS3 = ExitStack(); rS4 = ExitStack()
            for st_ in (rS1, rS2, rS3, rS4):
                top.enter_context(st_)
            S_all = alloc(rS1, "S_all", [128, 16, 128], F32, side="right")
            mset("pool", S_all[:], 0.0, [S_all.b])
            QTs = alloc(rS2, "QTs", [128, 12, 128], BF16, side="right")
            KTs = alloc(rS2, "KTs", [128, 12, 128], BF16, side="right")
            Vs = alloc(rS2, "Vs", [128, 12, 128], BF16, side="right")
            KTh = [alloc(rS3, f"KTh{g}", [128, 4, 128 * GROUPS[g][1]], BF16, side="right") for g in range(3)]
            Vh = [alloc(rS3, f"Vh{g}", [128, GROUPS[g][1], 512], BF16, side="right") for g in range(3)]

            with ExitStack() as p1:
                hspecs = [(-128, 1)] + [(-512 + r, 4) for r in range(4)]
                htab = {("h", 0, 0, 2): 0}
                for r in range(4):
                    htab[("h", 1, r, 2)] = 1 + r
                for beta in (1, 2):
                    for r in range(16):
                        htab[("h", 2, r, beta)] = len(hspecs)
                        hspecs.append((-3072 + 1024 * beta + r, 16))
                htabs = build_tables(p1, hspecs)
                hb = make_hgrn_bufs(p1)
                nb = make_norm(p1, sqj=_View(hb["s1"][:].bitcast(BF16), hb["s1"].b))
                WR = [alloc(p1, f"wr{i}", [128, KC, 256], BF16) for i in range(2)]
                rtA = alloc(p1, "rtA", [128, 128], F32)
                rtB = alloc(p1, "rtB", [128, 128], F32)
                krot = alloc(p1, "krot", [128, 256], BF16)
                vsh = alloc(p1, "vsh", [128, 256], BF16)

                def wslot():
                    state["w"] ^= 1
                    return WR[state["w"]]

                load_gain(nb, n_mix)
                if "skip_p1" in dbg:
                    for g_ in range(3):
                        mset("pool", KTh[g_][:], 0.0, [KTh[g_].b])
                        mset("pool", Vh[g_][:], 0.0, [Vh[g_].b])
                for beta in (range(NHB) if "skip_p1" not in dbg else ()):
                    for tl in range(8):
                        r0 = beta * 1024 + tl * 128
                        x_t = norm_tile(nb, src_dram=xh[r0:r0 + 128, :])
                        transpose_tile(x_t, xnT, tl * 128)
                    def p1_load(h):
                        W = wslot()
                        load_w(W[:, :, 0:128], w_in[:, FH + h * 128:FH + (h + 1) * 128], W.b)
                        load_w(W[:, :, 128:256], w_in[:, IH + h * 128:IH + (h + 1) * 128], W.b)
                        return W
                    import os
                    UB = tuple(int(v) for v in os.environ.get("P1_UB", "5,6,7").split(","))
                    if os.environ.get("P1_PIPE", "1") == "1":
                        Wn = p1_load(0)
                        hgrn_proj(hb, Wn, 0, 128)
                        for h in range(16):
                            hgrn_set_parity(hb, h % 2)
                            if h + 1 < 16:
                                Wn = p1_load(h + 1)
                            hgrn_prep(hb, h)
                            if h + 1 < 16:
                                hgrn_proj(hb, Wn, 0, 128)
                            hgrn_tok(hb)
                            hgrn_state_chain(hb, S_all, h, ubanks=UB)
                    else:
                        for h in range(16):
                            Wn = p1_load(h)
                            hgrn_common(hb, S_all, h, Wn, 0, 128)
                            hgrn_state_chain(hb, S_all, h, ubanks=UB)
                    for g, (win, dil) in enumerate(GROUPS):
                        first_tok = NHB * 1024 - win
                        lo = max(first_tok, beta * 1024)
                        if lo >= (beta + 1) * 1024:
                            continue
                        loc0 = lo - beta * 1024
                        per_r = (1024 - loc0) // dil
                        n_off = (lo - first_tok) // dil
                        for hf in range(2):
                            Wk = wslot(); load_w(Wk[:, :, :], w_in[:, KA + g * 512 + hf * 256:KA + g * 512 + (hf + 1) * 256], Wk.b)
                            Wv = wslot(); load_w(Wv[:, :, :], w_in[:, VA + g * 512 + hf * 256:VA + g * 512 + (hf + 1) * 256], Wv.b)
                            for r in range(dil):
                                tabi = htab[("h", g, r, beta if g == 2 else 2)]
                                cols = slice(loc0 + r, 1024, dil)
                                bi = next_bank()
                                for kc in range(KC):
                                    k.mm(pbank(bi)[0:per_r, 0:256], xnT[:, kc, cols], Wk[:, kc, :], start=(kc == 0), stop=(kc == KC - 1),
                                         reads=[xnT.b, Wk.b], writes=[PB[bi]])
                                rope(krot[0:per_r, :], pbank(bi)[0:per_r, 0:256], htabs, tabi, per_r, 2, [PB[bi]], krot.b, rtA, rtB)
                                bj = next_bank()
                                for hh in range(2):
                                    k.tr(pbank_bf(bj)[:, hh * 128:hh * 128 + per_r], krot[0:per_r, hh * 128:(hh + 1) * 128],
                                         ident_b[0:per_r, 0:per_r], reads=[krot.b, ident_b.b], writes=[PB[bj]], inc=(hh == 1))
                                dst = KTh[g][:, hf * 2:hf * 2 + 2, r * 128 + n_off:r * 128 + n_off + per_r]
                                cp(evac_eng(), dst, pbank_bf(bj)[:, 0:256].rearrange("p (h n) -> p h n", h=2)[:, :, 0:per_r], [PB[bj]], [KTh[g].b])
                                bv = next_bank()
                                for kc in range(KC):
                                    k.mm(pbank(bv)[0:per_r, 0:256], xnT[:, kc, cols], Wv[:, kc, :], start=(kc == 0), stop=(kc == KC - 1),
                                         reads=[xnT.b, Wv.b], writes=[PB[bv]])
                                vdst = Vh[g][n_off:n_off + per_r, r, hf * 256:(hf + 1) * 256]
                                if n_off == 0:
                                    cp(evac_eng(), vdst, pbank(bv)[0:per_r, 0:256], [PB[bv]], [Vh[g].b])
                                else:
                                    cp(evac_eng(), vsh[0:per_r, :], pbank(bv)[0:per_r, 0:256], [PB[bv]], [vsh.b])
                                    k.dma("sp", vdst, vsh[0:per_r, :], reads=[vsh.b], writes=[Vh[g].b])
                k.barrier()
            if stop_after == "p1":
                dbg_dump("S_all", S_all[:], [128, 16, 128], [S_all.b])
                dbg_dump("KTh2", KTh[2][:], [128, 4, 2048], [KTh[2].b])
                dbg_dump("Vh2", Vh[2][:], [128, 16, 512], [Vh[2].b])
                k.finish()
                return nc

            with ExitStack() as p2n:
                nb = make_norm(p2n)
                load_gain(nb, n_mix)
                for tl in range(9):
                    x_t = norm_tile(nb, src_dram=xo[tl * 128:(tl + 1) * 128, :])
                    transpose_tile(x_t, xnT, tl * 128)
                k.barrier()
            dbg_dump("xnT", xnT[:], [128, KC, TALL], [xnT.b])
            checkpoint("p2n")

            SCALE = float(128 ** -0.5)
            with ExitStack() as at:
                ospecs = []
                otab = {}
                for nb_ in range(8):
                    otab[("o", 0, 0, nb_)] = len(ospecs); ospecs.append((128 * nb_, 1))
                for r in range(4):
                    for nb_ in range(2):
                        otab[("o", 1, r, nb_)] = len(ospecs); ospecs.append((r + 4 * 128 * nb_, 4))
                for r in range(16):
                    otab[("o", 2, r, 0)] = len(ospecs); ospecs.append((r, 16))
                otab[("s",)] = len(ospecs); ospecs.append((None, None))
                otabs = build_tables(at, ospecs, sample_col=otab[("s",)])
                checkpoint("p2a_tab")
                if True:
                    pa = at
                    QT = [alloc(pa, f"QT{g}", [128, 1024], BF16) for g in range(3)]
                    KT = [alloc(pa, f"KT{g}", [128, 1024], BF16) for g in range(3)]
                    Vo = [alloc(pa, f"Vo{g}", [128, 8 if g < 2 else 16, 128], BF16) for g in range(3)]
                    Oacc = alloc(pa, "Oacc", [128, 1024], F32)
                    Dacc = alloc(pa, "Dacc", [128, 1024], F32)
                    Pp = alloc(pa, "Pp", [128, 512], BF16)
                    Pc = alloc(pa, "Pc", [128, 512], BF16)
                    Wqs = [alloc(pa, f"Wq{i}", [128, KC, 384], BF16) for i in range(2)]
                    rtA = alloc(pa, "rtA2", [128, 64], F32)
                    rtB = alloc(pa, "rtB2", [128, 64], F32)
                    krot = alloc(pa, "krot2", [128, 256], BF16)
                    kstage = alloc(pa, "kstage", [128, 128], F32)
                    vstage = alloc(pa, "vstage", [128, 128], F32)
                    wqi = 0
                    for h in (range(4) if "skip_mix" not in dbg else ()):
                        for g, (win, dil) in enumerate(GROUPS):
                            W = Wqs[wqi]; wqi ^= 1
                            c0 = g * 512 + h * 128
                            load_w(W[:, :, 0:128], w_in[:, QA + c0:QA + c0 + 128], W.b)
                            load_w(W[:, :, 128:256], w_in[:, KA + c0:KA + c0 + 128], W.b)
                            load_w(W[:, :, 256:384], w_in[:, VA + c0:VA + c0 + 128], W.b)
                            R = min(128, 1024 // dil)
                            nbk = (1024 // dil) // R
                            tiles = [(r, nb_) for r in range(dil) for nb_ in range(nbk)] + [("s", 0)]
                            for (r, nb_) in tiles:
                                if r == "s":
                                    cols = slice(1024, 1152); nr = 128; tabi = otab[("s",)]
                                else:
                                    t0 = r + dil * nb_ * R
                                    cols = slice(t0, t0 + dil * (R - 1) + 1, dil); nr = R; tabi = otab[("o", g, r, nb_)]
                                bi = next_bank()
                                pq = pbank(bi)
                                for which in range(3):
                                    for kc in range(KC):
                                        k.mm(pq[0:nr, which * 128:(which + 1) * 128], xnT[:, kc, cols], W[:, kc, which * 128:(which + 1) * 128],
                                             start=(kc == 0), stop=(kc == KC - 1), reads=[xnT.b, W.b], writes=[PB[bi]],
                                             inc=(kc == KC - 1 and which == 2))
                                rope(krot[0:nr, 0:128], pq[0:nr, 0:128], otabs, tabi, nr, 1, [PB[bi]], krot.b, rtA, rtB)
                                rope(kstage[0:nr, :], pq[0:nr, 128:256], otabs, tabi, nr, 1, [PB[bi]], kstage.b, rtA, rtB)
                                k.dma("sp", kvo[g, cols, 0, h * 128:(h + 1) * 128], kstage[0:nr, :], reads=[kstage.b])
                                cp("act", krot[0:nr, 128:256], kstage[0:nr, :], [kstage.b], [krot.b])
                                cp("act", vstage[0:nr, :], pq[0:nr, 256:384], [PB[bi]], [vstage.b])
                                k.dma("sp", kvo[g, cols, 1, h * 128:(h + 1) * 128], vstage[0:nr, :], reads=[vstage.b])
                                bj = next_bank()
                                k.tr(pbank_bf(bj)[:, 0:nr], krot[0:nr, 0:128], ident_b[0:nr, 0:nr], reads=[krot.b, ident_b.b], writes=[PB[bj]], inc=False)
                                k.tr(pbank_bf(bj)[:, 128:128 + nr], krot[0:nr, 128:256], ident_b[0:nr, 0:nr], reads=[krot.b, ident_b.b], writes=[PB[bj]])
                                if r == "s":
                                    cp("dve", QTs[:, g * 4 + h, :], pbank_bf(bj)[:, 0:128], [PB[bj]], [QTs.b])
                                    cp("dve", KTs[:, g * 4 + h, :], pbank_bf(bj)[:, 128:256], [PB[bj]], [KTs.b])
                                    cp("act", Vs[:, g * 4 + h, :], pq[:, 256:384], [PB[bi]], [Vs.b])
                                else:
                                    si = r * nbk + nb_
                                    cp("dve", QT[g][:, si * R:(si + 1) * R], pbank_bf(bj)[:, 0:nr], [PB[bj]], [QT[g].b])
                                    cp("dve", KT[g][:, si * R:(si + 1) * R], pbank_bf(bj)[:, 128:128 + nr], [PB[bj]], [KT[g].b])
                                    cp("act", Vo[g][0:nr, si, :], pq[0:nr, 256:384], [PB[bi]], [Vo[g].b])
                            checkpoint("p2a_proj%d%d" % (h, g))
                            nblk = dil * nbk
                            per = 512 // R
                            mprev_t = mprev if R == 128 else mprev64
                            mcur_t = mcur if R == 128 else mcur64
                            for reg in range(nblk // per):
                                blocks = list(range(reg * per, (reg + 1) * per))
                                bp, bc_, bo, bd_ = 0, 1, 2, 3
                                halo_cols = []
                                for ii, si in enumerate(blocks):
                                    r, nb_ = si // nbk, si % nbk
                                    qs = QT[g][:, si * R:(si + 1) * R]
                                    if nb_ == 0:
                                        kprev = KTh[g][:, h, r * 128:(r + 1) * 128]; kb = KTh[g].b
                                        halo_cols.append(ii)
                                    else:
                                        kprev = KT[g][:, (si - 1) * R:si * R]; kb = KT[g].b
                                    k.mm(pbank(bp)[:, ii * R:(ii + 1) * R], kprev, qs, reads=[kb, QT[g].b], writes=[PB[bp]], inc=(ii == per - 1))
                                    k.mm(pbank(bc_)[0:R, ii * R:(ii + 1) * R], KT[g][:, si * R:(si + 1) * R], qs, reads=[KT[g].b, QT[g].b],
                                         writes=[PB[bc_]], inc=(ii == per - 1))
                                act(Pp[:, :], pbank(bp), AF.Exp, [PB[bp]], [Pp.b], scale=SCALE)
                                act(Pc[0:R, :], pbank(bc_)[0:R, :], AF.Exp, [PB[bc_]], [Pc.b], scale=SCALE)
                                tt("dve", Pp[:, :], Pp[:, :], mprev_t[:].rearrange("p a b -> p (a b)"), ALU.mult, [Pp.b, mprev_t.b], [Pp.b])
                                tt("dve", Pc[0:R, :], Pc[0:R, :], mcur_t[0:R].rearrange("p a b -> p (a b)"), ALU.mult, [Pc.b, mcur_t.b], [Pc.b])
                                for ii in halo_cols:
                                    sl = slice(ii * R, (ii + 1) * R)
                                    if g == 2:
                                        ts("dve", Pp[0:64, sl], Pp[0:64, sl], hval[0:64, 1:2], None, ALU.mult, None, [Pp.b, hval.b], [Pp.b])
                                        ts("dve", Pp[64:128, sl], Pp[64:128, sl], hval[64:128, 2:3], None, ALU.mult, None, [Pp.b, hval.b], [Pp.b])
                                    else:
                                        ts("dve", Pp[:, sl], Pp[:, sl], hval[:, 2:3], None, ALU.mult, None, [Pp.b, hval.b], [Pp.b])
                                for ii, si in enumerate(blocks):
                                    r, nb_ = si // nbk, si % nbk
                                    if nb_ == 0:
                                        vprev = Vh[g][:, r, h * 128:(h + 1) * 128]; vb_ = Vh[g].b
                                    else:
                                        vprev = Vo[g][:, si - 1, :]; vb_ = Vo[g].b
                                    sl = slice(ii * R, (ii + 1) * R)
                                    k.mm(pbank(bo)[:, sl], vprev, Pp[:, sl], start=True, stop=False, reads=[vb_, Pp.b], writes=[PB[bo]])
                                    k.mm(pbank(bo)[:, sl], Vo[g][0:R, si, :], Pc[0:R, sl], start=False, stop=True, reads=[Vo[g].b, Pc.b],
                                         writes=[PB[bo]], inc=(ii == per - 1))
                                    k.mm(pbank(bd_)[:, sl], ones_b[:, :], Pp[:, sl], start=True, stop=False, reads=[ones_b.b, Pp.b], writes=[PB[bd_]])
                                    k.mm(pbank(bd_)[:, sl], ones_b[0:R, :], Pc[0:R, sl], start=False, stop=True, reads=[ones_b.b, Pc.b],
                                         writes=[PB[bd_]], inc=(ii == per - 1))
                                for ii, si in enumerate(blocks):
                                    r, nb_ = si // nbk, si % nbk
                                    t0 = r + dil * nb_ * R
                                    dsl = slice(t0, t0 + dil * (R - 1) + 1, dil)
                                    sl = slice(ii * R, (ii + 1) * R)
                                    if g == 0:
                                        cp("dve", Oacc[:, dsl], pbank(bo)[:, sl], [PB[bo]], [Oacc.b])
                                        cp("act", Dacc[:, dsl], pbank(bd_)[:, sl], [PB[bd_]], [Dacc.b])
                                    else:
                                        tt("dve", Oacc[:, dsl], Oacc[:, dsl], pbank(bo)[:, sl], ALU.add, [Oacc.b, PB[bo]], [Oacc.b])
                                        tt("dve", Dacc[:, dsl], Dacc[:, dsl], pbank(bd_)[:, sl], ALU.add, [Dacc.b, PB[bd_]], [Dacc.b])
                        k.op("dve", lambda: V.reciprocal(out=Dacc[:, :], in_=Dacc[:, :]), [Dacc.b], [Dacc.b])
                        tt("dve", attnT[:, h, 0:1024], Oacc[:, :], Dacc[:, :], ALU.mult, [Oacc.b, Dacc.b], [attnT.b])
                        checkpoint("p2a_slot%d" % h)
                    k.barrier()
            rS3.close()
            dbg_dump("attnT_p", attnT[:], [128, 4, TALL], [attnT.b])
            checkpoint("p2a_prompt")
            if True:
                with ExitStack() as sa:
                    smk = alloc(sa, "smk", [128, 13, 4, 8], BF16)
                    smn = alloc(sa, "smn", [128, 3, 128], BF16)
                    mset("pool", smk[:], 1.0, [smk.b])
                    asel(smk[:, 0, :, :], [[0, 4], [-1, 8]], ALU.is_ge, 0, 1, smk.b)
                    for rho in range(4):
                        tI = 1 + rho
                        mset("pool", smk[:, tI, :, :], 0.0, [smk.b])
                        mset("pool", smk[:, tI, :, rho:rho + 1], 1.0, [smk.b])
                        mset("pool", smk[:, tI, :, rho + 4:rho + 5], 1.0, [smk.b])
                        mset("pool", smk[0:1, tI, :, rho + 4:rho + 5], 0.0, [smk.b])
                    for rho in range(8):
                        tI = 5 + rho
                        mset("pool", smk[:, tI, :, :], 0.0, [smk.b])
                        mset("pool", smk[:, tI, :, rho:rho + 1], 1.0, [smk.b])
                    mset("pool", smn[:], 1.0, [smn.b])
                    asel(smn[:], [[0, 3], [1, 128]], ALU.is_ge, 0, -1, smn.b)
                    asel(smn[:].rearrange("p g (b i) -> p g b i", i=8), [[0, 3], [-8, 16], [0, 8]], ALU.is_ge, 0, 1, smn.b)
                    asel(smn[:, 2, :], [[1, 128]], ALU.is_equal, 0, -1, smn.b)
                    for dlt in (1, 2, 3, 5, 6, 7):
                        asel(smn[:, 1, :], [[1, 128]], ALU.not_equal, -dlt, -1, smn.b)
                    checkpoint("sa_masks")
                    ck = [alloc(sa, f"ck{i}", [128, 13, 1024], BF16) for i in range(2)]
                    ckT = alloc(sa, "ckT", [128, 13, 4, 128], BF16)
                    Ps = alloc(sa, "Ps", [128, 13, 4, 8], BF16)
                    Osm = alloc(sa, "Osm", [128, 4, 128], F32)
                    Dsm = alloc(sa, "Dsm", [128, 4, 128], F32)
                    Pn = alloc(sa, "Pn", [128, 128], BF16)
                    for h in (range(4) if "skip_mix" not in dbg else ()):
                        for g in range(3):
                            gh_ = g * 4 + h
                            k.mm(pbank(0)[:, 0:128], KTs[:, gh_, :], QTs[:, gh_, :], reads=[KTs.b, QTs.b], writes=[PB[0]])
                            act(Pn[:, :], pbank(0)[:, 0:128], AF.Exp, [PB[0]], [Pn.b], scale=SCALE)
                            tt("dve", Pn[:, :], Pn[:, :], smn[:, g, :], ALU.mult, [Pn.b, smn.b], [Pn.b])
                            k.mm(pbank(1)[:, 0:128], Vs[:, gh_, :], Pn[:, :], reads=[Vs.b, Pn.b], writes=[PB[1]])
                            k.mm(pbank(2)[:, 0:128], ones_b[:, :], Pn[:, :], reads=[ones_b.b, Pn.b], writes=[PB[2]])
                            if g == 0:
                                cp("dve", Osm[:, h, :], pbank(1)[:, 0:128], [PB[1]], [Osm.b])
                                cp("dve", Dsm[:, h, :], pbank(2)[:, 0:128], [PB[2]], [Dsm.b])
                            else:
                                tt("dve", Osm[:, h, :], Osm[:, h, :], pbank(1)[:, 0:128], ALU.add, [Osm.b, PB[1]], [Osm.b])
                                tt("dve", Dsm[:, h, :], Dsm[:, h, :], pbank(2)[:, 0:128], ALU.add, [Dsm.b, PB[2]], [Dsm.b])
                    checkpoint("sa_new")
                    for b in (range(16) if "skip_mix" not in dbg else ()):
                        if b == 1:
                            checkpoint("sa_b0")
                        C = ck[b % 2]
                        k.dma("pool", C[:, 0, :], c_d[0][b, :, :], writes=[C.b])
                        k.dma("pool", C[:, 1:5, :], c_d[1][b].rearrange("(m r) c -> m r c", r=4), writes=[C.b])
                        k.dma("pool", C[:, 5:13, :], c_d[2][b].rearrange("(m r) c -> m r c", r=16)[:, 0:8, :], writes=[C.b])
                        for tI in range(13):
                            bj = next_bank()
                            for hh in range(4):
                                k.tr(pbank_bf(bj)[:, hh * 128:(hh + 1) * 128], C[:, tI, hh * 128:(hh + 1) * 128], ident_b[:],
                                     reads=[C.b, ident_b.b], writes=[PB[bj]], inc=(hh == 3))
                            cp(evac_eng(), ckT[:, tI, :, :], pbank_bf(bj)[:, 0:512].rearrange("p (h n) -> p h n", h=4), [PB[bj]], [ckT.b])
                        bs = next_bank()
                        for tI in range(13):
                            g = 0 if tI == 0 else (1 if tI < 5 else 2)
                            for hh in range(4):
                                o = pbank(bs)[:, (tI * 4 + hh) * 8:(tI * 4 + hh + 1) * 8]
                                k.mm(o, ckT[:, tI, hh, :], QTs[:, g * 4 + hh, b * 8:(b + 1) * 8], reads=[ckT.b, QTs.b], writes=[PB[bs]],
                                     inc=(tI == 12 and hh == 3))
                        psf = Ps[:].rearrange("p a h i -> p (a h i)")
                        act(psf, pbank(bs)[:, 0:416], AF.Exp, [PB[bs]], [Ps.b], scale=SCALE)
                        tt("dve", psf, psf, smk[:].rearrange("p a h i -> p (a h i)"), ALU.mult, [Ps.b, smk.b], [Ps.b])
                        bo = next_bank()
                        for hh in range(4):
                            for tI in range(13):
                                k.mm(pbank(bo)[:, hh * 8:(hh + 1) * 8], C[:, tI, 512 + hh * 128:512 + (hh + 1) * 128], Ps[:, tI, hh, :],
                                     start=(tI == 0), stop=(tI == 12), reads=[C.b, Ps.b], writes=[PB[bo]], inc=False)
                            for tI in range(13):
                                k.mm(pbank(bo)[:, 32 + hh * 8:32 + (hh + 1) * 8], ones_b[:, :], Ps[:, tI, hh, :],
                                     start=(tI == 0), stop=(tI == 12), reads=[ones_b.b, Ps.b], writes=[PB[bo]], inc=(tI == 12 and hh == 3))
                        tt("dve", Osm[:, :, b * 8:(b + 1) * 8], Osm[:, :, b * 8:(b + 1) * 8], pbank(bo)[:, 0:32].rearrange("p (h i) -> p h i", h=4),
                           ALU.add, [Osm.b, PB[bo]], [Osm.b])
                        tt("dve", Dsm[:, :, b * 8:(b + 1) * 8], Dsm[:, :, b * 8:(b + 1) * 8], pbank(bo)[:, 32:64].rearrange("p (h i) -> p h i", h=4),
                           ALU.add, [Dsm.b, PB[bo]], [Dsm.b])
                    k.op("dve", lambda: V.reciprocal(out=Dsm[:], in_=Dsm[:]), [Dsm.b], [Dsm.b])
                    tt("dve", attnT[:, :, 1024:1152], Osm[:], Dsm[:], ALU.mult, [Osm.b, Dsm.b], [attnT.b])
                    k.barrier()
            rS2.close()
            dbg_dump("attnT", attnT[:], [128, 4, TALL], [attnT.b])
            if stop_after == "p2a":
                k.finish()
                return nc

            lh = ExitStack(); top.enter_context(lh)
            hgT = alloc(lh, "hgT", [128, 16, TALL], BF16)
            with ExitStack() as p2b:
                WR = [alloc(p2b, f"wrb{i}", [128, KC, 512], BF16) for i in range(2)]
                hb = make_hgrn_bufs(p2b)
                qT = alloc(p2b, "qT", [128, 1024], BF16)
                AT = alloc(p2b, "AT", [128, 1024], BF16)
                o2 = alloc(p2b, "o2", [128, 1024], BF16)
                Sdb = [alloc(p2b, f"Sdb{i}", [128, 128], BF16) for i in range(2)]
                S0 = alloc(p2b, "S0", [128, 16, 128], F32)
                Sn = alloc(p2b, "Sn", [128, 16, 128], F32)
                sm = {nm: alloc(p2b, "sm_" + nm, [128, 128], F32) for nm in ("a", "b", "c", "d", "e")}
                smb = {nm: alloc(p2b, "smb_" + nm, [128, 128], BF16) for nm in ("kT", "qT", "ktok", "V", "AT", "Vm", "o2")}
                sdec = alloc(p2b, "sdec", [128, 16], F32)
                s1, s2, s3, s4 = hb["s1"], hb["s2"], hb["s3"], hb["s4"]
                def p2b_load(h):
                    state["w"] ^= 1
                    W_ = WR[state["w"]]
                    load_w(W_[:, :, 0:128], w_in[:, FH + h * 128:FH + (h + 1) * 128], W_.b)
                    load_w(W_[:, :, 128:256], w_in[:, IH + h * 128:IH + (h + 1) * 128], W_.b)
                    load_w(W_[:, :, 256:384], w_in[:, QH + h * 128:QH + (h + 1) * 128], W_.b)
                    load_w(W_[:, :, 384:512], w_in[:, OG + h * 128:OG + (h + 1) * 128], W_.b)
                    return W_
                hgrn_set_parity(hb, 0)
                Wnext = p2b_load(0) if "skip_mix" not in dbg else None
                for h in (range(16) if "skip_mix" not in dbg else ()):
                    W = Wnext
                    k.dma("sp", S0[:], st_d[:, h, :, :].rearrange("b k v -> k b v"), writes=[S0.b])
                    hgrn_common(hb, S_all, h, W, 0, 128)
                    if h + 1 < 16:
                        Wnext = p2b_load(h + 1)
                    for half in range(2):
                        for kc in range(KC):
                            k.mm(pbank(half), W[:, kc, 256:384], xnT[:, kc, half * 512:(half + 1) * 512],
                                 start=(kc == 0), stop=(kc == KC - 1), reads=[W.b, xnT.b], writes=[PB[half]])
                    qb = [PB[0], PB[1]]
                    act(s1[:], pwide(0), AF.Sigmoid, qb, [s1.b])
                    act(s4[:], s3[:], AF.Exp, [s3.b], [s4.b])
                    tt("dve", s1[:], pwide(0), s1[:], ALU.mult, qb + [s1.b], [s1.b])
                    tt("dve", qT[:], s1[:], s4[:], ALU.mult, [s1.b, s4.b], [qT.b])
                    for tl in range(8):
                        bi = 2 + tl // 4
                        k.mm(pbank(bi)[:, (tl % 4) * 128:(tl % 4 + 1) * 128], hb["kT"][:, tl * 128:(tl + 1) * 128], qT[:, tl * 128:(tl + 1) * 128],
                             reads=[hb["kT"].b, qT.b], writes=[PB[bi]], inc=(tl % 4 == 3))
                    tt("dve", AT[:], pwide(1), mbd[:].rearrange("p a b -> p (a b)"), ALU.mult, [PB[2], PB[3], mbd.b], [AT.b])

                    def on_chunk(c, sp_ap, sp_b):
                        tl, pr = c // 2, (c % 2) * 64
                        sd = Sdb[c % 2]
                        ts("dve", sd[:], sp_ap, hb["dec"][:, c:c + 1], None, ALU.mult, None, [sp_b, hb["dec"].b], [sd.b])
                        bi = 6 + c // 8
                        o = pbank(bi)[:, (c % 8) * 64:(c % 8 + 1) * 64]
                        k.mm(o, sd[:], qT[:, c * 64:(c + 1) * 64], start=True, stop=False, reads=[sd.b, qT.b], writes=[PB[bi]])
                        k.mm(o, hb["Vb"][pr:pr + 64, tl, :], AT[pr:pr + 64, c * 64:(c + 1) * 64], start=False, stop=True,
                             reads=[hb["Vb"].b, AT.b], writes=[PB[bi]], inc=(c % 8 == 7))

                    hgrn_state_chain(hb, S_all, h, on_chunk=on_chunk, ubanks=(4, 5))
                    k.dma("sp", hp_d[h], S_all[:, h, :], reads=[S_all.b])
                    ob = [PB[6], PB[7]]
                    act(o2[:], pwide(3), AF.Square, ob, [o2.b])
                    for half in range(2):
                        k.mm(pbank(2 + half), ones_b[:, :], o2[:, half * 512:(half + 1) * 512], reads=[ones_b.b, o2.b], writes=[PB[2 + half]])
                    ts("dve", s2[:], pwide(1), 1.0 / 128, EPS, ALU.mult, ALU.add, [PB[2], PB[3]], [s2.b])
                    act(s2[:], s2[:], AF.Ln, [s2.b], [s2.b])
                    act(s2[:], s2[:], AF.Exp, [s2.b], [s2.b], scale=-0.5)
                    tt("dve", s3[:], pwide(3), s2[:], ALU.mult, ob + [s2.b], [s3.b])
                    for half in range(2):
                        for kc in range(KC):
                            k.mm(pbank(half), W[:, kc, 384:512], xnT[:, kc, half * 512:(half + 1) * 512],
                                 start=(kc == 0), stop=(kc == KC - 1), reads=[W.b, xnT.b], writes=[PB[half]])
                    act(s1[:], pwide(0), AF.Sigmoid, qb, [s1.b])
                    tt("dve", s1[:], pwide(0), s1[:], ALU.mult, qb + [s1.b], [s1.b])
                    stt(hgT[:, h, 0:1024], s3[:], hnc[:, 0:1], s1[:], ALU.mult, ALU.mult, [s3.b, hnc.b, s1.b], [hgT.b])

                    sc_ = slice(1024, 1152)
                    pb5 = pbank(4)
                    for which, c0_ in ((0, 0), (2, 256), (3, 384)):
                        for kc in range(KC):
                            k.mm(pb5[:, which * 128:(which + 1) * 128] if which == 0 else pb5[:, (1 if which == 2 else 3) * 128:(2 if which == 2 else 4) * 128],
                                 W[:, kc, c0_:c0_ + 128], xnT[:, kc, sc_], start=(kc == 0), stop=(kc == KC - 1),
                                 reads=[W.b, xnT.b], writes=[PB[4]], inc=False)
                    for kc in range(KC):
                        k.mm(pb5[:, 256:384], xnT[:, kc, sc_], W[:, kc, 128:256], start=(kc == 0), stop=(kc == KC - 1),
                             reads=[W.b, xnT.b], writes=[PB[4]])
                    fh_ps, qh_ps, v_ps, og_ps = pb5[:, 0:128], pb5[:, 128:256], pb5[:, 256:384], pb5[:, 384:512]
                    a_, b_, c_, d_, e_ = sm["a"], sm["b"], sm["c"], sm["d"], sm["e"]
                    act(a_[:], fh_ps, AF.Sigmoid, [PB[4]], [a_.b])
                    act(b_[:], fh_ps, AF.Sigmoid, [PB[4]], [b_.b], scale=-1.0)
                    cp("act", smb["V"][:], v_ps, [PB[4]], [smb["V"].b])
                    ts("dve", a_[:], a_[:], lbc[:, h, 1:2], lbc[:, h, 0:1], ALU.mult, ALU.add, [a_.b, lbc.b], [a_.b])
                    act(a_[:], a_[:], AF.Ln, [a_.b], [a_.b])
                    k.op("dve", lambda: V.tensor_tensor_scan(out=c_[:], data0=rm8[:], data1=a_[:], initial=0.0, op0=ALU.mult, op1=ALU.add),
                         [rm8.b, a_.b], [c_.b])
                    c3 = c_[:].rearrange("p (c t) -> p c t", t=8)
                    act(sdec[:], c3[:, :, 7], AF.Exp, [c_.b], [sdec.b])
                    tt("dve", c3, c3, c3[:, :, 7:8].to_broadcast([128, 16, 8]), ALU.subtract, [c_.b], [c_.b])
                    act(d_[:], c_[:], AF.Exp, [c_.b], [d_.b], scale=-1.0)
                    stt(smb["kT"][:], b_[:], lbc[:, h, 1:2], d_[:], ALU.mult, ALU.mult, [b_.b, lbc.b, d_.b], [smb["kT"].b])
                    act(a_[:], qh_ps, AF.Sigmoid, [PB[4]], [a_.b])
                    act(d_[:], c_[:], AF.Exp, [c_.b], [d_.b])
                    tt("dve", a_[:], qh_ps, a_[:], ALU.mult, [PB[4], a_.b], [a_.b])
                    tt("dve", smb["qT"][:], a_[:], d_[:], ALU.mult, [a_.b, d_.b], [smb["qT"].b])
                    act(e_[:], og_ps, AF.Sigmoid, [PB[4]], [e_.b])
                    tt("dve", e_[:], og_ps, e_[:], ALU.mult, [PB[4], e_.b], [e_.b])
                    k.tr(pbank_bf(5)[:, 0:128], smb["kT"][:], ident_b[:], reads=[smb["kT"].b, ident_b.b], writes=[PB[5]])
                    cp("act", smb["ktok"][:], pbank_bf(5)[:, 0:128], [PB[5]], [smb["ktok"].b])
                    k.mm(pbank(5)[:, 128:256], smb["kT"][:], smb["qT"][:], reads=[smb["kT"].b, smb["qT"].b], writes=[PB[5]])
                    tt("dve", smb["AT"][:], pbank(5)[:, 128:256], mbd8[:], ALU.mult, [PB[5], mbd8.b], [smb["AT"].b])
                    k.mm(pbank(5)[:, 256:384], smb["V"][:], smb["AT"][:], reads=[smb["V"].b, smb["AT"].b], writes=[PB[5]])
                    cp("act", b_[:], pbank(5)[:, 256:384], [PB[5]], [b_.b])
                    for b in range(16):
                        sd = Sdb[b % 2]
                        ts("dve", sd[:], S0[:, b, :], sdec[:, b:b + 1], None, ALU.mult, None, [S0.b, sdec.b], [sd.b])
                        k.mm(pbank(6)[:, b * 8:(b + 1) * 8], sd[:], smb["qT"][:, b * 8:(b + 1) * 8], reads=[sd.b, smb["qT"].b], writes=[PB[6]])
                        ts("dve", smb["Vm"][:], smb["V"][:], sel16[:, b:b + 1], None, ALU.mult, None, [smb["V"].b, sel16.b], [smb["Vm"].b])
                        uo = pbank(7)[:, (b % 4) * 128:(b % 4 + 1) * 128]
                        k.mm(uo, smb["ktok"][:], smb["Vm"][:], reads=[smb["ktok"].b, smb["Vm"].b], writes=[PB[7]])
                        stt(Sn[:, b, :], S0[:, b, :], sdec[:, b:b + 1], uo, ALU.mult, ALU.add, [S0.b, sdec.b, PB[7]], [Sn.b])
                    k.dma("sp", hs_d[:, h, :, :].rearrange("b k v -> k b v"), Sn[:], reads=[Sn.b])
                    tt("dve", b_[:], b_[:], pbank(6)[:, 0:128], ALU.add, [b_.b, PB[6]], [b_.b])
                    act(smb["o2"][:], b_[:], AF.Square, [b_.b], [smb["o2"].b])
                    k.mm(pbank(5)[:, 384:512], ones_b[:, :], smb["o2"][:], reads=[ones_b.b, smb["o2"].b], writes=[PB[5]])
                    ts("dve", a_[:], pbank(5)[:, 384:512], 1.0 / 128, EPS, ALU.mult, ALU.add, [PB[5]], [a_.b])
                    act(a_[:], a_[:], AF.Ln, [a_.b], [a_.b])
                    act(a_[:], a_[:], AF.Exp, [a_.b], [a_.b], scale=-0.5)
                    tt("dve", b_[:], b_[:], a_[:], ALU.mult, [b_.b, a_.b], [b_.b])
                    stt(hgT[:, h, 1024:1152], b_[:], hnc[:, 0:1], e_[:], ALU.mult, ALU.mult, [b_.b, hnc.b, e_.b], [hgT.b])
                k.barrier()
            rS1.close()
            dbg_dump("hgT", hgT[:], [128, 16, TALL], [hgT.b])
            if stop_after == "p2b":
                k.finish()
                return nc

            mT = alloc(rS4, "mT", [128, 16, TALL], BF16, side="right")
            TB3 = ((0, 512), (512, 1024), (1024, 1152))
            with ExitStack() as p2c:
                WR = [alloc(p2c, f"wrc{i}", [128, KC, 512], BF16) for i in range(2)]
                g1 = alloc(p2c, "g1", [128, 512], F32)
                g2 = alloc(p2c, "g2", [128, 512], F32)
                for i in (range(16) if "skip_mix" not in dbg else ()):
                    state["w"] ^= 1
                    W = WR[state["w"]]
                    cs = slice(i * 128, (i + 1) * 128)
                    load_w(W[:, :, 0:128], w_in[:, GA + i * 128:GA + (i + 1) * 128], W.b)
                    load_w(W[:, :, 128:256], w_in[:, GH + i * 128:GH + (i + 1) * 128], W.b)
                    load_w(W[:, :, 256:384], wph[:, cs], W.b)
                    load_w(W[:, 0:4, 384:512], wpa[:, cs], W.b)
                    for (a0, a1) in TB3:
                        n = a1 - a0
                        ba, bb, bc2, bd2 = next_bank(), next_bank(), next_bank(), next_bank()
                        for kc in range(KC):
                            k.mm(pbank(ba)[:, 0:n], W[:, kc, 0:128], xnT[:, kc, a0:a1], start=(kc == 0), stop=(kc == KC - 1), reads=[W.b, xnT.b], writes=[PB[ba]])
                        for kc in range(KC):
                            k.mm(pbank(bb)[:, 0:n], W[:, kc, 128:256], xnT[:, kc, a0:a1], start=(kc == 0), stop=(kc == KC - 1), reads=[W.b, xnT.b], writes=[PB[bb]])
                        for kc in range(4):
                            k.mm(pbank(bc2)[:, 0:n], W[:, kc, 384:512], attnT[:, kc, a0:a1], start=(kc == 0), stop=(kc == 3), reads=[W.b, attnT.b], writes=[PB[bc2]])
                        for kc in range(KC):
                            k.mm(pbank(bd2)[:, 0:n], W[:, kc, 256:384], hgT[:, kc, a0:a1], start=(kc == 0), stop=(kc == KC - 1), reads=[W.b, hgT.b], writes=[PB[bd2]])
                        act(g1[:, 0:n], pbank(ba)[:, 0:n], AF.Sigmoid, [PB[ba]], [g1.b])
                        act(g2[:, 0:n], pbank(bb)[:, 0:n], AF.Sigmoid, [PB[bb]], [g2.b])
                        tt("dve", g1[:, 0:n], g1[:, 0:n], pbank(bc2)[:, 0:n], ALU.mult, [g1.b, PB[bc2]], [g1.b])
                        tt("dve", g2[:, 0:n], g2[:, 0:n], pbank(bd2)[:, 0:n], ALU.mult, [g2.b, PB[bd2]], [g2.b])
                        tt("dve", mT[:, i, a0:a1], g1[:, 0:n], g2[:, 0:n], ALU.add, [g1.b, g2.b], [mT.b])
                k.barrier()
            lh.close()
            la.close()
        dbg_dump("mT", mT[:], [128, 16, TALL], [mT.b])
        if stop_after == "p2c":
            k.finish()
            return nc

        xres = alloc(top, "xres", [128, 9, D], F32)
        for tl in range(9):
            k.dma("sp", xres[:, tl, :], xo[tl * 128:(tl + 1) * 128, :], writes=[xres.b])
        with ExitStack() as p2d:
            WR = [alloc(p2d, f"wrd{i}", [128, KC, 512], BF16) for i in range(2)]
            for cb in (range(4) if "skip_mix" not in dbg else ()):
                state["w"] ^= 1
                W = WR[state["w"]]
                load_w(W[:, :, :], w_out[:, cb * 512:(cb + 1) * 512], W.b)
                for tl in range(9):
                    bi = next_bank()
                    for kc in range(KC):
                        k.mm(pbank(bi), mT[:, kc, tl * 128:(tl + 1) * 128], W[:, kc, :], start=(kc == 0), stop=(kc == KC - 1),
                             reads=[mT.b, W.b], writes=[PB[bi]])
                    xs = xres[:, tl, cb * 512:(cb + 1) * 512]
                    tt("dve", xs, xs, pbank(bi), ALU.add, [xres.b, PB[bi]], [xres.b])
            k.barrier()
        rS4.close()
        import os
        for _ in range(int(os.environ.get("DUMMY_ACT", "0"))):
            act(rs[:, 4:5], rs[:, 4:5], AF.Copy, [rs.b], [rs.b])
        for _ in range(int(os.environ.get("DUMMY_DVE", "0"))):
            cp("dve", rs[:, 5:6], rs[:, 5:6], [rs.b], [rs.b])
        dbg_dump("x1", xres[:], [128, 9, D], [xres.b])
        if stop_after == "p2d":
            k.finish()
            return nc

        SCALE = float(128 ** -0.5)
        with ExitStack() as p3:
            nb = make_norm(p3, n_xt=1)
            checkpoint("p3_alloc")
            load_gain(nb, n_cross)
            checkpoint("p3_gain")
            import os
            for tl in [int(v) for v in os.environ.get("P3_TLIST", "0,1,2,3,4,5,6,7,8").split(",")]:
                x_t = norm_tile(nb, src_sb=(xres[:, tl, :], xres.b))
                if tl == 0:
                    checkpoint("p3_n0")
                transpose_tile(x_t, xnT, tl * 128)
                if tl == 0:
                    checkpoint("p3_t0")
            checkpoint("p3_norm")
            WR = [alloc(p3, f"wre{i}", [128, KC, 256], BF16) for i in range(2)]

            def wslot3():
                state["w"] ^= 1
                return WR[state["w"]]

            memT = alloc(p3, "memT", [128, KC, 256], BF16)
            KmT = alloc(p3, "KmT", [128, 4, 256], BF16)
            Vm = alloc(p3, "Vm", [128, 2, 512], BF16)
            qcT = alloc(p3, "qcT", [128, 4, TALL], BF16)
            ocT = alloc(p3, "ocT", [128, 4, TALL], BF16)
            mst = alloc(p3, "mst", [128, 256], F32)
            load_gain(nb, n_mem)
            for mt in range(2):
                x_t = norm_tile(nb, src_dram=mp_d[mt * 128:(mt + 1) * 128, :])
                transpose_tile(x_t, memT, mt * 128)
            checkpoint("p3_memn")
            for cb in range(4):
                W = wslot3()
                load_w(W[:, :, :], w_ckv[:, cb * 256:(cb + 1) * 256], W.b)
                for mt in range(2):
                    bi = next_bank()
                    for kc in range(KC):
                        k.mm(pbank(bi)[:, 0:256], memT[:, kc, mt * 128:(mt + 1) * 128], W[:, kc, :], start=(kc == 0), stop=(kc == KC - 1),
                             reads=[memT.b, W.b], writes=[PB[bi]])
                    MSK = os.environ.get("MEMKV_SKIP", "")
                    cp("act", mst[:], pbank(bi)[:, 0:256], [PB[bi]], [mst.b])
                    if "dma" not in MSK:
                        k.dma("sp", mkv_d[mt * 128:(mt + 1) * 128, cb * 256:(cb + 1) * 256], mst[:], reads=[mst.b])
                    if cb >= 2 and "vm" not in MSK:
                        cp("dve", Vm[:, mt, (cb - 2) * 256:(cb - 1) * 256], mst[:], [mst.b], [Vm.b])
                if cb < 2 and "kt" not in MSK:
                    for hh in range(2):
                        bi = next_bank()
                        for kc in range(KC):
                            k.mm(pbank(bi)[:, 0:256], W[:, kc, hh * 128:(hh + 1) * 128], memT[:, kc, :], start=(kc == 0), stop=(kc == KC - 1),
                                 reads=[memT.b, W.b], writes=[PB[bi]])
                        cp("act", KmT[:, cb * 2 + hh, :], pbank(bi)[:, 0:256], [PB[bi]], [KmT.b])
            checkpoint("p3_mem")
            for cb in range(2):
                W = wslot3()
                load_w(W[:, :, :], w_cq[:, cb * 256:(cb + 1) * 256], W.b)
                for hh in range(2):
                    h = cb * 2 + hh
                    for (a0, a1) in TB3:
                        n = a1 - a0
                        bi = next_bank()
                        for kc in range(KC):
                            k.mm(pbank(bi)[:, 0:n], W[:, kc, hh * 128:(hh + 1) * 128], xnT[:, kc, a0:a1], start=(kc == 0), stop=(kc == KC - 1),
                                 reads=[W.b, xnT.b], writes=[PB[bi]])
                        cp(evac_eng(), qcT[:, h, a0:a1], pbank(bi)[:, 0:n], [PB[bi]], [qcT.b])
            checkpoint("p3_q")
            Pm = alloc(p3, "Pm", [128, 2, 512], BF16)
            rec = alloc(p3, "rec", [128, 512], F32)
            for h in range(4):
                for tb in range(2):
                    a0, a1 = tb * 512, (tb + 1) * 512
                    for mt in range(2):
                        k.mm(pbank(mt), KmT[:, h, mt * 128:(mt + 1) * 128], qcT[:, h, a0:a1], reads=[KmT.b, qcT.b], writes=[PB[mt]])
                    act(Pm[:].rearrange("p a b -> p (a b)"), pwide(0), AF.Exp, [PB[0], PB[1]], [Pm.b], scale=SCALE)
                    for mt in range(2):
                        k.mm(pbank(2), Vm[:, mt, h * 128:(h + 1) * 128], Pm[:, mt, :], start=(mt == 0), stop=(mt == 1), reads=[Vm.b, Pm.b], writes=[PB[2]])
                    for mt in range(2):
                        k.mm(pbank(3), ones_b[:, :], Pm[:, mt, :], start=(mt == 0), stop=(mt == 1), reads=[ones_b.b, Pm.b], writes=[PB[3]])
                    k.op("dve", lambda: V.reciprocal(out=rec[:], in_=pbank(3)), [PB[3]], [rec.b])
                    tt("dve", ocT[:, h, a0:a1], pbank(2), rec[:], ALU.mult, [PB[2], rec.b], [ocT.b])
            checkpoint("p3_prompt")
            cmb = [alloc(p3, f"cmb{i}", [128, 2, 1024], BF16) for i in range(2)]
            cKT = alloc(p3, "cKT", [128, 2, 4, 128], BF16)
            Psm = alloc(p3, "Psm", [128, 64], BF16)
            Osc = alloc(p3, "Osc", [128, 4, 128], F32)
            Dsc = alloc(p3, "Dsc", [128, 4, 128], F32)
            for b in range(16):
                C = cmb[b % 2]
                k.dma("pool", C[:], cm_d[b].rearrange("(t m) c -> m t c", m=128), writes=[C.b])
                bj = next_bank()
                for mt in range(2):
                    for hh in range(4):
                        k.tr(pbank_bf(bj)[:, (mt * 4 + hh) * 128:(mt * 4 + hh + 1) * 128], C[:, mt, hh * 128:(hh + 1) * 128], ident_b[:],
                             reads=[C.b, ident_b.b], writes=[PB[bj]], inc=(mt == 1 and hh == 3))
                cp(evac_eng(), cKT[:].rearrange("p a h n -> p (a h n)"), pbank_bf(bj), [PB[bj]], [cKT.b])
                bs = next_bank()
                for hh in range(4):
                    for mt in range(2):
                        k.mm(pbank(bs)[:, (hh * 2 + mt) * 8:(hh * 2 + mt + 1) * 8], cKT[:, mt, hh, :], qcT[:, hh, 1024 + b * 8:1024 + (b + 1) * 8],
                             reads=[cKT.b, qcT.b], writes=[PB[bs]], inc=(hh == 3 and mt == 1))
                act(Psm[:], pbank(bs)[:, 0:64], AF.Exp, [PB[bs]], [Psm.b], scale=SCALE)
                bo = next_bank()
                for hh in range(4):
                    for mt in range(2):
                        k.mm(pbank(bo)[:, hh * 8:(hh + 1) * 8], C[:, mt, 512 + hh * 128:512 + (hh + 1) * 128], Psm[:, (hh * 2 + mt) * 8:(hh * 2 + mt + 1) * 8],
                             start=(mt == 0), stop=(mt == 1), reads=[C.b, Psm.b], writes=[PB[bo]], inc=False)
                    for mt in range(2):
                        k.mm(pbank(bo)[:, 32 + hh * 8:32 + (hh + 1) * 8], ones_b[:, :], Psm[:, (hh * 2 + mt) * 8:(hh * 2 + mt + 1) * 8],
                             start=(mt == 0), stop=(mt == 1), reads=[ones_b.b, Psm.b], writes=[PB[bo]], inc=(hh == 3 and mt == 1))
                cp("dve", Osc[:, :, b * 8:(b + 1) * 8], pbank(bo)[:, 0:32].rearrange("p (h i) -> p h i", h=4), [PB[bo]], [Osc.b])
                cp("dve", Dsc[:, :, b * 8:(b + 1) * 8], pbank(bo)[:, 32:64].rearrange("p (h i) -> p h i", h=4), [PB[bo]], [Dsc.b])
            k.op("dve", lambda: V.reciprocal(out=Dsc[:], in_=Dsc[:]), [Dsc.b], [Dsc.b])
            tt("dve", ocT[:, :, 1024:1152], Osc[:], Dsc[:], ALU.mult, [Osc.b, Dsc.b], [ocT.b])
            checkpoint("p3_sample")
            for cb in range(8):
                W = wslot3()
                load_w(W[:, 0:4, :], w_co[:, cb * 256:(cb + 1) * 256], W.b)
                for tl in range(9):
                    bi = next_bank()
                    for kc in range(4):
                        k.mm(pbank(bi)[:, 0:256], ocT[:, kc, tl * 128:(tl + 1) * 128], W[:, kc, :], start=(kc == 0), stop=(kc == 3),
                             reads=[ocT.b, W.b], writes=[PB[bi]])
                    xs = xres[:, tl, cb * 256:(cb + 1) * 256]
                    tt("dve", xs, xs, pbank(bi)[:, 0:256], ALU.add, [xres.b, PB[bi]], [xres.b])
            k.barrier()
        dbg_dump("x2", xres[:], [128, 9, D], [xres.b])
        if stop_after == "p3":
            k.finish()
            return nc

        cw = alloc(top, "cw", [128, 9, 32], F32)
        with ExitStack() as p4n:
            nb = make_norm(p4n, n_xt=1)
            load_gain(nb, n_ffn)
            x32 = alloc(p4n, "x32", [128, KC, 128], F32)
            wr32 = alloc(p4n, "wr32", [128, KC, 36], F32)
            bb = alloc(p4n, "bb", [128, 36], F32)
            L = alloc(p4n, "L", [128, 36], F32)
            r_ = alloc(p4n, "r_", [128, 64], F32)
            k.dma("sp", wr32[:], w_r.rearrange("(kc p) c -> p kc c", p=128), writes=[wr32.b])
            k.dma("sp", bb[:], b_r.partition_broadcast(128), writes=[bb.b])
            for tl in range(9):
                x_t = norm_tile(nb, src_sb=(xres[:, tl, :], xres.b))
                transpose_tile(x_t, xnT, tl * 128, f32dst=x32)
                bi = next_bank()
                for kc in range(KC):
                    k.mm(pbank(bi)[:, 0:36], x32[:, kc, :], wr32[:, kc, :], start=(kc == 0), stop=(kc == KC - 1), reads=[x32.b, wr32.b], writes=[PB[bi]])
                tt("dve", L[:], pbank(bi)[:, 0:36], bb[:], ALU.add, [PB[bi], bb.b], [L.b])
                R_ = [r_.b]
                k.op("dve", lambda: V.reduce_max(out=r_[:, 0:1], in_=L[:, 0:4], axis=AX.X), [L.b], R_)
                ts("dve", r_[:, 4:8], L[:, 0:4], r_[:, 0:1], None, ALU.subtract, None, [L.b] + R_, R_)
                act(r_[:, 4:8], r_[:, 4:8], AF.Exp, R_, R_, accum_out=r_[:, 1:2])
                k.op("dve", lambda: V.reciprocal(out=r_[:, 2:3], in_=r_[:, 1:2]), R_, R_)
                ts("dve", r_[:, 8:12], L[:, 0:4], r_[:, 0:1], None, ALU.is_ge, None, [L.b] + R_, R_)
                ts("dve", r_[:, 16:24], L[:, 4:12], r_[:, 8:9], None, ALU.mult, None, [L.b] + R_, R_)
                for g in range(1, 4):
                    stt(r_[:, 16:24], L[:, 4 + 8 * g:12 + 8 * g], r_[:, 8 + g:9 + g], r_[:, 16:24], ALU.mult, ALU.add, [L.b] + R_, R_)
                k.op("dve", lambda: V.reduce_max(out=r_[:, 3:4], in_=r_[:, 16:24], axis=AX.X), R_, R_)
                ts("dve", r_[:, 24:32], r_[:, 16:24], r_[:, 3:4], None, ALU.is_ge, None, R_, R_)
                stt(r_[:, 32:40], r_[:, 24:32], -1e30, r_[:, 16:24], ALU.mult, ALU.add, R_, R_)
                k.op("dve", lambda: V.reduce_max(out=r_[:, 12:13], in_=r_[:, 32:40], axis=AX.X), R_, R_)
                ts("dve", r_[:, 40:48], r_[:, 32:40], r_[:, 12:13], None, ALU.is_ge, None, R_, R_)
                tt("dve", r_[:, 13:14], r_[:, 12:13], r_[:, 3:4], ALU.subtract, R_, R_)
                act(r_[:, 13:14], r_[:, 13:14], AF.Exp, R_, R_)
                ts("dve", r_[:, 14:15], r_[:, 13:14], 1.0, None, ALU.add, None, R_, R_)
                k.op("dve", lambda: V.reciprocal(out=r_[:, 14:15], in_=r_[:, 14:15]), R_, R_)
                tt("dve", r_[:, 14:15], r_[:, 14:15], r_[:, 2:3], ALU.mult, R_, R_)
                tt("dve", r_[:, 15:16], r_[:, 14:15], r_[:, 13:14], ALU.mult, R_, R_)
                ts("dve", r_[:, 48:56], r_[:, 24:32], r_[:, 14:15], None, ALU.mult, None, R_, R_)
                stt(r_[:, 48:56], r_[:, 40:48], r_[:, 15:16], r_[:, 48:56], ALU.mult, ALU.add, R_, R_)
                for g in range(4):
                    ts("dve", cw[:, tl, g * 8:(g + 1) * 8], r_[:, 48:56], r_[:, 8 + g:9 + g], None, ALU.mult, None, R_, [cw.b])
            k.barrier()
        dbg_dump("cw", cw[:], [128, 9, 32], [cw.b])
        with ExitStack() as p4:
            GU = [alloc(p4, f"gu{i}", [128, KC, 256], BF16) for i in range(3)]
            WD = [alloc(p4, f"wd{i}", [128, 4, D], BF16) for i in range(2)]
            hT = alloc(p4, "hT", [128, 4, TALL], BF16)
            sg = [alloc(p4, f"sg{i}", [128, 512], F32) for i in range(2)]
            gi = 0
            for e in range(N_EXP):
                Wd = WD[e % 2]
                for fb in range(4):
                    W = GU[gi % 3]; gi += 1
                    load_w(W[:, :, 0:128], weg[e, :, fb * 128:(fb + 1) * 128], W.b)
                    load_w(W[:, :, 128:256], weu[e, :, fb * 128:(fb + 1) * 128], W.b)
                    if fb == 1:
                        k.dma("pool", Wd[:], wed[e].rearrange("(kc p) c -> p kc c", p=128), writes=[Wd.b])
                    for ti_, (a0, a1) in enumerate(TB3):
                        n = a1 - a0
                        ba, bb2 = next_bank(), next_bank()
                        for kc in range(KC):
                            k.mm(pbank(ba)[:, 0:n], W[:, kc, 0:128], xnT[:, kc, a0:a1], start=(kc == 0), stop=(kc == KC - 1), reads=[W.b, xnT.b], writes=[PB[ba]])
                        for kc in range(KC):
                            k.mm(pbank(bb2)[:, 0:n], W[:, kc, 128:256], xnT[:, kc, a0:a1], start=(kc == 0), stop=(kc == KC - 1), reads=[W.b, xnT.b], writes=[PB[bb2]])
                        s_ = sg[ti_ % 2]
                        act(s_[:, 0:n], pbank(ba)[:, 0:n], AF.Silu, [PB[ba]], [s_.b])
                        tt("dve", hT[:, fb, a0:a1], s_[:, 0:n], pbank(bb2)[:, 0:n], ALU.mult, [s_.b, PB[bb2]], [hT.b])
                for tl in range(9):
                    for cb in range(4):
                        bi = next_bank()
                        for fb in range(4):
                            k.mm(pbank(bi), hT[:, fb, tl * 128:(tl + 1) * 128], Wd[:, fb, cb * 512:(cb + 1) * 512], start=(fb == 0), stop=(fb == 3),
                                 reads=[hT.b, Wd.b], writes=[PB[bi]])
                        xs = xres[:, tl, cb * 512:(cb + 1) * 512]
                        stt(xs, pbank(bi), cw[:, tl, e:e + 1], xs, ALU.mult, ALU.add, [PB[bi], cw.b, xres.b], [xres.b])
            k.barrier()
        dbg_dump("x3", xres[:], [128, 9, D], [xres.b])

        with ExitStack() as p5:
            nb = make_norm(p5, n_xt=2)
            load_gain(nb, n_fin)
            for tl in range(9):
                x_t = norm_tile(nb, src_sb=(xres[:, tl, :], xres.b))
                k.dma("sp", y_d[tl * 128:(tl + 1) * 128, :], x_t[:, :], reads=[x_t.b])
            k.finish()
    return nc


_CACHE = {}


def make_core_inputs(c, inp):
    s, j = c // 4, c % 4
    f32 = np.float32
    xp = inp["x_prompt"]
    xh = np.zeros((NHB * 1024, D), f32)
    lo = 1024 * j - NHB * 1024
    for beta in range(NHB):
        t0 = lo + beta * 1024
        if t0 >= 0:
            xh[beta * 1024:(beta + 1) * 1024] = xp[s, t0:t0 + 1024]
    xo = np.concatenate([xp[s, 1024 * j:1024 * (j + 1)], inp["x_sample"][16 * c:16 * (c + 1)].reshape(128, D)], axis=0)
    hval = np.zeros((128, 3), f32)
    for beta in range(NHB):
        hval[:, beta] = 1.0 if (lo + beta * 1024) >= 0 else 0.0
    pos0 = np.full((128, 1), 1024.0 * j, f32)
    sl = slice(16 * c, 16 * (c + 1))
    m = {
        "xh": xh, "xo": np.ascontiguousarray(xo), "hval": hval, "pos0": pos0,
        "c1": np.ascontiguousarray(inp["cache_swa1"][0, sl].reshape(16, 128, 1024)),
        "c2": np.ascontiguousarray(inp["cache_swa2"][0, sl].reshape(16, 512, 1024)),
        "c3": np.ascontiguousarray(inp["cache_swa3"][0, sl].reshape(16, 2048, 1024)),
        "st": np.ascontiguousarray(inp["state_hgrn"][0, sl]),
        "cm": np.ascontiguousarray(inp["cache_mem_kv"][0, sl].reshape(16, 256, 1024)),
        "mp": np.ascontiguousarray(inp["mem_prompt"][s]),
    }
    return m


def shared_inputs(inp):
    f32 = np.float32
    g = lambda n: np.ascontiguousarray(np.asarray(inp[n], f32))
    return {
        "lbl": g("hgrn_lb_logits"),
        "n_mix": g("norm_mix")[0], "n_cross": g("norm_cross")[0], "n_mem": g("norm_mem")[0],
        "n_ffn": g("norm_ffn")[0], "n_fin": g("norm_final"), "hn": g("hgrn_norm")[0],
        "w_in": g("w_in")[0], "wpa": g("w_proj_attn")[0], "wph": g("w_proj_hgrn")[0], "w_out": g("w_out")[0],
        "w_cq": g("w_cq")[0], "w_ckv": g("w_ckv")[0], "w_co": g("w_co")[0],
        "w_r": np.ascontiguousarray(np.concatenate([g("w_rg")[0], g("w_re")[0]], axis=1)),
        "b_r": np.ascontiguousarray(np.concatenate([g("b_rg")[0], g("b_re")[0]], axis=0)),
        "weg": g("w_e_gate")[0].reshape(N_EXP, D, 512), "weu": g("w_e_up")[0].reshape(N_EXP, D, 512),
        "wed": g("w_e_down")[0].reshape(N_EXP, 512, D),
    }


def assemble(res):
    f32 = np.float32
    y_p = np.zeros((2, 4096, D), f32); y_s = np.zeros((128, 8, D), f32)
    swa_p = [np.zeros((1, 2, w, 2, 4, 128), f32) for w in (128, 512, 2048)]
    swa_s = [np.zeros((1, 128, 8, 2, 4, 128), f32) for _ in range(3)]
    hg_p = np.zeros((1, 2, 16, 128, 128), f32); hg_s = np.zeros((1, 128, 16, 128, 128), f32)
    mkv = np.zeros((1, 2, 256, 2, 4, 128), f32)
    for c in range(8):
        r = res[c]
        s, j = c // 4, c % 4
        y_p[s, 1024 * j:1024 * (j + 1)] = r["y"][0:1024]
        y_s[16 * c:16 * (c + 1)] = r["y"][1024:].reshape(16, 8, D)
        kv = r["kvo"]
        for g in range(3):
            swa_s[g][0, 16 * c:16 * (c + 1)] = kv[g, 1024:1152].reshape(16, 8, 2, 4, 128)
        if j == 3:
            swa_p[0][0, s] = kv[0, 896:1024].reshape(128, 2, 4, 128)
            swa_p[1][0, s] = kv[1, 512:1024].reshape(512, 2, 4, 128)
            swa_p[2][0, s, 1024:2048] = kv[2, 0:1024].reshape(1024, 2, 4, 128)
            hg_p[0, s] = r["hp"]
        if j == 2:
            swa_p[2][0, s, 0:1024] = kv[2, 0:1024].reshape(1024, 2, 4, 128)
        if j == 0:
            mkv[0, s] = r["mkv"].reshape(256, 2, 4, 128)
        hg_s[0, 16 * c:16 * (c + 1)] = r["hs"]
    return (y_p, y_s, swa_p[0], swa_p[1], swa_p[2], hg_p, mkv, swa_s[0], swa_s[1], swa_s[2], hg_s)


def kernel(**inputs):
    inp = {k_: np.asarray(v) for k_, v in inputs.items()}
    if "nc" not in _CACHE:
        _CACHE["nc"] = build_program()
    nc = _CACHE["nc"]
    shared = shared_inputs(inp)
    in_maps = []
    for c in range(8):
        m = make_core_inputs(c, inp)
        m.update(shared)
        in_maps.append(m)
    res = run_bass_kernel_spmd(nc, in_maps, core_ids=list(range(8)))
    return assemble(res.results)
```

```python
import numpy as np
from contextlib import ExitStack
import concourse.bass as bass
import concourse.mybir as mybir
from concourse.bass_utils import run_bass_kernel_spmd

F32 = mybir.dt.float32
BF16 = mybir.dt.bfloat16
I32 = mybir.dt.int32
AF = mybir.ActivationFunctionType
ALU = mybir.AluOpType
AX = mybir.AxisListType

SAME_ENGINE_SYNC = True
N_DMA_SEMS = 32

D = 2048
KC = 16
TOWN = 1024
NSMP = 128
TALL = 1152
NHB = 3
N_EXP = 32
QA, KA, VA, QH, FH, IH, OG, GA, GH = 0, 1536, 3072, 4608, 6656, 8704, 10752, 12800, 14848
IN_W = 16896
EPS = 1e-6
PI = float(np.pi)
TWO_PI = float(2 * np.pi)
GROUPS = ((128, 1), (512, 4), (2048, 16))
DEBUG = {}


class Buf:
    __slots__ = ("name", "w", "r")

    def __init__(self, name):
        self.name = name
        self.w = None
        self.r = {}


class K:
    def __init__(self, nc):
        self.nc = nc
        self.eng = {"pe": nc.tensor, "act": nc.scalar, "dve": nc.vector, "pool": nc.gpsimd, "sp": nc.sync}
        self.sem = {}
        self.cnt = {}
        for e in ("pe", "act", "dve", "pool"):
            self.sem[e] = nc.alloc_semaphore(name=f"prog_{e}")
            self.cnt[e] = 0
        self.dsem = [nc.alloc_semaphore(name=f"dma_{i}") for i in range(N_DMA_SEMS)]
        self.dval = [0] * N_DMA_SEMS
        self.dnext = 0
        self.waited = {}
        self.pe_dirty = False
        self.n_inst = 0

    def _wait(self, w, ev, war=False):
        if ev is None:
            return
        kind, key, val = ev
        if kind == "eng":
            if key == w and (w == "pe" or war or not SAME_ENGINE_SYNC):
                return
            if key == "pe" and val > self.cnt["pe"]:
                self.pe_flush()
            sem = self.sem[key]
            wk = (w, "e" + key)
        else:
            sem = self.dsem[key]
            wk = (w, "d%d" % key)
        if self.waited.get(wk, 0) >= val:
            return
        self.eng[w].wait_ge(sem, val)
        self.waited[wk] = val

    def _deps(self, w, reads, writes):
        for b in reads:
            self._wait(w, b.w)
        for b in writes:
            self._wait(w, b.w)
            for ev in b.r.values():
                self._wait(w, ev, war=True)

    def _record(self, ev, reads, writes):
        rk = (ev[0], ev[1])
        for b in reads:
            b.r[rk] = ev
        for b in writes:
            b.w = ev
            b.r = {}

    def op(self, e, fn, reads=(), writes=()):
        self._deps(e, reads, writes)
        inst = fn()
        self.cnt[e] += 1
        inst.then_inc(self.sem[e], 1)
        self._record(("eng", e, self.cnt[e]), reads, writes)
        self.n_inst += 1
        return inst

    def mm(self, out, lhsT, rhs, start=True, stop=True, reads=(), writes=(), inc=None, **kw):
        self._deps("pe", reads, writes)
        inst = self.nc.tensor.matmul(out, lhsT, rhs, start=start, stop=stop, **kw)
        self._pe_after(inst, reads, writes, stop if inc is None else inc)
        return inst

    def tr(self, out, in_, ident, reads=(), writes=(), inc=True):
        self._deps("pe", reads, writes)
        inst = self.nc.tensor.transpose(out, in_, ident)
        self._pe_after(inst, reads, writes, inc)
        return inst

    def _pe_after(self, inst, reads, writes, inc):
        self.n_inst += 1
        nxt = self.cnt["pe"] + 1
        self._record(("eng", "pe", nxt), reads, writes)
        if inc:
            inst.then_inc(self.sem["pe"], 1)
            self.cnt["pe"] = nxt
            self.pe_dirty = False
        else:
            self.pe_dirty = True

    def pe_flush(self):
        if self.pe_dirty:
            inst = self.nc.tensor.nop()
            inst.then_inc(self.sem["pe"], 1)
            self.cnt["pe"] += 1
            self.pe_dirty = False

    def dma(self, q, out, in_, reads=(), writes=(), **kw):
        self._deps(q, reads, writes)
        i = self.dnext
        self.dnext = (i + 1) % N_DMA_SEMS
        if self.dval[i] > 0:
            self._wait(q, ("dma", i, self.dval[i]))
        inst = self.eng[q].dma_start(out=out, in_=in_, **kw)
        self.dval[i] += 16
        inst.then_inc(self.dsem[i], 16)
        ev = ("dma", i, self.dval[i])
        self._record(ev, reads, writes)
        self.n_inst += 1
        return ev

    def barrier(self):
        self.pe_flush()
        for w in ("pe", "act", "dve", "pool", "sp"):
            for p in ("pe", "act", "dve", "pool"):
                if p != w and self.cnt[p] > 0:
                    self._wait(w, ("eng", p, self.cnt[p]))
            for i in range(N_DMA_SEMS):
                if self.dval[i] > 0:
                    self._wait(w, ("dma", i, self.dval[i]))

    def finish(self):
        self.pe_flush()
        for p in ("pe", "act", "dve", "pool"):
            if self.cnt[p] > 0:
                self._wait("sp", ("eng", p, self.cnt[p]))
        for i in range(N_DMA_SEMS):
            if self.dval[i] > 0:
                self._wait("sp", ("dma", i, self.dval[i]))
        done = self.nc.alloc_semaphore(name="prog_done")
        for e in ("pe", "act", "dve", "sp"):
            self.eng[e].nop().then_inc(done, 1)
        self.eng["pool"].wait_ge(done, 4)


class TB:
    __slots__ = ("t", "b")

    def __init__(self, t, name):
        self.t = t
        self.b = Buf(name)

    def __getitem__(self, key):
        return self.t[key]


class _Stop(Exception):
    pass


def build_program(stop_after=None, dbg=None):
    holder = {}
    try:
        return _build_inner(stop_after, dbg, holder)
    except _Stop:
        return holder["nc"]


def _build_inner(stop_after, dbg, holder):
    dbg = dbg or {}
    nc = bass.Bass("TRN2", target_bir_lowering=False)
    holder["nc"] = nc

    def din(name, shape):
        return nc.dram_tensor(name, list(shape), F32, kind="ExternalInput").ap()

    def dout(name, shape):
        return nc.dram_tensor(name, list(shape), F32, kind="ExternalOutput").ap()

    xh = din("xh", [NHB * 1024, D]); xo = din("xo", [TALL, D])
    hval_d = din("hval", [128, 3]); pos0_d = din("pos0", [128, 1])
    c_d = [din("c1", [16, 128, 1024]), din("c2", [16, 512, 1024]), din("c3", [16, 2048, 1024])]
    st_d = din("st", [16, 16, 128, 128]); cm_d = din("cm", [16, 256, 1024]); mp_d = din("mp", [256, D])
    lbl_d = din("lbl", [2, 2048])
    n_mix = din("n_mix", [D]); n_cross = din("n_cross", [D]); n_mem = din("n_mem", [D])
    n_ffn = din("n_ffn", [D]); n_fin = din("n_fin", [D]); hn_d = din("hn", [128])
    w_in = din("w_in", [D, IN_W]); wpa = din("wpa", [512, D]); wph = din("wph", [D, D])
    w_out = din("w_out", [D, D]); w_cq = din("w_cq", [D, 512]); w_ckv = din("w_ckv", [D, 1024])
    w_co = din("w_co", [512, D]); w_r = din("w_r", [D, 36]); b_r = din("b_r", [36])
    weg = din("weg", [N_EXP, D, 512]); weu = din("weu", [N_EXP, D, 512]); wed = din("wed", [N_EXP, 512, D])
    y_d = dout("y", [TALL, D]); kvo = dout("kvo", [3, TALL, 2, 512]); hp_d = dout("hp", [16, 128, 128])
    mkv_d = dout("mkv", [256, 1024]); hs_d = dout("hs", [16, 16, 128, 128])

    k = K(nc)
    V = nc.vector
    A = nc.scalar
    G = nc.gpsimd

    def act(out, in_, func, reads, writes, **kw):
        return k.op("act", lambda: A.activation(out=out, in_=in_, func=func, **kw), reads, writes)

    def ts(e, out, in0, s1, s2, op0, op1, reads, writes):
        eng = V if e == "dve" else G
        if op1 is None:
            return k.op(e, lambda: eng.tensor_scalar(out=out, in0=in0, scalar1=s1, scalar2=None, op0=op0), reads, writes)
        return k.op(e, lambda: eng.tensor_scalar(out=out, in0=in0, scalar1=s1, scalar2=s2, op0=op0, op1=op1), reads, writes)

    def tt(e, out, in0, in1, op, reads, writes):
        eng = V if e == "dve" else G
        return k.op(e, lambda: eng.tensor_tensor(out=out, in0=in0, in1=in1, op=op), reads, writes)

    def stt(out, in0, scalar, in1, op0, op1, reads, writes):
        return k.op("dve", lambda: V.scalar_tensor_tensor(out=out, in0=in0, scalar=scalar, in1=in1, op0=op0, op1=op1), reads, writes)

    def cp(e, out, in_, reads, writes):
        if e == "act":
            return act(out, in_, AF.Copy, reads, writes)
        eng = V if e == "dve" else G
        return k.op(e, lambda: eng.tensor_copy(out=out, in_=in_), reads, writes)

    def mset(e, ap, val, writes):
        eng = V if e == "dve" else G
        return k.op(e, lambda: eng.memset(ap, val), (), writes)

    def asel(out, pattern, cmp_, base, cm, buf, fill=0.0):
        return k.op("pool", lambda: G.affine_select(out=out, in_=out, pattern=pattern, compare_op=cmp_, fill=fill, base=base,
                                                    channel_multiplier=cm), [buf], [buf])

    def rsqrt_cols(dst, src, scale, buf):
        ts("dve", dst, src, scale, EPS, ALU.mult, ALU.add, [buf], [buf])
        act(dst, dst, AF.Ln, [buf], [buf])
        act(dst, dst, AF.Exp, [buf], [buf], scale=-0.5)

    with ExitStack() as top:
        uniq = [0]

        def alloc(stack, name, shape, dt, side="left"):
            uniq[0] += 1
            nm = "sb%d_%s" % (uniq[0], name)
            return TB(stack.enter_context(nc.sbuf_tensor(nm, list(shape), dt, side=side)), nm)

        def checkpoint(name):
            if stop_after == name:
                k.finish()
                raise _Stop()

        def dbg_dump(name, ap, shape, reads):
            if name in dbg:
                o = dout("dbg_" + name, shape)
                k.dma("pool", o, ap, reads=reads)

        pst = [top.enter_context(nc.psum_tensor(f"ps{i}", [128, 1024], F32)) for i in range(4)]
        PB = [Buf(f"psb{i}") for i in range(8)]

        def pbank(i):
            return pst[i // 2][:, (i % 2) * 512:(i % 2 + 1) * 512]

        def pwide(i):
            return pst[i][:, :]

        def pbank_bf(i):
            return pbank(i).bitcast(BF16)

        ident_f = alloc(top, "ident_f", [128, 128], F32)
        ident_b = alloc(top, "ident_b", [128, 128], BF16)
        ones_b = alloc(top, "ones_b", [128, 128], BF16)
        mcur = alloc(top, "mcur", [128, 4, 128], BF16)
        mprev = alloc(top, "mprev", [128, 4, 128], BF16)
        mcur64 = alloc(top, "mcur64", [128, 8, 64], BF16)
        mprev64 = alloc(top, "mprev64", [128, 8, 64], BF16)
        mbd = alloc(top, "mbd", [128, 8, 128], F32)
        rm = alloc(top, "rm", [128, 1024], F32)
        rm8 = alloc(top, "rm8", [128, 128], F32)
        mbd8 = alloc(top, "mbd8", [128, 128], F32)
        sel16 = alloc(top, "sel16", [128, 16], F32)
        small = alloc(top, "small", [128, 64], F32)
        lbc = alloc(top, "lbc", [128, 16, 3], F32)
        hnc = alloc(top, "hnc", [128, 1], F32)
        hval = alloc(top, "hval", [128, 4], F32)
        pos0 = alloc(top, "pos0", [128, 1], F32)
        rs = alloc(top, "rs", [128, 8], F32)

        mset("pool", ident_f[:], 0.0, [ident_f.b])
        asel(ident_f[:], [[-1, 128]], ALU.not_equal, 0, 1, ident_f.b, fill=1.0)
        cp("dve", ident_b[:], ident_f[:], [ident_f.b], [ident_b.b])
        mset("pool", ones_b[:], 1.0, [ones_b.b])
        mset("pool", mcur[:], 1.0, [mcur.b])
        asel(mcur[:], [[0, 4], [1, 128]], ALU.is_ge, 0, -1, mcur.b)
        mset("pool", mprev[:], 1.0, [mprev.b])
        asel(mprev[:], [[0, 4], [-1, 128]], ALU.is_ge, 0, 1, mprev.b)
        mset("pool", mcur64[:], 1.0, [mcur64.b])
        asel(mcur64[:], [[0, 8], [1, 64]], ALU.is_ge, 0, -1, mcur64.b)
        mset("pool", mprev64[:], 1.0, [mprev64.b])
        asel(mprev64[:], [[0, 8], [-1, 64]], ALU.is_ge, 0, 1, mprev64.b)
        mset("pool", mbd[:], 1.0, [mbd.b])
        asel(mbd[:], [[0, 8], [1, 128]], ALU.is_ge, 0, -1, mbd.b)
        mset("pool", mbd[0:64, :, 64:128], 0.0, [mbd.b])
        mset("pool", rm[:], 1.0, [rm.b])
        mset("pool", rm[:].rearrange("p (c t) -> p c t", t=64)[:, :, 0:1], 0.0, [rm.b])
        mset("pool", rm8[:], 1.0, [rm8.b])
        mset("pool", rm8[:].rearrange("p (c t) -> p c t", t=8)[:, :, 0:1], 0.0, [rm8.b])
        mset("pool", mbd8[:], 1.0, [mbd8.b])
        asel(mbd8[:], [[1, 128]], ALU.is_ge, 0, -1, mbd8.b)
        asel(mbd8[:].rearrange("p (b i) -> p b i", i=8), [[-8, 16], [0, 8]], ALU.is_ge, 0, 1, mbd8.b)
        mset("pool", sel16[:], 1.0, [sel16.b])
        asel(sel16[:], [[-8, 16]], ALU.is_ge, 0, 1, sel16.b)
        asel(sel16[:], [[8, 16]], ALU.is_ge, 7, -1, sel16.b)
        mset("pool", small[:], 0.0, [small.b])
        k.dma("sp", hval[:, 0:3], hval_d, writes=[hval.b])
        k.dma("sp", pos0[:], pos0_d, writes=[pos0.b])
        with nc.allow_non_contiguous_dma(reason="tiny param vectors"):
            k.dma("sp", hnc[:], hn_d.rearrange("(p o) -> p o", o=1), writes=[hnc.b])
            k.dma("sp", small[:, 0:16], lbl_d[0, :].rearrange("(h p) -> p h", p=128), writes=[small.b])
            k.dma("sp", small[:, 16:32], lbl_d[1, :].rearrange("(h p) -> p h", p=128), writes=[small.b])
        tt("dve", small[:, 32:48], small[:, 0:16], small[:, 16:32], ALU.subtract, [small.b], [small.b])
        act(lbc[:, :, 0], small[:, 32:48], AF.Sigmoid, [small.b], [lbc.b])
        ts("dve", lbc[:, :, 1], lbc[:, :, 0], -1.0, 1.0, ALU.mult, ALU.add, [lbc.b], [lbc.b])

        xnT = alloc(top, "xnT", [128, KC, TALL], BF16)
        state = {"xt": 0, "ps": 0, "ev": 0, "w": 0}

        def next_bank():
            state["ps"] = (state["ps"] + 1) % 8
            return state["ps"]

        def evac_eng():
            state["ev"] ^= 1
            return "act" if state["ev"] else "dve"

        def load_w(dst_ap, src_cols_ap, buf):
            k.dma("pool", dst_ap, src_cols_ap.rearrange("(kc p) c -> p kc c", p=128), writes=[buf])

        class _View:
            def __init__(self, ap, b):
                self.ap = ap
                self.b = b

            def __getitem__(self, key):
                return self.ap[key]

        def make_norm(stack, n_xt=2, sqj=None):
            nb = {}
            nb["gb"] = alloc(stack, "gb", [128, D], F32)
            nb["xt"] = [alloc(stack, f"xt{i}", [128, D], F32) for i in range(n_xt)]
            nb["sqj"] = sqj if sqj is not None else alloc(stack, "sqj", [128, D], BF16)
            nb["i"] = 0
            return nb

        def load_gain(nb, vec_d):
            k.dma("sp", nb["gb"][:], vec_d.partition_broadcast(128), writes=[nb["gb"].b])

        def norm_tile(nb, src_dram=None, src_sb=None, nrows=128):
            x_t = nb["xt"][nb["i"]]; nb["i"] = (nb["i"] + 1) % len(nb["xt"])
            gbt = nb["gb"]; sqj = nb["sqj"]
            if src_dram is not None:
                k.dma("sp", x_t[0:nrows, :], src_dram, writes=[x_t.b])
                xin, xb = x_t[0:nrows, :], x_t.b
            else:
                xin, xb = src_sb
            act(sqj[0:nrows, :], xin, AF.Square, [xb], [sqj.b, rs.b], accum_out=rs[0:nrows, 0:1])
            rsqrt_cols(rs[0:nrows, 1:2], rs[0:nrows, 0:1], 1.0 / D, rs.b)
            stt(x_t[0:nrows, :], xin, rs[0:nrows, 1:2], gbt[0:nrows, :], ALU.mult, ALU.mult, [xb, rs.b, gbt.b], [x_t.b])
            return x_t

        def transpose_tile(x_t, dstT, col0, nrows=128, f32dst=None):
            for q in range(4):
                bi = next_bank()
                for a in range(4):
                    kc = q * 4 + a
                    k.tr(pbank(bi)[:, a * 128:a * 128 + nrows], x_t[0:nrows, kc * 128:(kc + 1) * 128],
                         ident_f[0:nrows, 0:nrows], reads=[x_t.b, ident_f.b], writes=[PB[bi]], inc=(a == 3))
                src = pbank(bi).rearrange("p (a b) -> p a b", b=128)[:, :, 0:nrows]
                if f32dst is None:
                    cp(evac_eng(), dstT[:, q * 4:q * 4 + 4, col0:col0 + nrows], src, [PB[bi]], [dstT.b])
                else:
                    cp("act", f32dst[:, q * 4:q * 4 + 4, 0:nrows], src, [PB[bi]], [f32dst.b])
                    cp("dve", dstT[:, q * 4:q * 4 + 4, col0:col0 + nrows], f32dst[:, q * 4:q * 4 + 4, 0:nrows], [f32dst.b], [dstT.b])

        def build_tables(stack, specs, sample_col=None):
            n = len(specs)
            cosT = alloc(stack, "cosT", [128, n, 64], F32)
            sinT = alloc(stack, "sinT", [128, n, 64], F32)
            with ExitStack() as tmp:
                posi = alloc(tmp, "posi", [128, n], I32)
                posf = alloc(tmp, "posf", [128, n], F32)
                invf = alloc(tmp, "invf", [128, 64], F32)
                invi = alloc(tmp, "invi", [128, 64], I32)
                ang = alloc(tmp, "ang", [128, n, 64], F32)
                t1 = alloc(tmp, "t1", [128, n, 64], F32)
                t2 = alloc(tmp, "t2", [128, n, 64], F32)
                ti = alloc(tmp, "ti", [128, n, 64], I32)
                for ci, (base, cm_) in enumerate(specs):
                    if base is None:
                        k.op("pool", lambda ci=ci: G.iota(posi[:, ci:ci + 1], [[0, 1]], base=0, channel_multiplier=1), (), [posi.b])
                    else:
                        k.op("pool", lambda ci=ci, base=base, cm_=cm_: G.iota(posi[:, ci:ci + 1], [[0, 1]], base=base, channel_multiplier=cm_), (), [posi.b])
                cp("dve", posf[:], posi[:], [posi.b], [posf.b])
                ts("dve", posf[:], posf[:], pos0[:, 0:1], 0.0, ALU.add, ALU.max, [posf.b, pos0.b], [posf.b])
                if sample_col is not None:
                    sc = sample_col
                    k.op("dve", lambda: V.tensor_single_scalar(out=posi[:, sc:sc + 1], in_=posi[:, sc:sc + 1], scalar=7, op=ALU.bitwise_and), [posi.b], [posi.b])
                    cp("dve", posf[:, sc:sc + 1], posi[:, sc:sc + 1], [posi.b], [posf.b])
                    ts("dve", posf[:, sc:sc + 1], posf[:, sc:sc + 1], 2048.0, None, ALU.add, None, [posf.b], [posf.b])
                k.op("pool", lambda: G.iota(invi[:], [[1, 64]], base=0, channel_multiplier=0), (), [invi.b])
                cp("dve", invf[:], invi[:], [invi.b], [invf.b])
                act(invf[:], invf[:], AF.Exp, [invf.b], [invf.b], scale=-float(np.log(10000.0)) / 64.0)
                tt("dve", ang[:], posf[:].unsqueeze(2).to_broadcast([128, n, 64]),
                   invf[:].unsqueeze(1).to_broadcast([128, n, 64]), ALU.mult, [posf.b, invf.b], [ang.b])

                def sin_of(dst, shift):
                    ts("dve", t1[:], ang[:], shift, 1.0 / TWO_PI, ALU.add, ALU.mult, [ang.b], [t1.b])
                    cp("dve", ti[:], t1[:], [t1.b], [ti.b])
                    cp("dve", t2[:], ti[:], [ti.b], [t2.b])
                    ts("dve", t1[:], ang[:], shift, None, ALU.add, None, [ang.b], [t1.b])
                    stt(t1[:], t2[:], -TWO_PI, t1[:], ALU.mult, ALU.add, [t2.b, t1.b], [t1.b])
                    k.op("dve", lambda: V.tensor_single_scalar(out=t2[:], in_=t1[:], scalar=PI, op=ALU.is_gt), [t1.b], [t2.b])
                    stt(t1[:], t2[:], -TWO_PI, t1[:], ALU.mult, ALU.add, [t2.b, t1.b], [t1.b])
                    k.op("dve", lambda: V.tensor_single_scalar(out=t2[:], in_=t1[:], scalar=-PI, op=ALU.is_lt), [t1.b], [t2.b])
                    stt(t1[:], t2[:], TWO_PI, t1[:], ALU.mult, ALU.add, [t2.b, t1.b], [t1.b])
                    ts("dve", t1[:], t1[:], PI, -PI, ALU.min, ALU.max, [t1.b], [t1.b])
                    act(dst[:], t1[:], AF.Sin, [t1.b], [dst.b])

                sin_of(sinT, 0.0)
                sin_of(cosT, PI / 2)
                k.barrier()
            return cosT, sinT

        def rope(dst_ap, src_ps_ap, tabs, tabi, nrows, nh, src_bufs, dst_buf, tmpA, tmpB):
            cosT, sinT = tabs
            s4 = src_ps_ap.rearrange("p (h t f) -> p h t f", h=nh, t=2)
            d4 = dst_ap.rearrange("p (h t f) -> p h t f", h=nh, t=2)
            cosb = cosT[0:nrows, tabi, :].unsqueeze(1).to_broadcast([nrows, nh, 64])
            sinb = sinT[0:nrows, tabi, :].unsqueeze(1).to_broadcast([nrows, nh, 64])
            a3 = tmpA[0:nrows, 0:nh * 64].rearrange("p (h f) -> p h f", h=nh)
            b3 = tmpB[0:nrows, 0:nh * 64].rearrange("p (h f) -> p h f", h=nh)
            x1 = s4[:, :, 0, :]; x2 = s4[:, :, 1, :]
            sb_ = list(src_bufs)
            tt("dve", a3, x1, cosb, ALU.mult, sb_ + [cosT.b], [tmpA.b])
            tt("dve", b3, x2, sinb, ALU.mult, sb_ + [sinT.b], [tmpB.b])
            tt("dve", d4[:, :, 0, :], a3, b3, ALU.subtract, [tmpA.b, tmpB.b], [dst_buf])
            tt("dve", a3, x2, cosb, ALU.mult, sb_ + [cosT.b], [tmpA.b])
            tt("dve", b3, x1, sinb, ALU.mult, sb_ + [sinT.b], [tmpB.b])
            tt("dve", d4[:, :, 1, :], a3, b3, ALU.add, [tmpA.b, tmpB.b], [dst_buf])

        def make_hgrn_bufs(stack):
            hb = {}
            for nm in ("s1", "s2", "s3", "s4"):
                hb[nm] = alloc(stack, "h_" + nm, [128, 1024], F32)
            hb["decs"] = [alloc(stack, f"h_dec{i}", [128, 16], F32) for i in range(2)]
            hb["kT"] = alloc(stack, "h_kT", [128, 1024], BF16)
            hb["ktoks"] = [alloc(stack, f"h_ktok{i}", [128, 8, 128], BF16) for i in range(2)]
            hb["Vbs"] = [alloc(stack, f"h_Vb{i}", [128, 8, 128], BF16) for i in range(2)]
            hb["p"] = 0
            hb["dec"], hb["ktok"], hb["Vb"] = hb["decs"][0], hb["ktoks"][0], hb["Vbs"][0]
            hb["Scur"] = [alloc(stack, f"h_Scur{i}", [128, 128], F32) for i in range(2)]
            return hb

        def hgrn_set_parity(hb, p):
            hb["p"] = p
            hb["dec"], hb["ktok"], hb["Vb"] = hb["decs"][p], hb["ktoks"][p], hb["Vbs"][p]

        def hgrn_proj(hb, W, fcol, icol, bufs=None):
            bF, bI = bufs if bufs is not None else (W.b, W.b)
            for half in range(2):
                for kc in range(KC):
                    k.mm(pbank(half), W[:, kc, fcol:fcol + 128], xnT[:, kc, half * 512:(half + 1) * 512],
                         start=(kc == 0), stop=(kc == KC - 1), reads=[bF, xnT.b], writes=[PB[half]])
            for tl in range(8):
                bi = 2 + tl // 4
                o = pbank(bi)[:, (tl % 4) * 128:(tl % 4 + 1) * 128]
                for kc in range(KC):
                    k.mm(o, xnT[:, kc, tl * 128:(tl + 1) * 128], W[:, kc, icol:icol + 128],
                         start=(kc == 0), stop=(kc == KC - 1), reads=[bI, xnT.b], writes=[PB[bi]], inc=(kc == KC - 1 and tl % 4 == 3))

        def hgrn_prep(hb, h):
            s1, s2, s3, s4 = hb["s1"], hb["s2"], hb["s3"], hb["s4"]
            kT, Vb, dec = hb["kT"], hb["Vb"], hb["dec"]
            fps = pwide(0); fb = [PB[0], PB[1]]
            act(s1[:], fps, AF.Sigmoid, fb, [s1.b])
            act(s2[:], fps, AF.Sigmoid, fb, [s2.b], scale=-1.0)
            cp("act", Vb[:], pwide(1).rearrange("p (a b) -> p a b", b=128), [PB[2], PB[3]], [Vb.b])
            ts("dve", s1[:], s1[:], lbc[:, h, 1:2], lbc[:, h, 0:1], ALU.mult, ALU.add, [s1.b, lbc.b], [s1.b])
            act(s1[:], s1[:], AF.Ln, [s1.b], [s1.b])
            k.op("dve", lambda: V.tensor_tensor_scan(out=s3[:], data0=rm[:], data1=s1[:], initial=0.0, op0=ALU.mult, op1=ALU.add),
                 [rm.b, s1.b], [s3.b])
            b3 = s3[:].rearrange("p (c t) -> p c t", t=64)
            act(dec[:, 0:16], b3[:, :, 63], AF.Exp, [s3.b], [dec.b])
            tt("dve", b3, b3, b3[:, :, 63:64].to_broadcast([128, 16, 64]), ALU.subtract, [s3.b], [s3.b])
            act(s4[:], s3[:], AF.Exp, [s3.b], [s4.b], scale=-1.0)
            stt(kT[:], s2[:], lbc[:, h, 1:2], s4[:], ALU.mult, ALU.mult, [s2.b, lbc.b, s4.b], [kT.b])

        def hgrn_tok(hb):
            kT, ktok = hb["kT"], hb["ktok"]
            for tl in range(8):
                k.tr(pbank_bf(4)[:, tl * 128:(tl + 1) * 128], kT[:, tl * 128:(tl + 1) * 128], ident_b[:],
                     reads=[kT.b, ident_b.b], writes=[PB[4]], inc=(tl == 7))
            cp("act", ktok[:], pbank_bf(4).rearrange("p (a b) -> p a b", b=128), [PB[4]], [ktok.b])

        def hgrn_common(hb, S_all, h, W, fcol, icol):
            hgrn_proj(hb, W, fcol, icol)
            hgrn_prep(hb, h)
            hgrn_tok(hb)

        def hgrn_state_chain(hb, S_all, h, on_chunk=None, ubanks=(5,)):
            ktok, Vb, dec, Scur = hb["ktok"], hb["Vb"], hb["dec"], hb["Scur"]
            cur = None
            for c in range(16):
                tl, pr = c // 2, (c % 2) * 64
                if cur is None:
                    sp_ap, sp_b = S_all[:, h, :], S_all.b
                else:
                    sp_ap, sp_b = Scur[cur][:], Scur[cur].b
                if on_chunk is not None:
                    on_chunk(c, sp_ap, sp_b)
                sl_ = c % (4 * len(ubanks))
                ubi = ubanks[sl_ // 4]
                uo = pbank(ubi)[:, (sl_ % 4) * 128:(sl_ % 4 + 1) * 128]
                k.mm(uo, ktok[pr:pr + 64, tl, :], Vb[pr:pr + 64, tl, :], reads=[ktok.b, Vb.b], writes=[PB[ubi]])
                last = (c == 15)
                if last:
                    d_ap, d_b = S_all[:, h, :], S_all.b
                else:
                    nxt = 0 if cur is None else cur ^ 1
                    d_ap, d_b = Scur[nxt][:], Scur[nxt].b
                stt(d_ap, sp_ap, dec[:, c:c + 1], uo, ALU.mult, ALU.add, [sp_b, dec.b, PB[ubi]], [d_b])
                if not last:
                    cur = 0 if cur is None else cur ^ 1

        if True:
            la = ExitStack(); top.enter_context(la)
            attnT = alloc(la, "attnT", [128, 4, TALL], BF16)
            rS1 = ExitStack(); rS2 = ExitStack(); rS3 = ExitStack(); rS4 = ExitStack()
            for st_ in (rS1, rS2, rS3, rS4):
                top.enter_context(st_)
            S_all = alloc(rS1, "S_all", [128, 16, 128], F32, side="right")
            mset("pool", S_all[:], 0.0, [S_all.b])
            QTs = alloc(rS2, "QTs", [128, 12, 128], BF16, side="right")
            KTs = alloc(rS2, "KTs", [128, 12, 128], BF16, side="right")
            Vs = alloc(rS2, "Vs", [128, 12, 128], BF16, side="right")
            KTh = [alloc(rS3, f"KTh{g}", [128, 4, 128 * GROUPS[g][1]], BF16, side="right") for g in range(3)]
            Vh = [alloc(rS3, f"Vh{g}", [128, GROUPS[g][1], 512], BF16, side="right") for g in range(3)]

            with ExitStack() as p1:
                hspecs = [(-128, 1)] + [(-512 + r, 4) for r in range(4)]
                htab = {("h", 0, 0, 2): 0}
                for r in range(4):
                    htab[("h", 1, r, 2)] = 1 + r
                for beta in (1, 2):
                    for r in range(16):
                        htab[("h", 2, r, beta)] = len(hspecs)
                        hspecs.append((-3072 + 1024 * beta + r, 16))
                htabs = build_tables(p1, hspecs)
                hb = make_hgrn_bufs(p1)
                nb = make_norm(p1, sqj=_View(hb["s1"][:].bitcast(BF16), hb["s1"].b))
                WR = [alloc(p1, f"wr{i}", [128, KC, 256], BF16) for i in range(2)]
                rtA = alloc(p1, "rtA", [128, 128], F32)
                rtB = alloc(p1, "rtB", [128, 128], F32)
                krot = alloc(p1, "krot", [128, 256], BF16)
                vsh = alloc(p1, "vsh", [128, 256], BF16)

                def wslot():
                    state["w"] ^= 1
                    return WR[state["w"]]

                wsub = {W_.b.name: (Buf(W_.b.name + "_f"), Buf(W_.b.name + "_i")) for W_ in WR}
                load_gain(nb, n_mix)
                if "skip_p1" in dbg:
                    for g_ in range(3):
                        mset("pool", KTh[g_][:], 0.0, [KTh[g_].b])
                        mset("pool", Vh[g_][:], 0.0, [Vh[g_].b])
                for beta in (range(NHB) if "skip_p1" not in dbg else ()):
                    for tl in range(8):
                        r0 = beta * 1024 + tl * 128
                        x_t = norm_tile(nb, src_dram=xh[r0:r0 + 128, :])
                        transpose_tile(x_t, xnT, tl * 128)
                    def p1_load(h):
                        W = wslot()
                        bF, bI = wsub[W.b.name]
                        load_w(W[:, :, 0:128], w_in[:, FH + h * 128:FH + (h + 1) * 128], bF)
                        load_w(W[:, :, 128:256], w_in[:, IH + h * 128:IH + (h + 1) * 128], bI)
                        return W
                    import os
                    UB = tuple(int(v) for v in os.environ.get("P1_UB", "5,6,7").split(","))
                    if os.environ.get("P1_PIPE", "1") == "1":
                        Wn = p1_load(0)
                        hgrn_proj(hb, Wn, 0, 128, bufs=wsub[Wn.b.name])
                        for h in range(16):
                            hgrn_set_parity(hb, h % 2)
                            if h + 1 < 16:
                                Wn = p1_load(h + 1)
                            hgrn_prep(hb, h)
                            if h + 1 < 16:
                                hgrn_proj(hb, Wn, 0, 128, bufs=wsub[Wn.b.name])
                            hgrn_tok(hb)
                            hgrn_state_chain(hb, S_all, h, ubanks=UB)
                    else:
                        for h in range(16):
                            Wn = p1_load(h)
                            hgrn_proj(hb, Wn, 0, 128, bufs=wsub[Wn.b.name]); hgrn_prep(hb, h); hgrn_tok(hb)
                            hgrn_state_chain(hb, S_all, h, ubanks=UB)
                    for g, (win, dil) in enumerate(GROUPS):
                        first_tok = NHB * 1024 - win
                        lo = max(first_tok, beta * 1024)
                        if lo >= (beta + 1) * 1024:
                            continue
                        loc0 = lo - beta * 1024
                        per_r = (1024 - loc0) // dil
                        n_off = (lo - first_tok) // dil
                        for hf in range(2):
                            Wk = wslot()
                            kbufs = [Wk.b] + list(wsub[Wk.b.name])
                            k.dma("pool", Wk[:, :, :], w_in[:, KA + g * 512 + hf * 256:KA + g * 512 + (hf + 1) * 256].rearrange("(kc p) c -> p kc c", p=128), writes=kbufs)
                            Wv = wslot()
                            vbufs = [Wv.b] + list(wsub[Wv.b.name])
                            k.dma("pool", Wv[:, :, :], w_in[:, VA + g * 512 + hf * 256:VA + g * 512 + (hf + 1) * 256].rearrange("(kc p) c -> p kc c", p=128), writes=vbufs)
                            for r in range(dil):
                                tabi = htab[("h", g, r, beta if g == 2 else 2)]
                                cols = slice(loc0 + r, 1024, dil)
                                bi = next_bank()
                                for kc in range(KC):
                                    k.mm(pbank(bi)[0:per_r, 0:256], xnT[:, kc, cols], Wk[:, kc, :], start=(kc == 0), stop=(kc == KC - 1),
                                         reads=[xnT.b] + kbufs, writes=[PB[bi]])
                                rope(krot[0:per_r, :], pbank(bi)[0:per_r, 0:256], htabs, tabi, per_r, 2, [PB[bi]], krot.b, rtA, rtB)
                                bj = next_bank()
                                for hh in range(2):
                                    k.tr(pbank_bf(bj)[:, hh * 128:hh * 128 + per_r], krot[0:per_r, hh * 128:(hh + 1) * 128],
                                         ident_b[0:per_r, 0:per_r], reads=[krot.b, ident_b.b], writes=[PB[bj]], inc=(hh == 1))
                                dst = KTh[g][:, hf * 2:hf * 2 + 2, r * 128 + n_off:r * 128 + n_off + per_r]
                                cp(evac_eng(), dst, pbank_bf(bj)[:, 0:256].rearrange("p (h n) -> p h n", h=2)[:, :, 0:per_r], [PB[bj]], [KTh[g].b])
                                bv = next_bank()
                                for kc in range(KC):
                                    k.mm(pbank(bv)[0:per_r, 0:256], xnT[:, kc, cols], Wv[:, kc, :], start=(kc == 0), stop=(kc == KC - 1),
                                         reads=[xnT.b] + vbufs, writes=[PB[bv]])
                                vdst = Vh[g][n_off:n_off + per_r, r, hf * 256:(hf + 1) * 256]
                                if n_off == 0:
                                    cp(evac_eng(), vdst, pbank(bv)[0:per_r, 0:256], [PB[bv]], [Vh[g].b])
                                else:
                                    cp(evac_eng(), vsh[0:per_r, :], pbank(bv)[0:per_r, 0:256], [PB[bv]], [vsh.b])
                                    k.dma("sp", vdst, vsh[0:per_r, :], reads=[vsh.b], writes=[Vh[g].b])
                k.barrier()
            if stop_after == "p1":
                dbg_dump("S_all", S_all[:], [128, 16, 128], [S_all.b])
                dbg_dump("KTh2", KTh[2][:], [128, 4, 2048], [KTh[2].b])
                dbg_dump("Vh2", Vh[2][:], [128, 16, 512], [Vh[2].b])
                k.finish()
                return nc

            with ExitStack() as p2n:
                nb = make_norm(p2n)
                load_gain(nb, n_mix)
                for tl in range(9):
                    x_t = norm_tile(nb, src_dram=xo[tl * 128:(tl + 1) * 128, :])
                    transpose_tile(x_t, xnT, tl * 128)
                k.barrier()
            dbg_dump("xnT", xnT[:], [128, KC, TALL], [xnT.b])
            checkpoint("p2n")

            SCALE = float(128 ** -0.5)
            with ExitStack() as at:
                ospecs = []
                otab = {}
                for nb_ in range(8):
                    otab[("o", 0, 0, nb_)] = len(ospecs); ospecs.append((128 * nb_, 1))
                for r in range(4):
                    for nb_ in range(2):
                        otab[("o", 1, r, nb_)] = len(ospecs); ospecs.append((r + 4 * 128 * nb_, 4))
                for r in range(16):
                    otab[("o", 2, r, 0)] = len(ospecs); ospecs.append((r, 16))
                otab[("s",)] = len(ospecs); ospecs.append((None, None))
                otabs = build_tables(at, ospecs, sample_col=otab[("s",)])
                checkpoint("p2a_tab")
                if True:
                    pa = at
                    QT = [alloc(pa, f"QT{g}", [128, 1024], BF16) for g in range(3)]
                    KT = [alloc(pa, f"KT{g}", [128, 1024], BF16) for g in range(3)]
                    Vo = [alloc(pa, f"Vo{g}", [128, 8 if g < 2 else 16, 128], BF16) for g in range(3)]
                    Oacc = alloc(pa, "Oacc", [128, 1024], F32)
                    Dacc = alloc(pa, "Dacc", [128, 1024], F32)
                    Pp = alloc(pa, "Pp", [128, 512], BF16)
                    Pc = alloc(pa, "Pc", [128, 512], BF16)
                    Wqs = [alloc(pa, f"Wq{i}", [128, KC, 384], BF16) for i in range(2)]
                    rtA = alloc(pa, "rtA2", [128, 64], F32)
                    rtB = alloc(pa, "rtB2", [128, 64], F32)
                    krot = alloc(pa, "krot2", [128, 256], BF16)
                    kstage = alloc(pa, "kstage", [128, 128], F32)
                    vstage = alloc(pa, "vstage", [128, 128], F32)
                    wqi = 0
                    for h in (range(4) if "skip_mix" not in dbg else ()):
                        for g, (win, dil) in enumerate(GROUPS):
                            W = Wqs[wqi]; wqi ^= 1
                            c0 = g * 512 + h * 128
                            load_w(W[:, :, 0:128], w_in[:, QA + c0:QA + c0 + 128], W.b)
                            load_w(W[:, :, 128:256], w_in[:, KA + c0:KA + c0 + 128], W.b)
                            load_w(W[:, :, 256:384], w_in[:, VA + c0:VA + c0 + 128], W.b)
                            R = min(128, 1024 // dil)
                            nbk = (1024 // dil) // R
                            tiles = [(r, nb_) for r in range(dil) for nb_ in range(nbk)] + [("s", 0)]
                            for (r, nb_) in tiles:
                                if r == "s":
                                    cols = slice(1024, 1152); nr = 128; tabi = otab[("s",)]
                                else:
                                    t0 = r + dil * nb_ * R
                                    cols = slice(t0, t0 + dil * (R - 1) + 1, dil); nr = R; tabi = otab[("o", g, r, nb_)]
                                bi = next_bank()
                                pq = pbank(bi)
                                for which in range(3):
                                    for kc in range(KC):
                                        k.mm(pq[0:nr, which * 128:(which + 1) * 128], xnT[:, kc, cols], W[:, kc, which * 128:(which + 1) * 128],
                                             start=(kc == 0), stop=(kc == KC - 1), reads=[xnT.b, W.b], writes=[PB[bi]],
                                             inc=(kc == KC - 1 and which == 2))
                                rope(krot[0:nr, 0:128], pq[0:nr, 0:128], otabs, tabi, nr, 1, [PB[bi]], krot.b, rtA, rtB)
                                rope(kstage[0:nr, :], pq[0:nr, 128:256], otabs, tabi, nr, 1, [PB[bi]], kstage.b, rtA, rtB)
                                k.dma("sp", kvo[g, cols, 0, h * 128:(h + 1) * 128], kstage[0:nr, :], reads=[kstage.b])
                                cp("act", krot[0:nr, 128:256], kstage[0:nr, :], [kstage.b], [krot.b])
                                cp("act", vstage[0:nr, :], pq[0:nr, 256:384], [PB[bi]], [vstage.b])
                                k.dma("sp", kvo[g, cols, 1, h * 128:(h + 1) * 128], vstage[0:nr, :], reads=[vstage.b])
                                bj = next_bank()
                                k.tr(pbank_bf(bj)[:, 0:nr], krot[0:nr, 0:128], ident_b[0:nr, 0:nr], reads=[krot.b, ident_b.b], writes=[PB[bj]], inc=False)
                                k.tr(pbank_bf(bj)[:, 128:128 + nr], krot[0:nr, 128:256], ident_b[0:nr, 0:nr], reads=[krot.b, ident_b.b], writes=[PB[bj]])
                                if r == "s":
                                    cp("dve", QTs[:, g * 4 + h, :], pbank_bf(bj)[:, 0:128], [PB[bj]], [QTs.b])
                                    cp("dve", KTs[:, g * 4 + h, :], pbank_bf(bj)[:, 128:256], [PB[bj]], [KTs.b])
                                    cp("act", Vs[:, g * 4 + h, :], pq[:, 256:384], [PB[bi]], [Vs.b])
                                else:
                                    si = r * nbk + nb_
                                    cp("dve", QT[g][:, si * R:(si + 1) * R], pbank_bf(bj)[:, 0:nr], [PB[bj]], [QT[g].b])
                                    cp("dve", KT[g][:, si * R:(si + 1) * R], pbank_bf(bj)[:, 128:128 + nr], [PB[bj]], [KT[g].b])
                                    cp("act", Vo[g][0:nr, si, :], pq[0:nr, 256:384], [PB[bi]], [Vo[g].b])
                            checkpoint("p2a_proj%d%d" % (h, g))
                            nblk = dil * nbk
                            per = 512 // R
                            mprev_t = mprev if R == 128 else mprev64
                            mcur_t = mcur if R == 128 else mcur64
                            for reg in range(nblk // per):
                                blocks = list(range(reg * per, (reg + 1) * per))
                                bp, bc_, bo, bd_ = 0, 1, 2, 3
                                halo_cols = []
                                for ii, si in enumerate(blocks):
                                    r, nb_ = si // nbk, si % nbk
                                    qs = QT[g][:, si * R:(si + 1) * R]
                                    if nb_ == 0:
                                        kprev = KTh[g][:, h, r * 128:(r + 1) * 128]; kb = KTh[g].b
                                        halo_cols.append(ii)
                                    else:
                                        kprev = KT[g][:, (si - 1) * R:si * R]; kb = KT[g].b
                                    k.mm(pbank(bp)[:, ii * R:(ii + 1) * R], kprev, qs, reads=[kb, QT[g].b], writes=[PB[bp]], inc=(ii == per - 1))
                                    k.mm(pbank(bc_)[0:R, ii * R:(ii + 1) * R], KT[g][:, si * R:(si + 1) * R], qs, reads=[KT[g].b, QT[g].b],
                                         writes=[PB[bc_]], inc=(ii == per - 1))
                                act(Pp[:, :], pbank(bp), AF.Exp, [PB[bp]], [Pp.b], scale=SCALE)
                                act(Pc[0:R, :], pbank(bc_)[0:R, :], AF.Exp, [PB[bc_]], [Pc.b], scale=SCALE)
                                tt("dve", Pp[:, :], Pp[:, :], mprev_t[:].rearrange("p a b -> p (a b)"), ALU.mult, [Pp.b, mprev_t.b], [Pp.b])
                                tt("dve", Pc[0:R, :], Pc[0:R, :], mcur_t[0:R].rearrange("p a b -> p (a b)"), ALU.mult, [Pc.b, mcur_t.b], [Pc.b])
                                for ii in halo_cols:
                                    sl = slice(ii * R, (ii + 1) * R)
                                    if g == 2:
                                        ts("dve", Pp[0:64, sl], Pp[0:64, sl], hval[0:64, 1:2], None, ALU.mult, None, [Pp.b, hval.b], [Pp.b])
                                        ts("dve", Pp[64:128, sl], Pp[64:128, sl], hval[64:128, 2:3], None, ALU.mult, None, [Pp.b, hval.b], [Pp.b])
                                    else:
                                        ts("dve", Pp[:, sl], Pp[:, sl], hval[:, 2:3], None, ALU.mult, None, [Pp.b, hval.b], [Pp.b])
                                for ii, si in enumerate(blocks):
                                    r, nb_ = si // nbk, si % nbk
                                    if nb_ == 0:
                                        vprev = Vh[g][:, r, h * 128:(h + 1) * 128]; vb_ = Vh[g].b
                                    else:
                                        vprev = Vo[g][:, si - 1, :]; vb_ = Vo[g].b
                                    sl = slice(ii * R, (ii + 1) * R)
                                    k.mm(pbank(bo)[:, sl], vprev, Pp[:, sl], start=True, stop=False, reads=[vb_, Pp.b], writes=[PB[bo]])
                                    k.mm(pbank(bo)[:, sl], Vo[g][0:R, si, :], Pc[0:R, sl], start=False, stop=True, reads=[Vo[g].b, Pc.b],
                                         writes=[PB[bo]], inc=(ii == per - 1))
                                    k.mm(pbank(bd_)[:, sl], ones_b[:, :], Pp[:, sl], start=True, stop=False, reads=[ones_b.b, Pp.b], writes=[PB[bd_]])
                                    k.mm(pbank(bd_)[:, sl], ones_b[0:R, :], Pc[0:R, sl], start=False, stop=True, reads=[ones_b.b, Pc.b],
                                         writes=[PB[bd_]], inc=(ii == per - 1))
                                for ii, si in enumerate(blocks):
                                    r, nb_ = si // nbk, si % nbk
                                    t0 = r + dil * nb_ * R
                                    dsl = slice(t0, t0 + dil * (R - 1) + 1, dil)
                                    sl = slice(ii * R, (ii + 1) * R)
                                    if g == 0:
                                        cp("dve", Oacc[:, dsl], pbank(bo)[:, sl], [PB[bo]], [Oacc.b])
                                        cp("act", Dacc[:, dsl], pbank(bd_)[:, sl], [PB[bd_]], [Dacc.b])
                                    else:
                                        tt("dve", Oacc[:, dsl], Oacc[:, dsl], pbank(bo)[:, sl], ALU.add, [Oacc.b, PB[bo]], [Oacc.b])
                                        tt("dve", Dacc[:, dsl], Dacc[:, dsl], pbank(bd_)[:, sl], ALU.add, [Dacc.b, PB[bd_]], [Dacc.b])
                        k.op("dve", lambda: V.reciprocal(out=Dacc[:, :], in_=Dacc[:, :]), [Dacc.b], [Dacc.b])
                        tt("dve", attnT[:, h, 0:1024], Oacc[:, :], Dacc[:, :], ALU.mult, [Oacc.b, Dacc.b], [attnT.b])
                        checkpoint("p2a_slot%d" % h)
                    k.barrier()
            rS3.close()
            dbg_dump("attnT_p", attnT[:], [128, 4, TALL], [attnT.b])
            checkpoint("p2a_prompt")
            if True:
                with ExitStack() as sa:
                    smk = alloc(sa, "smk", [128, 13, 4, 8], BF16)
                    smn = alloc(sa, "smn", [128, 3, 128], BF16)
                    mset("pool", smk[:], 1.0, [smk.b])
                    asel(smk[:, 0, :, :], [[0, 4], [-1, 8]], ALU.is_ge, 0, 1, smk.b)
                    for rho in range(4):
                        tI = 1 + rho
                        mset("pool", smk[:, tI, :, :], 0.0, [smk.b])
                        mset("pool", smk[:, tI, :, rho:rho + 1], 1.0, [smk.b])
                        mset("pool", smk[:, tI, :, rho + 4:rho + 5], 1.0, [smk.b])
                        mset("pool", smk[0:1, tI, :, rho + 4:rho + 5], 0.0, [smk.b])
                    for rho in range(8):
                        tI = 5 + rho
                        mset("pool", smk[:, tI, :, :], 0.0, [smk.b])
                        mset("pool", smk[:, tI, :, rho:rho + 1], 1.0, [smk.b])
                    mset("pool", smn[:], 1.0, [smn.b])
                    asel(smn[:], [[0, 3], [1, 128]], ALU.is_ge, 0, -1, smn.b)
                    asel(smn[:].rearrange("p g (b i) -> p g b i", i=8), [[0, 3], [-8, 16], [0, 8]], ALU.is_ge, 0, 1, smn.b)
                    asel(smn[:, 2, :], [[1, 128]], ALU.is_equal, 0, -1, smn.b)
                    for dlt in (1, 2, 3, 5, 6, 7):
                        asel(smn[:, 1, :], [[1, 128]], ALU.not_equal, -dlt, -1, smn.b)
                    checkpoint("sa_masks")
                    ck = [alloc(sa, f"ck{i}", [128, 13, 1024], BF16) for i in range(2)]
                    ckT = alloc(sa, "ckT", [128, 13, 4, 128], BF16)
                    Ps = alloc(sa, "Ps", [128, 13, 4, 8], BF16)
                    Osm = alloc(sa, "Osm", [128, 4, 128], F32)
                    Dsm = alloc(sa, "Dsm", [128, 4, 128], F32)
                    Pn = alloc(sa, "Pn", [128, 128], BF16)
                    for h in (range(4) if "skip_mix" not in dbg else ()):
                        for g in range(3):
                            gh_ = g * 4 + h
                            k.mm(pbank(0)[:, 0:128], KTs[:, gh_, :], QTs[:, gh_, :], reads=[KTs.b, QTs.b], writes=[PB[0]])
                            act(Pn[:, :], pbank(0)[:, 0:128], AF.Exp, [PB[0]], [Pn.b], scale=SCALE)
                            tt("dve", Pn[:, :], Pn[:, :], smn[:, g, :], ALU.mult, [Pn.b, smn.b], [Pn.b])
                            k.mm(pbank(1)[:, 0:128], Vs[:, gh_, :], Pn[:, :], reads=[Vs.b, Pn.b], writes=[PB[1]])
                            k.mm(pbank(2)[:, 0:128], ones_b[:, :], Pn[:, :], reads=[ones_b.b, Pn.b], writes=[PB[2]])
                            if g == 0:
                                cp("dve", Osm[:, h, :], pbank(1)[:, 0:128], [PB[1]], [Osm.b])
                                cp("dve", Dsm[:, h, :], pbank(2)[:, 0:128], [PB[2]], [Dsm.b])
                            else:
                                tt("dve", Osm[:, h, :], Osm[:, h, :], pbank(1)[:, 0:128], ALU.add, [Osm.b, PB[1]], [Osm.b])
                                tt("dve", Dsm[:, h, :], Dsm[:, h, :], pbank(2)[:, 0:128], ALU.add, [Dsm.b, PB[2]], [Dsm.b])
                    checkpoint("sa_new")
                    for b in (range(16) if "skip_mix" not in dbg else ()):
                        if b == 1:
                            checkpoint("sa_b0")
                        C = ck[b % 2]
                        k.dma("pool", C[:, 0, :], c_d[0][b, :, :], writes=[C.b])
                        k.dma("pool", C[:, 1:5, :], c_d[1][b].rearrange("(m r) c -> m r c", r=4), writes=[C.b])
                        k.dma("pool", C[:, 5:13, :], c_d[2][b].rearrange("(m r) c -> m r c", r=16)[:, 0:8, :], writes=[C.b])
                        for tI in range(13):
                            bj = next_bank()
                            for hh in range(4):
                                k.tr(pbank_bf(bj)[:, hh * 128:(hh + 1) * 128], C[:, tI, hh * 128:(hh + 1) * 128], ident_b[:],
                                     reads=[C.b, ident_b.b], writes=[PB[bj]], inc=(hh == 3))
                            cp(evac_eng(), ckT[:, tI, :, :], pbank_bf(bj)[:, 0:512].rearrange("p (h n) -> p h n", h=4), [PB[bj]], [ckT.b])
                        bs = next_bank()
                        for tI in range(13):
                            g = 0 if tI == 0 else (1 if tI < 5 else 2)
                            for hh in range(4):
                                o = pbank(bs)[:, (tI * 4 + hh) * 8:(tI * 4 + hh + 1) * 8]
                                k.mm(o, ckT[:, tI, hh, :], QTs[:, g * 4 + hh, b * 8:(b + 1) * 8], reads=[ckT.b, QTs.b], writes=[PB[bs]],
                                     inc=(tI == 12 and hh == 3))
                        psf = Ps[:].rearrange("p a h i -> p (a h i)")
                        act(psf, pbank(bs)[:, 0:416], AF.Exp, [PB[bs]], [Ps.b], scale=SCALE)
                        tt("dve", psf, psf, smk[:].rearrange("p a h i -> p (a h i)"), ALU.mult, [Ps.b, smk.b], [Ps.b])
                        bo = next_bank()
                        for hh in range(4):
                            for tI in range(13):
                                k.mm(pbank(bo)[:, hh * 8:(hh + 1) * 8], C[:, tI, 512 + hh * 128:512 + (hh + 1) * 128], Ps[:, tI, hh, :],
                                     start=(tI == 0), stop=(tI == 12), reads=[C.b, Ps.b], writes=[PB[bo]], inc=False)
                            for tI in range(13):
                                k.mm(pbank(bo)[:, 32 + hh * 8:32 + (hh + 1) * 8], ones_b[:, :], Ps[:, tI, hh, :],
                                     start=(tI == 0), stop=(tI == 12), reads=[ones_b.b, Ps.b], writes=[PB[bo]], inc=(tI == 12 and hh == 3))
                        tt("dve", Osm[:, :, b * 8:(b + 1) * 8], Osm[:, :, b * 8:(b + 1) * 8], pbank(bo)[:, 0:32].rearrange("p (h i) -> p h i", h=4),
                           ALU.add, [Osm.b, PB[bo]], [Osm.b])
                        tt("dve", Dsm[:, :, b * 8:(b + 1) * 8], Dsm[:, :, b * 8:(b + 1) * 8], pbank(bo)[:, 32:64].rearrange("p (h i) -> p h i", h=4),
                           ALU.add, [Dsm.b, PB[bo]], [Dsm.b])
                    k.op("dve", lambda: V.reciprocal(out=Dsm[:], in_=Dsm[:]), [Dsm.b], [Dsm.b])
                    tt("dve", attnT[:, :, 1024:1152], Osm[:], Dsm[:], ALU.mult, [Osm.b, Dsm.b], [attnT.b])
                    k.barrier()
            rS2.close()
            dbg_dump("attnT", attnT[:], [128, 4, TALL], [attnT.b])
            if stop_after == "p2a":
                k.finish()
                return nc

            lh = ExitStack(); top.enter_context(lh)
            hgT = alloc(lh, "hgT", [128, 16, TALL], BF16)
            with ExitStack() as p2b:
                WR = [alloc(p2b, f"wrb{i}", [128, KC, 512], BF16) for i in range(2)]
                hb = make_hgrn_bufs(p2b)
                qT = alloc(p2b, "qT", [128, 1024], BF16)
                AT = alloc(p2b, "AT", [128, 1024], BF16)
                o2 = alloc(p2b, "o2", [128, 1024], BF16)
                Sdb = [alloc(p2b, f"Sdb{i}", [128, 128], BF16) for i in range(2)]
                S0 = alloc(p2b, "S0", [128, 16, 128], F32)
                Sn = alloc(p2b, "Sn", [128, 16, 128], F32)
                sm = {nm: alloc(p2b, "sm_" + nm, [128, 128], F32) for nm in ("a", "b", "c", "d", "e")}
                smb = {nm: alloc(p2b, "smb_" + nm, [128, 128], BF16) for nm in ("kT", "qT", "ktok", "V", "AT", "Vm", "o2")}
                sdec = alloc(p2b, "sdec", [128, 16], F32)
                s1, s2, s3, s4 = hb["s1"], hb["s2"], hb["s3"], hb["s4"]
                def p2b_load(h):
                    state["w"] ^= 1
                    W_ = WR[state["w"]]
                    load_w(W_[:, :, 0:128], w_in[:, FH + h * 128:FH + (h + 1) * 128], W_.b)
                    load_w(W_[:, :, 128:256], w_in[:, IH + h * 128:IH + (h + 1) * 128], W_.b)
                    load_w(W_[:, :, 256:384], w_in[:, QH + h * 128:QH + (h + 1) * 128], W_.b)
                    load_w(W_[:, :, 384:512], w_in[:, OG + h * 128:OG + (h + 1) * 128], W_.b)
                    return W_
                hgrn_set_parity(hb, 0)
                Wnext = p2b_load(0) if "skip_mix" not in dbg else None
                for h in (range(16) if "skip_mix" not in dbg else ()):
                    W = Wnext
                    k.dma("sp", S0[:], st_d[:, h, :, :].rearrange("b k v -> k b v"), writes=[S0.b])
                    hgrn_common(hb, S_all, h, W, 0, 128)
                    if h + 1 < 16:
                        Wnext = p2b_load(h + 1)
                    for half in range(2):
                        for kc in range(KC):
                            k.mm(pbank(half), W[:, kc, 256:384], xnT[:, kc, half * 512:(half + 1) * 512],
                                 start=(kc == 0), stop=(kc == KC - 1), reads=[W.b, xnT.b], writes=[PB[half]])
                    qb = [PB[0], PB[1]]
                    act(s1[:], pwide(0), AF.Sigmoid, qb, [s1.b])
                    act(s4[:], s3[:], AF.Exp, [s3.b], [s4.b])
                    tt("dve", s1[:], pwide(0), s1[:], ALU.mult, qb + [s1.b], [s1.b])
                    tt("dve", qT[:], s1[:], s4[:], ALU.mult, [s1.b, s4.b], [qT.b])
                    for tl in range(8):
                        bi = 2 + tl // 4
                        k.mm(pbank(bi)[:, (tl % 4) * 128:(tl % 4 + 1) * 128], hb["kT"][:, tl * 128:(tl + 1) * 128], qT[:, tl * 128:(tl + 1) * 128],
                             reads=[hb["kT"].b, qT.b], writes=[PB[bi]], inc=(tl % 4 == 3))
                    tt("dve", AT[:], pwide(1), mbd[:].rearrange("p a b -> p (a b)"), ALU.mult, [PB[2], PB[3], mbd.b], [AT.b])

                    def on_chunk(c, sp_ap, sp_b):
                        tl, pr = c // 2, (c % 2) * 64
                        sd = Sdb[c % 2]
                        ts("dve", sd[:], sp_ap, hb["dec"][:, c:c + 1], None, ALU.mult, None, [sp_b, hb["dec"].b], [sd.b])
                        bi = 6 + c // 8
                        o = pbank(bi)[:, (c % 8) * 64:(c % 8 + 1) * 64]
                        k.mm(o, sd[:], qT[:, c * 64:(c + 1) * 64], start=True, stop=False, reads=[sd.b, qT.b], writes=[PB[bi]])
                        k.mm(o, hb["Vb"][pr:pr + 64, tl, :], AT[pr:pr + 64, c * 64:(c + 1) * 64], start=False, stop=True,
                             reads=[hb["Vb"].b, AT.b], writes=[PB[bi]], inc=(c % 8 == 7))

                    hgrn_state_chain(hb, S_all, h, on_chunk=on_chunk, ubanks=(4, 5))
                    k.dma("sp", hp_d[h], S_all[:, h, :], reads=[S_all.b])
                    ob = [PB[6], PB[7]]
                    act(o2[:], pwide(3), AF.Square, ob, [o2.b])
                    for half in range(2):
                        k.mm(pbank(2 + half), ones_b[:, :], o2[:, half * 512:(half + 1) * 512], reads=[ones_b.b, o2.b], writes=[PB[2 + half]])
                    ts("dve", s2[:], pwide(1), 1.0 / 128, EPS, ALU.mult, ALU.add, [PB[2], PB[3]], [s2.b])
                    act(s2[:], s2[:], AF.Ln, [s2.b], [s2.b])
                    act(s2[:], s2[:], AF.Exp, [s2.b], [s2.b], scale=-0.5)
                    tt("dve", s3[:], pwide(3), s2[:], ALU.mult, ob + [s2.b], [s3.b])
                    for half in range(2):
                        for kc in range(KC):
                            k.mm(pbank(half), W[:, kc, 384:512], xnT[:, kc, half * 512:(half + 1) * 512],
                                 start=(kc == 0), stop=(kc == KC - 1), reads=[W.b, xnT.b], writes=[PB[half]])
                    act(s1[:], pwide(0), AF.Sigmoid, qb, [s1.b])
                    tt("dve", s1[:], pwide(0), s1[:], ALU.mult, qb + [s1.b], [s1.b])
                    stt(hgT[:, h, 0:1024], s3[:], hnc[:, 0:1], s1[:], ALU.mult, ALU.mult, [s3.b, hnc.b, s1.b], [hgT.b])

                    sc_ = slice(1024, 1152)
                    pb5 = pbank(4)
                    for which, c0_ in ((0, 0), (2, 256), (3, 384)):
                        for kc in range(KC):
                            k.mm(pb5[:, which * 128:(which + 1) * 128] if which == 0 else pb5[:, (1 if which == 2 else 3) * 128:(2 if which == 2 else 4) * 128],
                                 W[:, kc, c0_:c0_ + 128], xnT[:, kc, sc_], start=(kc == 0), stop=(kc == KC - 1),
                                 reads=[W.b, xnT.b], writes=[PB[4]], inc=False)
                    for kc in range(KC):
                        k.mm(pb5[:, 256:384], xnT[:, kc, sc_], W[:, kc, 128:256], start=(kc == 0), stop=(kc == KC - 1),
                             reads=[W.b, xnT.b], writes=[PB[4]])
                    fh_ps, qh_ps, v_ps, og_ps = pb5[:, 0:128], pb5[:, 128:256], pb5[:, 256:384], pb5[:, 384:512]
                    a_, b_, c_, d_, e_ = sm["a"], sm["b"], sm["c"], sm["d"], sm["e"]
                    act(a_[:], fh_ps, AF.Sigmoid, [PB[4]], [a_.b])
                    act(b_[:], fh_ps, AF.Sigmoid, [PB[4]], [b_.b], scale=-1.0)
                    cp("act", smb["V"][:], v_ps, [PB[4]], [smb["V"].b])
                    ts("dve", a_[:], a_[:], lbc[:, h, 1:2], lbc[:, h, 0:1], ALU.mult, ALU.add, [a_.b, lbc.b], [a_.b])
                    act(a_[:], a_[:], AF.Ln, [a_.b], [a_.b])
                    k.op("dve", lambda: V.tensor_tensor_scan(out=c_[:], data0=rm8[:], data1=a_[:], initial=0.0, op0=ALU.mult, op1=ALU.add),
                         [rm8.b, a_.b], [c_.b])
                    c3 = c_[:].rearrange("p (c t) -> p c t", t=8)
                    act(sdec[:], c3[:, :, 7], AF.Exp, [c_.b], [sdec.b])
                    tt("dve", c3, c3, c3[:, :, 7:8].to_broadcast([128, 16, 8]), ALU.subtract, [c_.b], [c_.b])
                    act(d_[:], c_[:], AF.Exp, [c_.b], [d_.b], scale=-1.0)
                    stt(smb["kT"][:], b_[:], lbc[:, h, 1:2], d_[:], ALU.mult, ALU.mult, [b_.b, lbc.b, d_.b], [smb["kT"].b])
                    act(a_[:], qh_ps, AF.Sigmoid, [PB[4]], [a_.b])
                    act(d_[:], c_[:], AF.Exp, [c_.b], [d_.b])
                    tt("dve", a_[:], qh_ps, a_[:], ALU.mult, [PB[4], a_.b], [a_.b])
                    tt("dve", smb["qT"][:], a_[:], d_[:], ALU.mult, [a_.b, d_.b], [smb["qT"].b])
                    act(e_[:], og_ps, AF.Sigmoid, [PB[4]], [e_.b])
                    tt("dve", e_[:], og_ps, e_[:], ALU.mult, [PB[4], e_.b], [e_.b])
                    k.tr(pbank_bf(5)[:, 0:128], smb["kT"][:], ident_b[:], reads=[smb["kT"].b, ident_b.b], writes=[PB[5]])
                    cp("act", smb["ktok"][:], pbank_bf(5)[:, 0:128], [PB[5]], [smb["ktok"].b])
                    k.mm(pbank(5)[:, 128:256], smb["kT"][:], smb["qT"][:], reads=[smb["kT"].b, smb["qT"].b], writes=[PB[5]])
                    tt("dve", smb["AT"][:], pbank(5)[:, 128:256], mbd8[:], ALU.mult, [PB[5], mbd8.b], [smb["AT"].b])
                    k.mm(pbank(5)[:, 256:384], smb["V"][:], smb["AT"][:], reads=[smb["V"].b, smb["AT"].b], writes=[PB[5]])
                    cp("act", b_[:], pbank(5)[:, 256:384], [PB[5]], [b_.b])
                    for b in range(16):
                        sd = Sdb[b % 2]
                        ts("dve", sd[:], S0[:, b, :], sdec[:, b:b + 1], None, ALU.mult, None, [S0.b, sdec.b], [sd.b])
                        k.mm(pbank(6)[:, b * 8:(b + 1) * 8], sd[:], smb["qT"][:, b * 8:(b + 1) * 8], reads=[sd.b, smb["qT"].b], writes=[PB[6]])
                        ts("dve", smb["Vm"][:], smb["V"][:], sel16[:, b:b + 1], None, ALU.mult, None, [smb["V"].b, sel16.b], [smb["Vm"].b])
                        uo = pbank(7)[:, (b % 4) * 128:(b % 4 + 1) * 128]
                        k.mm(uo, smb["ktok"][:], smb["Vm"][:], reads=[smb["ktok"].b, smb["Vm"].b], writes=[PB[7]])
                        stt(Sn[:, b, :], S0[:, b, :], sdec[:, b:b + 1], uo, ALU.mult, ALU.add, [S0.b, sdec.b, PB[7]], [Sn.b])
                    k.dma("sp", hs_d[:, h, :, :].rearrange("b k v -> k b v"), Sn[:], reads=[Sn.b])
                    tt("dve", b_[:], b_[:], pbank(6)[:, 0:128], ALU.add, [b_.b, PB[6]], [b_.b])
                    act(smb["o2"][:], b_[:], AF.Square, [b_.b], [smb["o2"].b])
                    k.mm(pbank(5)[:, 384:512], ones_b[:, :], smb["o2"][:], reads=[ones_b.b, smb["o2"].b], writes=[PB[5]])
                    ts("dve", a_[:], pbank(5)[:, 384:512], 1.0 / 128, EPS, ALU.mult, ALU.add, [PB[5]], [a_.b])
                    act(a_[:], a_[:], AF.Ln, [a_.b], [a_.b])
                    act(a_[:], a_[:], AF.Exp, [a_.b], [a_.b], scale=-0.5)
                    tt("dve", b_[:], b_[:], a_[:], ALU.mult, [b_.b, a_.b], [b_.b])
                    stt(hgT[:, h, 1024:1152], b_[:], hnc[:, 0:1], e_[:], ALU.mult, ALU.mult, [b_.b, hnc.b, e_.b], [hgT.b])
                k.barrier()
            rS1.close()
            dbg_dump("hgT", hgT[:], [128, 16, TALL], [hgT.b])
            if stop_after == "p2b":
                k.finish()
                return nc

            mT = alloc(rS4, "mT", [128, 16, TALL], BF16, side="right")
            TB3 = ((0, 512), (512, 1024), (1024, 1152))
            with ExitStack() as p2c:
                WR = [alloc(p2c, f"wrc{i}", [128, KC, 512], BF16) for i in range(2)]
                g1 = alloc(p2c, "g1", [128, 512], F32)
                g2 = alloc(p2c, "g2", [128, 512], F32)
                for i in (range(16) if "skip_mix" not in dbg else ()):
                    state["w"] ^= 1
                    W = WR[state["w"]]
                    cs = slice(i * 128, (i + 1) * 128)
                    load_w(W[:, :, 0:128], w_in[:, GA + i * 128:GA + (i + 1) * 128], W.b)
                    load_w(W[:, :, 128:256], w_in[:, GH + i * 128:GH + (i + 1) * 128], W.b)
                    load_w(W[:, :, 256:384], wph[:, cs], W.b)
                    load_w(W[:, 0:4, 384:512], wpa[:, cs], W.b)
                    for (a0, a1) in TB3:
                        n = a1 - a0
                        ba, bb, bc2, bd2 = next_bank(), next_bank(), next_bank(), next_bank()
                        for kc in range(KC):
                            k.mm(pbank(ba)[:, 0:n], W[:, kc, 0:128], xnT[:, kc, a0:a1], start=(kc == 0), stop=(kc == KC - 1), reads=[W.b, xnT.b], writes=[PB[ba]])
                        for kc in range(KC):
                            k.mm(pbank(bb)[:, 0:n], W[:, kc, 128:256], xnT[:, kc, a0:a1], start=(kc == 0), stop=(kc == KC - 1), reads=[W.b, xnT.b], writes=[PB[bb]])
                        for kc in range(4):
                            k.mm(pbank(bc2)[:, 0:n], W[:, kc, 384:512], attnT[:, kc, a0:a1], start=(kc == 0), stop=(kc == 3), reads=[W.b, attnT.b], writes=[PB[bc2]])
                        for kc in range(KC):
                            k.mm(pbank(bd2)[:, 0:n], W[:, kc, 256:384], hgT[:, kc, a0:a1], start=(kc == 0), stop=(kc == KC - 1), reads=[W.b, hgT.b], writes=[PB[bd2]])
                        act(g1[:, 0:n], pbank(ba)[:, 0:n], AF.Sigmoid, [PB[ba]], [g1.b])
                        act(g2[:, 0:n], pbank(bb)[:, 0:n], AF.Sigmoid, [PB[bb]], [g2.b])
                        tt("dve", g1[:, 0:n], g1[:, 0:n], pbank(bc2)[:, 0:n], ALU.mult, [g1.b, PB[bc2]], [g1.b])
                        tt("dve", g2[:, 0:n], g2[:, 0:n], pbank(bd2)[:, 0:n], ALU.mult, [g2.b, PB[bd2]], [g2.b])
                        tt("dve", mT[:, i, a0:a1], g1[:, 0:n], g2[:, 0:n], ALU.add, [g1.b, g2.b], [mT.b])
                k.barrier()
            lh.close()
            la.close()
        dbg_dump("mT", mT[:], [128, 16, TALL], [mT.b])
        if stop_after == "p2c":
            k.finish()
            return nc

        xres = alloc(top, "xres", [128, 9, D], F32)
        for tl in range(9):
            k.dma("sp", xres[:, tl, :], xo[tl * 128:(tl + 1) * 128, :], writes=[xres.b])
        with ExitStack() as p2d:
            WR = [alloc(p2d, f"wrd{i}", [128, KC, 512], BF16) for i in range(2)]
            for cb in (range(4) if "skip_mix" not in dbg else ()):
                state["w"] ^= 1
                W = WR[state["w"]]
                load_w(W[:, :, :], w_out[:, cb * 512:(cb + 1) * 512], W.b)
                for tl in range(9):
                    bi = next_bank()
                    for kc in range(KC):
                        k.mm(pbank(bi), mT[:, kc, tl * 128:(tl + 1) * 128], W[:, kc, :], start=(kc == 0), stop=(kc == KC - 1),
                             reads=[mT.b, W.b], writes=[PB[bi]])
                    xs = xres[:, tl, cb * 512:(cb + 1) * 512]
                    tt("dve", xs, xs, pbank(bi), ALU.add, [xres.b, PB[bi]], [xres.b])
            k.barrier()
        rS4.close()
        import os
        for _ in range(int(os.environ.get("DUMMY_ACT", "0"))):
            act(rs[:, 4:5], rs[:, 4:5], AF.Copy, [rs.b], [rs.b])
        for _ in range(int(os.environ.get("DUMMY_DVE", "0"))):
            cp("dve", rs[:, 5:6], rs[:, 5:6], [rs.b], [rs.b])
        dbg_dump("x1", xres[:], [128, 9, D], [xres.b])
        if stop_after == "p2d":
            k.finish()
            return nc

        SCALE = float(128 ** -0.5)
        with ExitStack() as p3:
            nb = make_norm(p3, n_xt=1)
            checkpoint("p3_alloc")
            load_gain(nb, n_cross)
            checkpoint("p3_gain")
            import os
            for tl in [int(v) for v in os.environ.get("P3_TLIST", "0,1,2,3,4,5,6,7,8").split(",")]:
                x_t = norm_tile(nb, src_sb=(xres[:, tl, :], xres.b))
                if tl == 0:
                    checkpoint("p3_n0")
                transpose_tile(x_t, xnT, tl * 128)
                if tl == 0:
                    checkpoint("p3_t0")
            checkpoint("p3_norm")
            WR = [alloc(p3, f"wre{i}", [128, KC, 256], BF16) for i in range(2)]

            def wslot3():
                state["w"] ^= 1
                return WR[state["w"]]

            memT = alloc(p3, "memT", [128, KC, 256], BF16)
            KmT = alloc(p3, "KmT", [128, 4, 256], BF16)
            Vm = alloc(p3, "Vm", [128, 2, 512], BF16)
            qcT = alloc(p3, "qcT", [128, 4, TALL], BF16)
            ocT = alloc(p3, "ocT", [128, 4, TALL], BF16)
            mst = alloc(p3, "mst", [128, 256], F32)
            load_gain(nb, n_mem)
            for mt in range(2):
                x_t = norm_tile(nb, src_dram=mp_d[mt * 128:(mt + 1) * 128, :])
                transpose_tile(x_t, memT, mt * 128)
            checkpoint("p3_memn")
            for cb in range(4):
                W = wslot3()
                load_w(W[:, :, :], w_ckv[:, cb * 256:(cb + 1) * 256], W.b)
                for mt in range(2):
                    bi = next_bank()
                    for kc in range(KC):
                        k.mm(pbank(bi)[:, 0:256], memT[:, kc, mt * 128:(mt + 1) * 128], W[:, kc, :], start=(kc == 0), stop=(kc == KC - 1),
                             reads=[memT.b, W.b], writes=[PB[bi]])
                    MSK = os.environ.get("MEMKV_SKIP", "")
                    cp("act", mst[:], pbank(bi)[:, 0:256], [PB[bi]], [mst.b])
                    if "dma" not in MSK:
                        k.dma("sp", mkv_d[mt * 128:(mt + 1) * 128, cb * 256:(cb + 1) * 256], mst[:], reads=[mst.b])
                    if cb >= 2 and "vm" not in MSK:
                        cp("dve", Vm[:, mt, (cb - 2) * 256:(cb - 1) * 256], mst[:], [mst.b], [Vm.b])
                if cb < 2 and "kt" not in MSK:
                    for hh in range(2):
                        bi = next_bank()
                        for kc in range(KC):
                            k.mm(pbank(bi)[:, 0:256], W[:, kc, hh * 128:(hh + 1) * 128], memT[:, kc, :], start=(kc == 0), stop=(kc == KC - 1),
                                 reads=[memT.b, W.b], writes=[PB[bi]])
                        cp("act", KmT[:, cb * 2 + hh, :], pbank(bi)[:, 0:256], [PB[bi]], [KmT.b])
            checkpoint("p3_mem")
            for cb in range(2):
                W = wslot3()
                load_w(W[:, :, :], w_cq[:, cb * 256:(cb + 1) * 256], W.b)
                for hh in range(2):
                    h = cb * 2 + hh
                    for (a0, a1) in TB3:
                        n = a1 - a0
                        bi = next_bank()
                        for kc in range(KC):
                            k.mm(pbank(bi)[:, 0:n], W[:, kc, hh * 128:(hh + 1) * 128], xnT[:, kc, a0:a1], start=(kc == 0), stop=(kc == KC - 1),
                                 reads=[W.b, xnT.b], writes=[PB[bi]])
                        cp(evac_eng(), qcT[:, h, a0:a1], pbank(bi)[:, 0:n], [PB[bi]], [qcT.b])
            checkpoint("p3_q")
            Pm = alloc(p3, "Pm", [128, 2, 512], BF16)
            rec = alloc(p3, "rec", [128, 512], F32)
            for h in range(4):
                for tb in range(2):
                    a0, a1 = tb * 512, (tb + 1) * 512
                    for mt in range(2):
                        k.mm(pbank(mt), KmT[:, h, mt * 128:(mt + 1) * 128], qcT[:, h, a0:a1], reads=[KmT.b, qcT.b], writes=[PB[mt]])
                    act(Pm[:].rearrange("p a b -> p (a b)"), pwide(0), AF.Exp, [PB[0], PB[1]], [Pm.b], scale=SCALE)
                    for mt in range(2):
                        k.mm(pbank(2), Vm[:, mt, h * 128:(h + 1) * 128], Pm[:, mt, :], start=(mt == 0), stop=(mt == 1), reads=[Vm.b, Pm.b], writes=[PB[2]])
                    for mt in range(2):
                        k.mm(pbank(3), ones_b[:, :], Pm[:, mt, :], start=(mt == 0), stop=(mt == 1), reads=[ones_b.b, Pm.b], writes=[PB[3]])
                    k.op("dve", lambda: V.reciprocal(out=rec[:], in_=pbank(3)), [PB[3]], [rec.b])
                    tt("dve", ocT[:, h, a0:a1], pbank(2), rec[:], ALU.mult, [PB[2], rec.b], [ocT.b])
            checkpoint("p3_prompt")
            cmb = [alloc(p3, f"cmb{i}", [128, 2, 1024], BF16) for i in range(2)]
            cKT = alloc(p3, "cKT", [128, 2, 4, 128], BF16)
            Psm = alloc(p3, "Psm", [128, 64], BF16)
            Osc = alloc(p3, "Osc", [128, 4, 128], F32)
            Dsc = alloc(p3, "Dsc", [128, 4, 128], F32)
            for b in range(16):
                C = cmb[b % 2]
                k.dma("pool", C[:], cm_d[b].rearrange("(t m) c -> m t c", m=128), writes=[C.b])
                bj = next_bank()
                for mt in range(2):
                    for hh in range(4):
                        k.tr(pbank_bf(bj)[:, (mt * 4 + hh) * 128:(mt * 4 + hh + 1) * 128], C[:, mt, hh * 128:(hh + 1) * 128], ident_b[:],
                             reads=[C.b, ident_b.b], writes=[PB[bj]], inc=(mt == 1 and hh == 3))
                cp(evac_eng(), cKT[:].rearrange("p a h n -> p (a h n)"), pbank_bf(bj), [PB[bj]], [cKT.b])
                bs = next_bank()
                for hh in range(4):
                    for mt in range(2):
                        k.mm(pbank(bs)[:, (hh * 2 + mt) * 8:(hh * 2 + mt + 1) * 8], cKT[:, mt, hh, :], qcT[:, hh, 1024 + b * 8:1024 + (b + 1) * 8],
                             reads=[cKT.b, qcT.b], writes=[PB[bs]], inc=(hh == 3 and mt == 1))
                act(Psm[:], pbank(bs)[:, 0:64], AF.Exp, [PB[bs]], [Psm.b], scale=SCALE)
                bo = next_bank()
                for hh in range(4):
                    for mt in range(2):
                        k.mm(pbank(bo)[:, hh * 8:(hh + 1) * 8], C[:, mt, 512 + hh * 128:512 + (hh + 1) * 128], Psm[:, (hh * 2 + mt) * 8:(hh * 2 + mt + 1) * 8],
                             start=(mt == 0), stop=(mt == 1), reads=[C.b, Psm.b], writes=[PB[bo]], inc=False)
                    for mt in range(2):
                        k.mm(pbank(bo)[:, 32 + hh * 8:32 + (hh + 1) * 8], ones_b[:, :], Psm[:, (hh * 2 + mt) * 8:(hh * 2 + mt + 1) * 8],
                             start=(mt == 0), stop=(mt == 1), reads=[ones_b.b, Psm.b], writes=[PB[bo]], inc=(hh == 3 and mt == 1))
                cp("dve", Osc[:, :, b * 8:(b + 1) * 8], pbank(bo)[:, 0:32].rearrange("p (h i) -> p h i", h=4), [PB[bo]], [Osc.b])
                cp("dve", Dsc[:, :, b * 8:(b + 1) * 8], pbank(bo)[:, 32:64].rearrange("p (h i) -> p h i", h=4), [PB[bo]], [Dsc.b])
            k.op("dve", lambda: V.reciprocal(out=Dsc[:], in_=Dsc[:]), [Dsc.b], [Dsc.b])
            tt("dve", ocT[:, :, 1024:1152], Osc[:], Dsc[:], ALU.mult, [Osc.b, Dsc.b], [ocT.b])
            checkpoint("p3_sample")
            for cb in range(8):
                W = wslot3()
                load_w(W[:, 0:4, :], w_co[:, cb * 256:(cb + 1) * 256], W.b)
                for tl in range(9):
                    bi = next_bank()
                    for kc in range(4):
                        k.mm(pbank(bi)[:, 0:256], ocT[:, kc, tl * 128:(tl + 1) * 128], W[:, kc, :], start=(kc == 0), stop=(kc == 3),
                             reads=[ocT.b, W.b], writes=[PB[bi]])
                    xs = xres[:, tl, cb * 256:(cb + 1) * 256]
                    tt("dve", xs, xs, pbank(bi)[:, 0:256], ALU.add, [xres.b, PB[bi]], [xres.b])
            k.barrier()
        dbg_dump("x2", xres[:], [128, 9, D], [xres.b])
        if stop_after == "p3":
            k.finish()
            return nc

        cw = alloc(top, "cw", [128, 9, 32], F32)
        with ExitStack() as p4n:
            nb = make_norm(p4n, n_xt=1)
            load_gain(nb, n_ffn)
            x32 = alloc(p4n, "x32", [128, KC, 128], F32)
            wr32 = alloc(p4n, "wr32", [128, KC, 36], F32)
            bb = alloc(p4n, "bb", [128, 36], F32)
            L = alloc(p4n, "L", [128, 36], F32)
            r_ = alloc(p4n, "r_", [128, 64], F32)
            k.dma("sp", wr32[:], w_r.rearrange("(kc p) c -> p kc c", p=128), writes=[wr32.b])
            k.dma("sp", bb[:], b_r.partition_broadcast(128), writes=[bb.b])
            for tl in range(9):
                x_t = norm_tile(nb, src_sb=(xres[:, tl, :], xres.b))
                transpose_tile(x_t, xnT, tl * 128, f32dst=x32)
                bi = next_bank()
                for kc in range(KC):
                    k.mm(pbank(bi)[:, 0:36], x32[:, kc, :], wr32[:, kc, :], start=(kc == 0), stop=(kc == KC - 1), reads=[x32.b, wr32.b], writes=[PB[bi]])
                tt("dve", L[:], pbank(bi)[:, 0:36], bb[:], ALU.add, [PB[bi], bb.b], [L.b])
                R_ = [r_.b]
                k.op("dve", lambda: V.reduce_max(out=r_[:, 0:1], in_=L[:, 0:4], axis=AX.X), [L.b], R_)
                ts("dve", r_[:, 4:8], L[:, 0:4], r_[:, 0:1], None, ALU.subtract, None, [L.b] + R_, R_)
                act(r_[:, 4:8], r_[:, 4:8], AF.Exp, R_, R_, accum_out=r_[:, 1:2])
                k.op("dve", lambda: V.reciprocal(out=r_[:, 2:3], in_=r_[:, 1:2]), R_, R_)
                ts("dve", r_[:, 8:12], L[:, 0:4], r_[:, 0:1], None, ALU.is_ge, None, [L.b] + R_, R_)
                ts("dve", r_[:, 16:24], L[:, 4:12], r_[:, 8:9], None, ALU.mult, None, [L.b] + R_, R_)
                for g in range(1, 4):
                    stt(r_[:, 16:24], L[:, 4 + 8 * g:12 + 8 * g], r_[:, 8 + g:9 + g], r_[:, 16:24], ALU.mult, ALU.add, [L.b] + R_, R_)
                k.op("dve", lambda: V.reduce_max(out=r_[:, 3:4], in_=r_[:, 16:24], axis=AX.X), R_, R_)
                ts("dve", r_[:, 24:32], r_[:, 16:24], r_[:, 3:4], None, ALU.is_ge, None, R_, R_)
                stt(r_[:, 32:40], r_[:, 24:32], -1e30, r_[:, 16:24], ALU.mult, ALU.add, R_, R_)
                k.op("dve", lambda: V.reduce_max(out=r_[:, 12:13], in_=r_[:, 32:40], axis=AX.X), R_, R_)
                ts("dve", r_[:, 40:48], r_[:, 32:40], r_[:, 12:13], None, ALU.is_ge, None, R_, R_)
                tt("dve", r_[:, 13:14], r_[:, 12:13], r_[:, 3:4], ALU.subtract, R_, R_)
                act(r_[:, 13:14], r_[:, 13:14], AF.Exp, R_, R_)
                ts("dve", r_[:, 14:15], r_[:, 13:14], 1.0, None, ALU.add, None, R_, R_)
                k.op("dve", lambda: V.reciprocal(out=r_[:, 14:15], in_=r_[:, 14:15]), R_, R_)
                tt("dve", r_[:, 14:15], r_[:, 14:15], r_[:, 2:3], ALU.mult, R_, R_)
                tt("dve", r_[:, 15:16], r_[:, 14:15], r_[:, 13:14], ALU.mult, R_, R_)
                ts("dve", r_[:, 48:56], r_[:, 24:32], r_[:, 14:15], None, ALU.mult, None, R_, R_)
                stt(r_[:, 48:56], r_[:, 40:48], r_[:, 15:16], r_[:, 48:56], ALU.mult, ALU.add, R_, R_)
                for g in range(4):
                    ts("dve", cw[:, tl, g * 8:(g + 1) * 8], r_[:, 48:56], r_[:, 8 + g:9 + g], None, ALU.mult, None, R_, [cw.b])
            k.barrier()
        dbg_dump("cw", cw[:], [128, 9, 32], [cw.b])
        with ExitStack() as p4:
            GU = [alloc(p4, f"gu{i}", [128, KC, 256], BF16) for i in range(3)]
            WD = [alloc(p4, f"wd{i}", [128, 4, D], BF16) for i in range(2)]
            hT = alloc(p4, "hT", [128, 4, TALL], BF16)
            sg = [alloc(p4, f"sg{i}", [128, 512], F32) for i in range(2)]
            gi = 0
            for e in range(N_EXP):
                Wd = WD[e % 2]
                for fb in range(4):
                    W = GU[gi % 3]; gi += 1
                    load_w(W[:, :, 0:128], weg[e, :, fb * 128:(fb + 1) * 128], W.b)
                    load_w(W[:, :, 128:256], weu[e, :, fb * 128:(fb + 1) * 128], W.b)
                    if fb == 1:
                        k.dma("pool", Wd[:], wed[e].rearrange("(kc p) c -> p kc c", p=128), writes=[Wd.b])
                    for ti_, (a0, a1) in enumerate(TB3):
                        n = a1 - a0
                        ba, bb2 = next_bank(), next_bank()
                        for kc in range(KC):
                            k.mm(pbank(ba)[:, 0:n], W[:, kc, 0:128], xnT[:, kc, a0:a1], start=(kc == 0), stop=(kc == KC - 1), reads=[W.b, xnT.b], writes=[PB[ba]])
                        for kc in range(KC):
                            k.mm(pbank(bb2)[:, 0:n], W[:, kc, 128:256], xnT[:, kc, a0:a1], start=(kc == 0), stop=(kc == KC - 1), reads=[W.b, xnT.b], writes=[PB[bb2]])
                        s_ = sg[ti_ % 2]
                        act(s_[:, 0:n], pbank(ba)[:, 0:n], AF.Silu, [PB[ba]], [s_.b])
                        tt("dve", hT[:, fb, a0:a1], s_[:, 0:n], pbank(bb2)[:, 0:n], ALU.mult, [s_.b, PB[bb2]], [hT.b])
                for tl in range(9):
                    for cb in range(4):
                        bi = next_bank()
                        for fb in range(4):
                            k.mm(pbank(bi), hT[:, fb, tl * 128:(tl + 1) * 128], Wd[:, fb, cb * 512:(cb + 1) * 512], start=(fb == 0), stop=(fb == 3),
                                 reads=[hT.b, Wd.b], writes=[PB[bi]])
                        xs = xres[:, tl, cb * 512:(cb + 1) * 512]
                        stt(xs, pbank(bi), cw[:, tl, e:e + 1], xs, ALU.mult, ALU.add, [PB[bi], cw.b, xres.b], [xres.b])
            k.barrier()
        dbg_dump("x3", xres[:], [128, 9, D], [xres.b])

        with ExitStack() as p5:
            nb = make_norm(p5, n_xt=2)
            load_gain(nb, n_fin)
            for tl in range(9):
                x_t = norm_tile(nb, src_sb=(xres[:, tl, :], xres.b))
                k.dma("sp", y_d[tl * 128:(tl + 1) * 128, :], x_t[:, :], reads=[x_t.b])
            k.finish()
    return nc


_CACHE = {}


def make_core_inputs(c, inp):
    s, j = c // 4, c % 4
    f32 = np.float32
    xp = inp["x_prompt"]
    xh = np.zeros((NHB * 1024, D), f32)
    lo = 1024 * j - NHB * 1024
    for beta in range(NHB):
        t0 = lo + beta * 1024
        if t0 >= 0:
            xh[beta * 1024:(beta + 1) * 1024] = xp[s, t0:t0 + 1024]
    xo = np.concatenate([xp[s, 1024 * j:1024 * (j + 1)], inp["x_sample"][16 * c:16 * (c + 1)].reshape(128, D)], axis=0)
    hval = np.zeros((128, 3), f32)
    for beta in range(NHB):
        hval[:, beta] = 1.0 if (lo + beta * 1024) >= 0 else 0.0
    pos0 = np.full((128, 1), 1024.0 * j, f32)
    sl = slice(16 * c, 16 * (c + 1))
    m = {
        "xh": xh, "xo": np.ascontiguousarray(xo), "hval": hval, "pos0": pos0,
        "c1": np.ascontiguousarray(inp["cache_swa1"][0, sl].reshape(16, 128, 1024)),
        "c2": np.ascontiguousarray(inp["cache_swa2"][0, sl].reshape(16, 512, 1024)),
        "c3": np.ascontiguousarray(inp["cache_swa3"][0, sl].reshape(16, 2048, 1024)),
        "st": np.ascontiguousarray(inp["state_hgrn"][0, sl]),
        "cm": np.ascontiguousarray(inp["cache_mem_kv"][0, sl].reshape(16, 256, 1024)),
        "mp": np.ascontiguousarray(inp["mem_prompt"][s]),
    }
    return m


def shared_inputs(inp):
    f32 = np.float32
    g = lambda n: np.ascontiguousarray(np.asarray(inp[n], f32))
    return {
        "lbl": g("hgrn_lb_logits"),
        "n_mix": g("norm_mix")[0], "n_cross": g("norm_cross")[0], "n_mem": g("norm_mem")[0],
        "n_ffn": g("norm_ffn")[0], "n_fin": g("norm_final"), "hn": g("hgrn_norm")[0],
        "w_in": g("w_in")[0], "wpa": g("w_proj_attn")[0], "wph": g("w_proj_hgrn")[0], "w_out": g("w_out")[0],
        "w_cq": g("w_cq")[0], "w_ckv": g("w_ckv")[0], "w_co": g("w_co")[0],
        "w_r": np.ascontiguousarray(np.concatenate([g("w_rg")[0], g("w_re")[0]], axis=1)),
        "b_r": np.ascontiguousarray(np.concatenate([g("b_rg")[0], g("b_re")[0]], axis=0)),
        "weg": g("w_e_gate")[0].reshape(N_EXP, D, 512), "weu": g("w_e_up")[0].reshape(N_EXP, D, 512),
        "wed": g("w_e_down")[0].reshape(N_EXP, 512, D),
    }


def assemble(res):
    f32 = np.float32
    y_p = np.zeros((2, 4096, D), f32); y_s = np.zeros((128, 8, D), f32)
    swa_p = [np.zeros((1, 2, w, 2, 4, 128), f32) for w in (128, 512, 2048)]
    swa_s = [np.zeros((1, 128, 8, 2, 4, 128), f32) for _ in range(3)]
    hg_p = np.zeros((1, 2, 16, 128, 128), f32); hg_s = np.zeros((1, 128, 16, 128, 128), f32)
    mkv = np.zeros((1, 2, 256, 2, 4, 128), f32)
    for c in range(8):
        r = res[c]
        s, j = c // 4, c % 4
        y_p[s, 1024 * j:1024 * (j + 1)] = r["y"][0:1024]
        y_s[16 * c:16 * (c + 1)] = r["y"][1024:].reshape(16, 8, D)
        kv = r["kvo"]
        for g in range(3):
            swa_s[g][0, 16 * c:16 * (c + 1)] = kv[g, 1024:1152].reshape(16, 8, 2, 4, 128)
        if j == 3:
            swa_p[0][0, s] = kv[0, 896:1024].reshape(128, 2, 4, 128)
            swa_p[1][0, s] = kv[1, 512:1024].reshape(512, 2, 4, 128)
            swa_p[2][0, s, 1024:2048] = kv[2, 0:1024].reshape(1024, 2, 4, 128)
            hg_p[0, s] = r["hp"]
        if j == 2:
            swa_p[2][0, s, 0:1024] = kv[2, 0:1024].reshape(1024, 2, 4, 128)
        if j == 0:
            mkv[0, s] = r["mkv"].reshape(256, 2, 4, 128)
        hg_s[0, 16 * c:16 * (c + 1)] = r["hs"]
    return (y_p, y_s, swa_p[0], swa_p[1], swa_p[2], hg_p, mkv, swa_s[0], swa_s[1], swa_s[2], hg_s)


def kernel(**inputs):
    inp = {k_: np.asarray(v) for k_, v in inputs.items()}
    if "nc" not in _CACHE:
        _CACHE["nc"] = build_program()
    nc = _CACHE["nc"]
    shared = shared_inputs(inp)
    in_maps = []
    for c in range(8):
        m = make_core_inputs(c, inp)
        m.update(shared)
        in_maps.append(m)
    res = run_bass_kernel_spmd(nc, in_maps, core_ids=list(range(8)))
    return assemble(res.results)
```
